# Optimizing a Trainium2 kernel written in Bass

```python
import math
import jax, jax.numpy as jnp
from jax import lax
import numpy as np

D_MODEL = 1024
BATCH = 4
SEQ = 4096
DEPTH = 1

N_HEADS = 8
HEAD_DIM = D_MODEL // (2 * N_HEADS)
ATTN_WIDTH = N_HEADS * 2 * HEAD_DIM
ROT_DIM = HEAD_DIM // 4
ROPE_THETA = 500000.0
Q_BLOCK = 128
POOL_WINDOWS = (2, 4, 8, 16)
N_POOL_GROUPS = len(POOL_WINDOWS)
POOL_WIDTH = D_MODEL
POOL_GROUP_DIM = POOL_WIDTH // N_POOL_GROUPS
IN_COLS = POOL_WIDTH + 3 * ATTN_WIDTH + 2 * D_MODEL
N_EXPERTS = 16
EC_CAPACITY_FACTOR = 2
D_FF = ((8 * D_MODEL // 3 + 255) // 256) * 256
RMS_EPS = 1e-6
POS_OFFSET_MAX = 1024

kernel_name = "hybrid_pool_diffattn_ec_moe_encoder"


def rms_norm(x, g):
    xf = x.astype(jnp.float32)
    y = xf * lax.rsqrt(jnp.mean(xf * xf, axis=-1, keepdims=True) + RMS_EPS)
    return y.astype(x.dtype) * g


def pool_mixer(u, w_mix, scale):
    B, S, _ = u.shape
    ug = u.reshape(B, S, N_POOL_GROUPS, POOL_GROUP_DIM)
    cs = jnp.cumsum(ug.astype(jnp.float32), axis=1)
    cs = jnp.pad(cs, ((0, 0), (1, 0), (0, 0), (0, 0)))
    t = jnp.arange(S)
    outs = []
    for g, w in enumerate(POOL_WINDOWS):
        lo = jnp.clip(t - w // 2, 0, S)
        hi = jnp.clip(t + w // 2, 0, S)
        csg = cs[:, :, g]
        win = jnp.take(csg, hi, axis=1) - jnp.take(csg, lo, axis=1)
        cnt = (hi - lo).astype(jnp.float32)[None, :, None]
        pooled = (win / cnt).astype(u.dtype) - ug[:, :, g]
        outs.append(jnp.einsum('bsc,cd->bsd', pooled, w_mix[g]))
    return jnp.concatenate(outs, axis=-1) * scale


def rotary(x, pos):
    half = ROT_DIM // 2
    inv = ROPE_THETA ** (-jnp.arange(half, dtype=jnp.float32) * 2.0 / ROT_DIM)
    ang = pos.astype(jnp.float32)[..., None] * inv
    cos = jnp.cos(ang)[:, :, None, None, :]
    sin = jnp.sin(ang)[:, :, None, None, :]
    x1 = x[..., :half].astype(jnp.float32)
    x2 = x[..., half:ROT_DIM].astype(jnp.float32)
    r = jnp.concatenate([x1 * cos - x2 * sin, x2 * cos + x1 * sin], axis=-1).astype(x.dtype)
    return jnp.concatenate([r, x[..., ROT_DIM:]], axis=-1)


def diff_attention(q, k, v, lam):
    B, S = q.shape[:2]
    nb = S // Q_BLOCK
    qb = q.reshape(B, nb, Q_BLOCK, N_HEADS, 2, HEAD_DIM).transpose(1, 0, 2, 3, 4, 5)
    scale = HEAD_DIM ** -0.5

    def one_block(qblk):
        s = jnp.einsum('bqhmd,bkhmd->bhmqk', qblk, k, preferred_element_type=jnp.float32) * scale
        p = jax.nn.softmax(s, axis=-1)
        a = p[:, :, 0] - lam * p[:, :, 1]
        return jnp.einsum('bhqk,bkhe->bqhe', a.astype(v.dtype), v)

    o = lax.map(one_block, qb)
    return o.transpose(1, 0, 2, 3, 4).reshape(B, S, N_HEADS, 2 * HEAD_DIM)


def expert_choice_ffn(h, w_router, w_gate, w_up, w_down):
    B, S, D = h.shape
    cap = EC_CAPACITY_FACTOR * S // N_EXPERTS
    logits = jnp.einsum('bsd,de->bse', h, w_router, preferred_element_type=jnp.float32)
    aff = jax.nn.softmax(logits, axis=-1)
    vals, idx = lax.top_k(aff.transpose(0, 2, 1), cap)
    xs = jax.vmap(lambda hb, ib: hb[ib])(h, idx.reshape(B, -1)).reshape(B, N_EXPERTS, cap, D)
    g = jnp.einsum('becd,edf->becf', xs, w_gate)
    u = jnp.einsum('becd,edf->becf', xs, w_up)
    y = jnp.einsum('becf,efd->becd', jax.nn.silu(g) * u, w_down) * vals[..., None].astype(h.dtype)
    flat = (jnp.arange(B)[:, None, None] * S + idx).reshape(-1)
    out = jax.ops.segment_sum(y.reshape(-1, D), flat, num_segments=B * S)
    return out.reshape(B, S, D)


def setup_inputs(seed: int = 0) -> dict:
    key = jax.random.key(seed)
    ks = jax.random.split(key, 24)
    f32 = jnp.float32
    nrm = lambda k, shape, fan_in: jax.random.normal(k, shape, f32) * (fan_in ** -0.5)
    x = jax.random.normal(ks[0], (BATCH, SEQ, D_MODEL), f32)
    offsets = jax.random.randint(ks[1], (BATCH, 1), 0, POS_OFFSET_MAX, dtype=jnp.int32)
    positions = offsets + jnp.arange(SEQ, dtype=jnp.int32)[None, :]
    return {
        "x": x,
        "positions": positions,
        "g_mix": 1.0 + 0.02 * jax.random.normal(ks[2], (DEPTH, D_MODEL), f32),
        "w_in": nrm(ks[3], (DEPTH, D_MODEL, IN_COLS), D_MODEL),
        "w_pool_mix": nrm(ks[4], (DEPTH, N_POOL_GROUPS, POOL_GROUP_DIM, POOL_GROUP_DIM), POOL_GROUP_DIM),
        "pool_scale": 1.0 + 0.02 * jax.random.normal(ks[5], (DEPTH, POOL_WIDTH), f32),
        "w_pool_out": nrm(ks[6], (DEPTH, POOL_WIDTH, D_MODEL), POOL_WIDTH),
        "lam_q1": 0.1 * jax.random.normal(ks[7], (DEPTH, HEAD_DIM), f32),
        "lam_k1": 0.1 * jax.random.normal(ks[8], (DEPTH, HEAD_DIM), f32),
        "lam_q2": 0.1 * jax.random.normal(ks[9], (DEPTH, HEAD_DIM), f32),
        "lam_k2": 0.1 * jax.random.normal(ks[10], (DEPTH, HEAD_DIM), f32),
        "g_subln": 1.0 + 0.02 * jax.random.normal(ks[11], (DEPTH, 2 * HEAD_DIM), f32),
        "w_attn_out": nrm(ks[12], (DEPTH, ATTN_WIDTH, D_MODEL), ATTN_WIDTH),
        "w_out": nrm(ks[13], (DEPTH, D_MODEL, D_MODEL), D_MODEL),
        "g_ffn": 1.0 + 0.02 * jax.random.normal(ks[14], (DEPTH, D_MODEL), f32),
        "w_router": nrm(ks[15], (DEPTH, D_MODEL, N_EXPERTS), D_MODEL),
        "w_gate": nrm(ks[16], (DEPTH, N_EXPERTS, D_MODEL, D_FF), D_MODEL),
        "w_up": nrm(ks[17], (DEPTH, N_EXPERTS, D_MODEL, D_FF), D_MODEL),
        "w_down": nrm(ks[18], (DEPTH, N_EXPERTS, D_FF, D_MODEL), D_FF),
        "g_final": 1.0 + 0.02 * jax.random.normal(ks[19], (D_MODEL,), f32),
    }


def reference(x, positions, g_mix, w_in, w_pool_mix, pool_scale, w_pool_out, lam_q1, lam_k1,
              lam_q2, lam_k2, g_subln, w_attn_out, w_out, g_ffn, w_router, w_gate, w_up, w_down,
              g_final):
    B, S, _ = x.shape
    c_q = POOL_WIDTH
    c_k = c_q + ATTN_WIDTH
    c_v = c_k + ATTN_WIDTH
    c_gp = c_v + ATTN_WIDTH
    c_ga = c_gp + D_MODEL
    for l in range(DEPTH):
        lam_init = 0.8 - 0.6 * math.exp(-0.3 * l)
        h = rms_norm(x, g_mix[l])
        z = jnp.einsum('bsd,dc->bsc', h, w_in[l])
        u_pool = z[..., :c_q]
        q = z[..., c_q:c_k].reshape(B, S, N_HEADS, 2, HEAD_DIM)
        k = z[..., c_k:c_v].reshape(B, S, N_HEADS, 2, HEAD_DIM)
        v = z[..., c_v:c_gp].reshape(B, S, N_HEADS, 2 * HEAD_DIM)
        gate_pool = jax.nn.sigmoid(z[..., c_gp:c_ga])
        gate_attn = jax.nn.sigmoid(z[..., c_ga:])
        y_pool = jnp.einsum('bsc,cd->bsd', pool_mixer(u_pool, w_pool_mix[l], pool_scale[l]), w_pool_out[l])
        q = rotary(q, positions)
        k = rotary(k, positions)
        lam = (jnp.exp(jnp.sum(lam_q1[l].astype(jnp.float32) * lam_k1[l].astype(jnp.float32)))
               - jnp.exp(jnp.sum(lam_q2[l].astype(jnp.float32) * lam_k2[l].astype(jnp.float32)))
               + lam_init)
        o = diff_attention(q, k, v, lam)
        o = rms_norm(o, g_subln[l]) * (1.0 - lam_init)
        y_attn = jnp.einsum('bsc,cd->bsd', o.reshape(B, S, ATTN_WIDTH), w_attn_out[l])
        merged = gate_pool * y_pool + gate_attn * y_attn
        x = x + jnp.einsum('bsd,de->bse', merged, w_out[l])
        x = x + expert_choice_ffn(rms_norm(x, g_ffn[l]), w_router[l], w_gate[l], w_up[l], w_down[l])
    return rms_norm(x, g_final)
```

```python
import math
from contextlib import ExitStack

import numpy as np
import ml_dtypes
import concourse.bass as bass
import concourse.mybir as mybir
from concourse.bass_utils import run_bass_kernel_spmd

F32 = mybir.dt.float32
BF16 = mybir.dt.bfloat16
I32 = mybir.dt.int32
AF = mybir.ActivationFunctionType
ALU = mybir.AluOpType
AX = mybir.AxisListType

S = 4096
D = 1024
NT = 32
HQ = 2048
NH = 8
NE = 16
CAP = 512
DFF = 2816
NFC = 22
EPS = 1e-6
SCALE = 0.125
LAM_INIT = 0.8 - 0.6 * math.exp(0.0)
ROPE_THETA = 500000.0
WINS = (2, 4, 8, 16)
C_Q, C_K, C_V, C_GP, C_GA = 1024, 2048, 3072, 4096, 5120

ENGS = ("pe", "act", "dve", "pool", "sp")
DMAQ = ("sp", "act", "pool")
NDS = 14


class _Rec:
    __slots__ = ("eng", "fn", "deps", "is_dma", "sig", "sigval", "dsem", "dval", "prevwait", "stage")

    def __init__(self, eng, fn, is_dma, stage):
        self.eng = eng
        self.fn = fn
        self.deps = set()
        self.is_dma = is_dma
        self.sig = False
        self.sigval = 0
        self.dsem = None
        self.dval = 0
        self.prevwait = None
        self.stage = stage


class Prog:
    def __init__(self, nc, sems, dsems):
        self.nc = nc
        self.sems = sems
        self.dsems = dsems
        self.pending = {e: [] for e in ENGS}
        self.bufs = {}
        self.stage = 0
        self.sigc = {e: 0 for e in ENGS}
        self.dcount = {e: [0] * NDS for e in DMAQ}
        self.di = {e: 0 for e in DMAQ}
        self.waited = {e: {} for e in ENGS}

    def op(self, eng, fn, reads=(), writes=(), dma=False):
        r = _Rec(eng, fn, dma, self.stage)
        for k in reads:
            b = self.bufs.setdefault(k, [None, []])
            if b[0] is not None:
                r.deps.add(b[0])
        for k in writes:
            b = self.bufs.setdefault(k, [None, []])
            if b[0] is not None:
                d = b[0]
                if dma or d.is_dma or d.eng != eng:
                    r.deps.add(d)
            for d in b[1]:
                if dma or d.is_dma or d.eng != eng:
                    r.deps.add(d)
        for k in reads:
            self.bufs[k][1].append(r)
        for k in writes:
            self.bufs[k] = [r, []]
        r.deps.discard(r)
        self.pending[eng].append(r)
        return r

    def dma(self, eng, out, in_, reads=(), writes=(), **kw):
        return self.op(eng, lambda e: e.dma_start(out=out, in_=in_, **kw), reads, writes, dma=True)

    def flush(self):
        nc = self.nc
        st = self.stage
        allr = [r for e in ENGS for r in self.pending[e]]
        for r in allr:
            r.deps = {d for d in r.deps if d.stage == st}
            for d in r.deps:
                if not d.is_dma:
                    d.sig = True
        for e in ENGS:
            lastc = None
            for r in self.pending[e]:
                if not r.is_dma:
                    lastc = r
            if lastc is not None:
                lastc.sig = True
        bar = []
        if st > 0:
            for e in ENGS:
                if self.sigc[e] > 0:
                    bar.append((self.sems[e], self.sigc[e]))
            for e in DMAQ:
                for s in range(NDS):
                    if self.dcount[e][s] > 0:
                        bar.append((self.dsems[e][s], 16 * self.dcount[e][s]))
        for e in ENGS:
            for r in self.pending[e]:
                if r.is_dma:
                    s = self.di[e] % NDS
                    self.di[e] += 1
                    r.dsem = self.dsems[e][s]
                    if self.dcount[e][s] > 0:
                        r.prevwait = (r.dsem, 16 * self.dcount[e][s])
                    self.dcount[e][s] += 1
                    r.dval = 16 * self.dcount[e][s]
                elif r.sig:
                    self.sigc[e] += 1
                    r.sigval = self.sigc[e]
        pend = self.pending
        sems = self.sems
        waited_all = self.waited

        def run(e, engobj):
            waited = waited_all[e]

            def w(s, v):
                if waited.get(id(s), 0) >= v:
                    return
                waited[id(s)] = v
                engobj.wait_ge(s, v)

            for (s, v) in bar:
                w(s, v)
            for r in pend[e]:
                if r.prevwait is not None:
                    w(*r.prevwait)
                for d in r.deps:
                    if d.is_dma:
                        w(d.dsem, d.dval)
                    else:
                        w(sems[d.eng], d.sigval)
                ins = r.fn(engobj)
                if r.is_dma:
                    ins.then_inc(r.dsem, 16)
                elif r.sig:
                    ins.then_inc(sems[e], 1)

        with nc.Block() as block:
            @block.sync
            def _(eng):
                run("sp", eng)

            @block.scalar
            def _(eng):
                run("act", eng)

            @block.vector
            def _(eng):
                run("dve", eng)

            @block.gpsimd
            def _(eng):
                run("pool", eng)

            @block.tensor
            def _(eng):
                run("pe", eng)

        self.pending = {e: [] for e in ENGS}
        self.stage += 1

    def finish(self):
        nc = self.nc
        fin = []
        for e in DMAQ:
            for s in range(NDS):
                if self.dcount[e][s] > 0:
                    fin.append((self.dsems[e][s], 16 * self.dcount[e][s]))
        for e in ENGS:
            if self.sigc[e] > 0:
                fin.append((self.sems[e], self.sigc[e]))
        with nc.Block() as block:
            @block.sync
            def _(eng):
                for (s, v) in fin:
                    eng.wait_ge(s, v)


def _consts():
    cf = {}
    p = np.arange(128)
    cf["idf"] = np.eye(128, dtype=np.float32)
    cf["ones"] = np.ones((128, 128), np.float32)
    cf["utri"] = (p[:, None] <= p[None, :]).astype(np.float32)
    cf["gsum"] = ((p[:, None] // 8) == (p[None, :] // 8)).astype(np.float32)
    cf["iota"] = np.tile(np.arange(512, dtype=np.float32)[None, :], (128, 1))
    half = 8
    inv = ROPE_THETA ** (-np.arange(half, dtype=np.float32) * 2.0 / 16)
    rinv = np.zeros((128, 4), np.float32)
    for base in (0, 64):
        for j in range(8):
            rinv[base + j, 0] = inv[j]
            rinv[base + 8 + j, 0] = inv[j]
            rinv[base + j, 1] = -1.0
            rinv[base + 8 + j, 1] = 1.0
    cf["rinv"] = rinv
    T = np.arange(S).reshape(NT, 128).T
    thl = np.zeros((128, NT, 2), np.float32)
    thl[:, :, 0] = T // 64
    thl[:, :, 1] = T % 64
    cf["thl"] = thl.reshape(128, NT * 2)
    names = ["idf", "ones", "utri", "gsum", "iota", "rinv", "thl"]
    offs = {}
    o = 0
    for n in names:
        offs[n] = (o, cf[n].shape[1])
        o += cf[n].shape[1]
    cfa = np.concatenate([cf[n] for n in names], axis=1).astype(np.float32)
    perm = np.zeros((128, 128), np.float32)
    for base in (0, 64):
        for j in range(8):
            perm[base + j + 8, base + j] = 1.0
            perm[base + j, base + j + 8] = 1.0
    cb = np.concatenate([np.eye(128, dtype=np.float32), perm, np.ones((128, 128), np.float32)], axis=1).astype(ml_dtypes.bfloat16)
    return cfa, offs, cb


def _fix_tables(hf):
    fs = np.ones((128, 4, 8), np.float32)
    fe = np.ones((128, 4, 8), np.float32)
    for g, w in enumerate(WINS):
        for j in range(8):
            if hf == 0:
                t = j
                lo = max(t - w // 2, 0)
                hi = min(t + w // 2, S)
                fs[:, g, j] = w / (hi - lo)
            if hf == 1:
                t = S - 8 + j
                lo = max(t - w // 2, 0)
                hi = min(t + w // 2, S)
                fe[:, g, j] = w / (hi - lo)
    return np.concatenate([fs.reshape(128, 32), fe.reshape(128, 32)], axis=1)


def build(stop_after=99, dbg=False):
    nc = bass.Bass("TRN2", target_bir_lowering=False)
    cfa_np, CO, cb_np = _consts()
    NCF = cfa_np.shape[1]

    def din(name, shape, dt=F32):
        return nc.dram_tensor(name, list(shape), dt, kind="ExternalInput").ap()

    x = din("x", [S, D])
    xhalo = din("xhalo", [2, 256, D])
    pos = din("pos", [S], I32)
    g_mix = din("g_mix", [D])
    w_in = din("w_in", [D, 6144])
    w_pool_mix = din("w_pool_mix", [4, 256, 256])
    pool_scale = din("pool_scale", [D])
    w_pool_out = din("w_pool_out", [D, D])
    lamv = din("lamv", [4, 64])
    g_subln = din("g_subln", [128])
    w_attn_out = din("w_attn_out", [D, D])
    w_out = din("w_out", [D, D])
    g_ffn = din("g_ffn", [D])
    w_router = din("w_router", [D, NE])
    if stop_after >= 4:
        w_gate = din("w_gate", [NE, D, DFF])
        w_up = din("w_up", [NE, D, DFF])
        w_down = din("w_down", [NE, DFF, D])
    g_final = din("g_final", [D])
    cfa = din("cfa", [128, NCF])
    cba = din("cba", [128, 384], BF16)
    fixt = din("fixt", [2, 128, 64])
    out = nc.dram_tensor("out", [S, D], F32, kind="ExternalOutput").ap()

    def dscr(name, shape, dt):
        kind = "ExternalOutput" if dbg else "Internal"
        return nc.dram_tensor(name, list(shape), dt, kind=kind).ap()

    Abuf = dscr("Abuf", [2, D, HQ], BF16)
    Gabuf = dscr("Gabuf", [2, D, HQ], BF16)
    oTbuf = dscr("oTbuf", [2, D, HQ], BF16)
    x1buf = dscr("x1buf", [S, D], F32)
    h2buf = dscr("h2buf", [S, D], BF16)
    affbuf = dscr("affbuf", [S, NE], F32)
    affTbuf = dscr("affTbuf", [NE, S], F32)
    taubuf = dscr("taubuf", [128], F32)
    if dbg:
        hTdbg = dscr("hTdbg", [128, 8, S], BF16)
        wbdbg = dscr("wbdbg", [128, 8, 1024], BF16)

    with ExitStack() as es0:
        E0 = es0.enter_context
        sems = {e: E0(nc.semaphore("s_" + e)) for e in ENGS}
        dsems = {e: [E0(nc.semaphore(f"d_{e}{i}")) for i in range(NDS)] for e in DMAQ}
        P = Prog(nc, sems, dsems)
        cf = E0(nc.sbuf_tensor("cf", [128, NCF], F32))
        cb = E0(nc.sbuf_tensor("cb", [128, 384], BF16))
        P.dma("sp", cf[:], cfa, writes=["cf"])
        P.dma("sp", cb[:], cba, writes=["cb"])

        def CF(n):
            o, w = CO[n]
            return cf[:, o:o + w]

        idb = cb[:, 0:128]
        permb = cb[:, 128:256]
        onesb = cb[:, 256:384]
        idf = CF("idf")

        def rstd_from_ss(ss, tag, scale_n):
            P.op("dve", lambda e: e.tensor_scalar(out=ss, in0=ss, scalar1=1.0 / scale_n, scalar2=EPS, op0=ALU.mult, op1=ALU.add), reads=[tag], writes=[tag])
            P.op("act", lambda e: e.activation(out=ss, in_=ss, func=AF.Ln), reads=[tag], writes=[tag])
            P.op("act", lambda e: e.activation(out=ss, in_=ss, func=AF.Exp, scale=-0.5), reads=[tag], writes=[tag])

        with ExitStack() as esA:
            EA = esA.enter_context
            hT = EA(nc.sbuf_tensor("hT", [128, 8, S], BF16))
            hTh = EA(nc.sbuf_tensor("hTh", [128, 8, 512], BF16))
            with ExitStack() as es:
                Es = es.enter_context
                xt = [Es(nc.sbuf_tensor(f"xt{i}", [128, D], F32)) for i in range(6)]
                xn = [Es(nc.sbuf_tensor(f"xn{i}", [128, D], BF16)) for i in range(4)]
                junk = Es(nc.sbuf_tensor("junk0", [128, D], BF16))
                ssq = [Es(nc.sbuf_tensor(f"ss{i}", [128, 1], F32)) for i in range(6)]
                gm = Es(nc.sbuf_tensor("gm", [128, 8], F32))
                gexp = Es(nc.sbuf_tensor("gexp", [128, 8, 128], F32))
                pT = [Es(nc.psum_tensor(f"pT{i}", [128, 8, 128], BF16)) for i in range(4)]
                P.dma("sp", gm[:], g_mix.rearrange("(c p) -> p c", p=128), writes=["gm"], allow_slow_non_contiguous=True)
                for c in range(8):
                    P.op("dve", lambda e, c=c: e.tensor_scalar(out=gexp[:, c, :], in0=CF("ones"), scalar1=gm[:, c:c + 1], scalar2=None, op0=ALU.mult), reads=["cf", "gm"], writes=["gexp"])
                def s0_info(i):
                    if i < NT:
                        return x[i * 128:(i + 1) * 128, :], hT[:, :, i * 128:(i + 1) * 128], "hT"
                    j = i - NT
                    return xhalo[j // 2, (j % 2) * 128:(j % 2 + 1) * 128, :], hTh[:, :, j * 128:(j + 1) * 128], "hTh"

                def s0_a(i):
                    a = i % 6
                    src, dst, dkey = s0_info(i)
                    P.dma("sp", xt[a][:], src, writes=[f"xt{a}"])
                    P.op("act", lambda e, a=a: e.activation(out=junk[:], in_=xt[a][:], func=AF.Square, accum_out=ssq[a][:]), reads=[f"xt{a}"], writes=["junk0", f"ss{a}"])
                    P.op("dve", lambda e, a=a: e.tensor_scalar(out=ssq[a][:], in0=ssq[a][:], scalar1=1.0 / D, scalar2=EPS, op0=ALU.mult, op1=ALU.add), reads=[f"ss{a}"], writes=[f"ss{a}"])

                def s0_b(i):
                    a, b2, c2 = i % 6, i % 4, i % 4
                    src, dst, dkey = s0_info(i)
                    P.op("act", lambda e, a=a: e.activation(out=ssq[a][:], in_=ssq[a][:], func=AF.Ln), reads=[f"ss{a}"], writes=[f"ss{a}"])
                    P.op("act", lambda e, a=a: e.activation(out=ssq[a][:], in_=ssq[a][:], func=AF.Exp, scale=-0.5), reads=[f"ss{a}"], writes=[f"ss{a}"])
                    P.op("act", lambda e, a=a, b2=b2: e.activation(out=xn[b2][:], in_=xt[a][:], func=AF.Copy, scale=ssq[a][:]), reads=[f"xt{a}", f"ss{a}"], writes=[f"xn{b2}"])
                    for c in range(8):
                        P.op("pe", lambda e, c=c, b2=b2, c2=c2: e.transpose(out=pT[c2][:, c, :], in_=xn[b2][:, c * 128:(c + 1) * 128], identity=idb), reads=[f"xn{b2}", "cb"], writes=[f"pT{c2}"])
                    P.op("dve", lambda e, c2=c2, dst=dst: e.tensor_tensor(out=dst, in0=pT[c2][:], in1=gexp[:], op=ALU.mult), reads=[f"pT{c2}", "gexp"], writes=[dkey])

                s0_a(0)
                s0_a(1)
                for i in range(NT + 4):
                    if i + 2 < NT + 4:
                        s0_a(i + 2)
                    s0_b(i)
                if dbg:
                    for c in range(8):
                        P.dma("sp", hTdbg[:, c, :], hT[:, c, :], reads=["hT"])
                P.flush()
            if stop_after <= 0:
                P.finish()
                return nc

            with ExitStack() as es:
                Es = es.enter_context
                wb = [Es(nc.sbuf_tensor(f"wb{i}", [128, 8, 1024], BF16)) for i in range(2)]
                wmix = Es(nc.sbuf_tensor("wmix", [128, 8, 256], BF16))
                psc = Es(nc.sbuf_tensor("psc", [128, 8], F32))
                fx = Es(nc.sbuf_tensor("fx", [128, 64], F32))
                u = [Es(nc.sbuf_tensor(f"u{i}", [128, 2304], F32)) for i in range(2)]
                wa = [Es(nc.sbuf_tensor(f"wa{i}", [128, 2304], F32)) for i in range(2)]
                pooled = [Es(nc.sbuf_tensor(f"pooled{i}", [128, 2, HQ], BF16)) for i in range(2)]
                pmT = Es(nc.sbuf_tensor("pmT", [128, 8, HQ], BF16))
                sg = [Es(nc.sbuf_tensor(f"sg{i}", [128, 512], F32)) for i in range(2)]
                ast = [Es(nc.sbuf_tensor(f"ast{i}", [128, 512], BF16)) for i in range(3)]
                pu = [Es(nc.psum_tensor(f"pu{i}", [128, 512], F32)) for i in range(4)]
                P.dma("sp", psc[:], pool_scale.rearrange("(c p) -> p c", p=128), writes=["psc"], allow_slow_non_contiguous=True)
                for g in range(4):
                    P.dma("pool", wmix[:, 2 * g:2 * g + 2, :], w_pool_mix[g].rearrange("(c p) d -> p c d", p=128), writes=[f"wmix{g}"])

                def load_w(i, src_ap, key):
                    v = src_ap.rearrange("(c p) n -> p c n", p=128)
                    for q in range(4):
                        P.dma("pool", wb[i][:, :, q * 256:(q + 1) * 256], v[:, :, q * 256:(q + 1) * 256], writes=[f"{key}_{q}"])

                WK0 = [f"wb0_{q}" for q in range(4)]
                WK1 = [f"wb1_{q}" for q in range(4)]
                npu = [0]

                def next_pu():
                    i = npu[0] % 4
                    npu[0] += 1
                    return i

                nst = [0]
                for hf in range(2):
                    own0 = hf * HQ
                    P.dma("sp", fx[:], fixt[hf], writes=["fx"])
                    load_w(0, w_in[:, 0:1024], "wb0")
                    if dbg and hf == 0:
                        for c in range(8):
                            P.dma("sp", wbdbg[:, c, :], wb[0][:, c, :], reads=WK0)
                    load_w(1, w_pool_out, "wb1")
                    blocks = [(0, 128, lambda dk, hf=hf: hTh[:, dk, (2 * hf) * 128:(2 * hf + 1) * 128], "hTh")]
                    for tb in range(4):
                        blocks.append((128 + tb * 512, 512, lambda dk, tb=tb, own0=own0: hT[:, dk, own0 + tb * 512:own0 + (tb + 1) * 512], "hT"))
                    blocks.append((128 + HQ, 128, lambda dk, hf=hf: hTh[:, dk, (2 * hf + 1) * 128:(2 * hf + 2) * 128], "hTh"))
                    pend_mix = []
                    for cc in range(8):
                        g = cc // 2
                        w = WINS[g]
                        ub = u[cc % 2]
                        uk = f"u{cc % 2}"
                        for (c0, n, rf, rk) in blocks:
                            pi = next_pu()
                            for dk in range(8):
                                P.op("pe", lambda e, pi=pi, dk=dk, n=n, rf=rf, cc=cc: e.matmul(pu[pi][:, 0:n], lhsT=wb[0][:, dk, cc * 128:(cc + 1) * 128], rhs=rf(dk), start=(dk == 0), stop=(dk == 7)), reads=WK0 + [rk], writes=[f"pu{pi}"])
                            P.op("act", lambda e, pi=pi, c0=c0, n=n, ub=ub: e.activation(out=ub[:, c0:c0 + n], in_=pu[pi][:, 0:n], func=AF.Copy), reads=[f"pu{pi}"], writes=[uk])
                        if pend_mix:
                            pend_mix.pop(0)()
                        cur, ck = ub, uk
                        lvl = [(1, 2304, 1, 0), (2, 2303, 1, -1), (4, 2301, 2, -2), (8, 2297, 4, -4)]
                        eng_alt = ["dve", "dve"]
                        for li in range(g + 1):
                            lo, hi, sp_, sm = lvl[li]
                            dstt = wa[li % 2]
                            dk_ = f"wa{li % 2}"
                            if li == 0:
                                P.op("dve", lambda e, dstt=dstt, cur=cur, lo=lo, hi=hi: e.tensor_tensor(out=dstt[:, lo:hi], in0=cur[:, lo - 1:hi - 1], in1=cur[:, lo:hi], op=ALU.add), reads=[ck], writes=[dk_])
                            else:
                                P.op(eng_alt[li % 2], lambda e, dstt=dstt, cur=cur, lo=lo, hi=hi, sp_=sp_: e.tensor_tensor(out=dstt[:, lo:hi], in0=cur[:, lo - sp_:hi - sp_], in1=cur[:, lo + sp_:hi + sp_], op=ALU.add), reads=[ck], writes=[dk_])
                            cur, ck = dstt, dk_
                        P.op("dve", lambda e, cur=cur, g=g: e.tensor_tensor(out=cur[:, 128:136], in0=cur[:, 128:136], in1=fx[:, g * 8:(g + 1) * 8], op=ALU.mult), reads=[ck, "fx"], writes=[ck])
                        P.op("dve", lambda e, cur=cur, g=g: e.tensor_tensor(out=cur[:, 128 + HQ - 8:128 + HQ], in0=cur[:, 128 + HQ - 8:128 + HQ], in1=fx[:, 32 + g * 8:32 + (g + 1) * 8], op=ALU.mult), reads=[ck, "fx"], writes=[ck])
                        pk = f"pooled{g % 2}"
                        P.op("dve", lambda e, cur=cur, ub=ub, g=g, cc=cc, w=w: e.scalar_tensor_tensor(out=pooled[g % 2][:, cc % 2, :], in0=cur[:, 128:128 + HQ], scalar=1.0 / w, in1=ub[:, 128:128 + HQ], op0=ALU.mult, op1=ALU.subtract), reads=[ck, uk], writes=[pk])
                        if cc % 2 == 1:
                            def _mix(g=g, pk=pk):
                                for dc in range(2):
                                    for tb in range(4):
                                        pi = next_pu()
                                        for c in range(2):
                                            P.op("pe", lambda e, pi=pi, c=c, g=g, dc=dc, tb=tb: e.matmul(pu[pi][:], lhsT=wmix[:, 2 * g + c, dc * 128:(dc + 1) * 128], rhs=pooled[g % 2][:, c, tb * 512:(tb + 1) * 512], start=(c == 0), stop=(c == 1)), reads=[f"wmix{g}", pk], writes=[f"pu{pi}"])
                                        P.op("act", lambda e, pi=pi, g=g, dc=dc, tb=tb: e.activation(out=pmT[:, 2 * g + dc, tb * 512:(tb + 1) * 512], in_=pu[pi][:], func=AF.Copy, scale=psc[:, 2 * g + dc:2 * g + dc + 1]), reads=[f"pu{pi}", "psc"], writes=["pmT"])
                            pend_mix.append(_mix)
                    while pend_mix:
                        pend_mix.pop(0)()
                    load_w(0, w_in[:, C_GP:C_GP + 1024], "wb0")
                    for dc in range(8):
                        for tb in range(4):
                            p1 = next_pu()
                            for c in range(8):
                                P.op("pe", lambda e, p1=p1, c=c, dc=dc, tb=tb: e.matmul(pu[p1][:], lhsT=wb[1][:, c, dc * 128:(dc + 1) * 128], rhs=pmT[:, c, tb * 512:(tb + 1) * 512], start=(c == 0), stop=(c == 7)), reads=WK1 + ["pmT"], writes=[f"pu{p1}"])
                            p2 = next_pu()
                            for dk in range(8):
                                P.op("pe", lambda e, p2=p2, dk=dk, dc=dc, tb=tb, own0=own0: e.matmul(pu[p2][:], lhsT=wb[0][:, dk, dc * 128:(dc + 1) * 128], rhs=hT[:, dk, own0 + tb * 512:own0 + (tb + 1) * 512], start=(dk == 0), stop=(dk == 7)), reads=WK0 + ["hT"], writes=[f"pu{p2}"])
                            si = nst[0] % 2
                            ai = nst[0] % 3
                            nst[0] += 1
                            P.op("act", lambda e, p2=p2, si=si: e.activation(out=sg[si][:], in_=pu[p2][:], func=AF.Sigmoid), reads=[f"pu{p2}"], writes=[f"sg{si}"])
                            P.op("dve", lambda e, p1=p1, si=si, ai=ai: e.tensor_tensor(out=ast[ai][:], in0=pu[p1][:], in1=sg[si][:], op=ALU.mult), reads=[f"pu{p1}", f"sg{si}"], writes=[f"ast{ai}"])
                            P.dma("sp", Abuf[hf, dc * 128:(dc + 1) * 128, tb * 512:(tb + 1) * 512], ast[ai][:], reads=[f"ast{ai}"], writes=["Abuf"])
                    load_w(1, w_in[:, C_GA:C_GA + 1024], "wb1")
                    for dc in range(8):
                        for tb in range(4):
                            p2 = next_pu()
                            for dk in range(8):
                                P.op("pe", lambda e, p2=p2, dk=dk, dc=dc, tb=tb, own0=own0: e.matmul(pu[p2][:], lhsT=wb[1][:, dk, dc * 128:(dc + 1) * 128], rhs=hT[:, dk, own0 + tb * 512:own0 + (tb + 1) * 512], start=(dk == 0), stop=(dk == 7)), reads=WK1 + ["hT"], writes=[f"pu{p2}"])
                            ai = nst[0] % 3
                            nst[0] += 1
                            P.op("act", lambda e, p2=p2, ai=ai: e.activation(out=ast[ai][:], in_=pu[p2][:], func=AF.Sigmoid), reads=[f"pu{p2}"], writes=[f"ast{ai}"])
                            P.dma("sp", Gabuf[hf, dc * 128:(dc + 1) * 128, tb * 512:(tb + 1) * 512], ast[ai][:], reads=[f"ast{ai}"], writes=["Gabuf"])
                P.flush()
            if stop_after <= 1:
                P.finish()
                return nc

            with ExitStack() as es:
                Es = es.enter_context
                Ct = Es(nc.sbuf_tensor("Ct", [128, S], F32))
                St = Es(nc.sbuf_tensor("St", [128, S], F32))
                tmpi = Es(nc.sbuf_tensor("tmpi", [128, 1024], I32))
                tmpa = Es(nc.sbuf_tensor("tmpa", [128, 1024], F32))
                tmpb = Es(nc.sbuf_tensor("tmpb", [128, 1024], F32))
                kT = [Es(nc.sbuf_tensor(f"kT{i}", [128, S], BF16)) for i in range(2)]
                Vt = [Es(nc.sbuf_tensor(f"Vt{i}", [128, NT, 130], BF16)) for i in range(2)]
                qT = [Es(nc.sbuf_tensor(f"qT{i}", [128, HQ], BF16)) for i in range(2)]
                wq = [Es(nc.sbuf_tensor(f"wq{i}", [128, 8, 128], BF16)) for i in range(2)]
                wk = [Es(nc.sbuf_tensor(f"wk{i}", [128, 8, 128], BF16)) for i in range(2)]
                wv = [Es(nc.sbuf_tensor(f"wv{i}", [128, 8, 128], BF16)) for i in range(2)]
                zb = [Es(nc.sbuf_tensor(f"zb{i}", [128, 512], BF16)) for i in range(2)]
                t1 = [Es(nc.sbuf_tensor(f"t1{i}", [128, 512], F32)) for i in range(2)]
                t2 = [Es(nc.sbuf_tensor(f"t2{i}", [128, 512], F32)) for i in range(2)]
                PT = [Es(nc.sbuf_tensor(f"PT{i}", [128, 2, 512], BF16)) for i in range(4)]
                lamt = Es(nc.sbuf_tensor("lamt", [128, 256], F32))
                lamp = Es(nc.sbuf_tensor("lamp", [128, 128], F32))
                lsc = Es(nc.sbuf_tensor("lsc", [128, 4], F32))
                gsub = Es(nc.sbuf_tensor("gsub", [128, 128], F32))
                fin = Es(nc.sbuf_tensor("fin", [128, 4, 4], F32))
                o1 = [Es(nc.sbuf_tensor(f"o1{i}", [128, 128], F32)) for i in range(4)]
                o2 = [Es(nc.sbuf_tensor(f"o2{i}", [128, 128], F32)) for i in range(4)]
                ob = [Es(nc.sbuf_tensor(f"ob{i}", [128, 128], BF16)) for i in range(4)]
                ojunk = Es(nc.sbuf_tensor("ojunk", [128, 128], F32))
                ost = [Es(nc.sbuf_tensor(f"ost{i}", [128, 512], BF16)) for i in range(2)]
                pS2 = [Es(nc.psum_tensor(f"pS2_{i}", [128, 2, 512], F32)) for i in range(2)]

                class _V:
                    def __init__(self, t, m):
                        self.t, self.m = t, m

                    def __getitem__(self, idx):
                        if idx == slice(None):
                            return self.t[:, self.m, :]
                        return self.t[:, self.m, :][idx]

                BK = [_V(pS2[0], 0), _V(pS2[0], 1), _V(pS2[1], 0), _V(pS2[1], 1)] + [Es(nc.psum_tensor(f"BK{i}", [128, 512], F32)) for i in range(4, 8)]
                B7b = BK[7][:].bitcast(BF16)
                rinv = CF("rinv")
                for q4 in range(4):
                    cs = slice(q4 * 1024, (q4 + 1) * 1024)
                    P.dma("sp", tmpi[:], pos[q4 * 1024:(q4 + 1) * 1024].partition_broadcast(128), writes=["tmpi"])
                    P.op("dve", lambda e: e.tensor_copy(out=tmpa[:], in_=tmpi[:]), reads=["tmpi"], writes=["tmpa"])
                    for which in range(2):
                        dstT = St if which == 0 else Ct
                        dkey = f"St{q4}" if which == 0 else f"Ct{q4}"
                        P.op("dve", lambda e, which=which: e.tensor_scalar(out=tmpb[:], in0=tmpa[:], scalar1=rinv[:, 0:1], scalar2=(0.0 if which == 0 else math.pi / 2), op0=ALU.mult, op1=ALU.add), reads=["tmpa", "cf"], writes=["tmpb"])
                        P.op("dve", lambda e: e.tensor_scalar(out=tmpi[:], in0=tmpb[:], scalar1=1.0 / (2 * math.pi), scalar2=None, op0=ALU.mult), reads=["tmpb"], writes=["tmpi"])
                        P.op("dve", lambda e, dstT=dstT, cs=cs: e.tensor_copy(out=dstT[:, cs], in_=tmpi[:]), reads=["tmpi"], writes=[dkey])
                        P.op("dve", lambda e, dstT=dstT, cs=cs: e.scalar_tensor_tensor(out=tmpb[:], in0=dstT[:, cs], scalar=-2 * math.pi, in1=tmpb[:], op0=ALU.mult, op1=ALU.add), reads=[dkey, "tmpb"], writes=["tmpb"])
                        P.op("dve", lambda e, dstT=dstT, cs=cs: e.tensor_scalar(out=dstT[:, cs], in0=tmpb[:], scalar1=math.pi, scalar2=-2 * math.pi, op0=ALU.is_gt, op1=ALU.mult), reads=["tmpb"], writes=[dkey])
                        P.op("dve", lambda e, dstT=dstT, cs=cs: e.tensor_tensor(out=tmpb[:], in0=tmpb[:], in1=dstT[:, cs], op=ALU.add), reads=["tmpb", dkey], writes=["tmpb"])
                        P.op("act", lambda e, dstT=dstT, cs=cs: e.activation(out=dstT[:, cs], in_=tmpb[:], func=AF.Sin), reads=["tmpb"], writes=[dkey])
                        if which == 0:
                            P.op("dve", lambda e, cs=cs: e.tensor_scalar(out=St[:, cs], in0=St[:, cs], scalar1=rinv[:, 1:2], scalar2=None, op0=ALU.mult), reads=[f"St{q4}", "cf"], writes=[f"St{q4}"])
                P.dma("sp", lamt[:], lamv.rearrange("a b -> (a b)").partition_broadcast(128), writes=["lamt"])
                P.dma("sp", gsub[:], g_subln.partition_broadcast(128), writes=["gsub"])
                P.op("dve", lambda e: e.tensor_tensor(out=lamp[:, 0:64], in0=lamt[:, 0:64], in1=lamt[:, 64:128], op=ALU.mult), reads=["lamt"], writes=["lamp"])
                P.op("dve", lambda e: e.tensor_tensor(out=lamp[:, 64:128], in0=lamt[:, 128:192], in1=lamt[:, 192:256], op=ALU.mult), reads=["lamt"], writes=["lamp"])
                P.op("dve", lambda e: e.reduce_sum(out=lsc[:, 0:1], in_=lamp[:, 0:64], axis=AX.X), reads=["lamp"], writes=["lsc"])
                P.op("dve", lambda e: e.reduce_sum(out=lsc[:, 1:2], in_=lamp[:, 64:128], axis=AX.X), reads=["lamp"], writes=["lsc"])
                P.op("act", lambda e: e.activation(out=lsc[:, 0:2], in_=lsc[:, 0:2], func=AF.Exp), reads=["lsc"], writes=["lsc"])
                P.op("dve", lambda e: e.tensor_tensor(out=lsc[:, 2:3], in0=lsc[:, 1:2], in1=lsc[:, 0:1], op=ALU.subtract), reads=["lsc"], writes=["lsc"])
                P.op("dve", lambda e: e.tensor_scalar(out=lsc[:, 2:3], in0=lsc[:, 2:3], scalar1=-LAM_INIT, scalar2=None, op0=ALU.add), reads=["lsc"], writes=["lsc"])
                P.op("dve", lambda e: e.tensor_scalar(out=gsub[:], in0=gsub[:], scalar1=(1.0 - LAM_INIT), scalar2=None, op0=ALU.mult), reads=["gsub"], writes=["gsub"])
                for i in range(2):
                    P.op("dve", lambda e, i=i: e.memset(Vt[i][:, :, 128:130], 1.0), writes=[f"Vt{i}"])

                deferred = []
                ZB = [0, 4]
                ZPB = [1, 5]

                def proj_rot(wt, wkeys, dstt, dkey, hcol0, nblk, tcol0):
                    def mm(tb):
                        zb_ = ZB[tb % 2]
                        for dk in range(8):
                            P.op("pe", lambda e, dk=dk, tb=tb, zb_=zb_: e.matmul(BK[zb_][:], lhsT=wt[:, dk, :], rhs=hT[:, dk, hcol0 + tb * 512:hcol0 + (tb + 1) * 512], start=(dk == 0), stop=(dk == 7)), reads=wkeys + ["hT"], writes=[f"BK{zb_}"])

                    mm(0)
                    for tb in range(nblk):
                        zi = tb % 2
                        zb_ = ZB[tb % 2]
                        zp_ = ZPB[tb % 2]
                        tq = (tcol0 + tb * 512) // 1024
                        if tb + 1 < nblk:
                            mm(tb + 1)
                        P.op("act", lambda e, zi=zi, zb_=zb_: e.activation(out=zb[zi][:], in_=BK[zb_][:], func=AF.Copy), reads=[f"BK{zb_}"], writes=[f"zb{zi}"])
                        P.op("dve", lambda e, zi=zi, tb=tb: e.tensor_tensor(out=t1[zi][:], in0=zb[zi][:], in1=Ct[:, tcol0 + tb * 512:tcol0 + (tb + 1) * 512], op=ALU.mult), reads=[f"zb{zi}", f"Ct{tq}"], writes=[f"t1{zi}"])
                        P.op("pe", lambda e, zi=zi, zp_=zp_: e.matmul(BK[zp_][:], lhsT=permb, rhs=zb[zi][:], start=True, stop=True), reads=[f"zb{zi}", "cb"], writes=[f"BK{zp_}"])
                        P.op("dve", lambda e, zi=zi, tb=tb, zp_=zp_: e.tensor_tensor(out=t2[zi][:], in0=BK[zp_][:], in1=St[:, tcol0 + tb * 512:tcol0 + (tb + 1) * 512], op=ALU.mult), reads=[f"BK{zp_}", f"St{tq}"], writes=[f"t2{zi}"])
                        P.op("dve", lambda e, zi=zi, tb=tb: e.tensor_tensor(out=dstt[:, tb * 512:(tb + 1) * 512], in0=t1[zi][:], in1=t2[zi][:], op=ALU.add), reads=[f"t1{zi}", f"t2{zi}"], writes=[dkey])
                        if deferred:
                            deferred.pop(0)[1]()

                import os
                nq = [0]
                no = [0]
                for h in range(int(os.environ.get("KDBG_NH", NH))):
                    hb = h % 2
                    for (wt, nm_, c0) in ((wq, "wq", C_Q), (wk, "wk", C_K), (wv, "wv", C_V)):
                        P.dma("pool", wt[hb][:], w_in[:, c0 + h * 128:c0 + (h + 1) * 128].rearrange("(c p) n -> p c n", p=128), writes=[f"{nm_}{hb}"])
                    for tg in range(NT // 4):
                        bi = 2 + tg % 2
                        for j in range(4):
                            ti = tg * 4 + j
                            for dk in range(8):
                                P.op("pe", lambda e, dk=dk, ti=ti, j=j, bi=bi, hb=hb: e.matmul(BK[bi][:, j * 128:(j + 1) * 128], lhsT=hT[:, dk, ti * 128:(ti + 1) * 128], rhs=wv[hb][:, dk, :], start=(dk == 0), stop=(dk == 7)), reads=[f"wv{hb}", "hT"], writes=[f"BK{bi}"])
                        P.op("dve", lambda e, tg=tg, bi=bi, hb=hb: e.tensor_copy(out=Vt[hb][:, tg * 4:(tg + 1) * 4, 0:128], in_=BK[bi][:].rearrange("p (j n) -> p j n", j=4)), reads=[f"BK{bi}"], writes=[f"Vt{hb}"])
                    proj_rot(wk[hb], [f"wk{hb}"], kT[hb], f"kT{hb}", 0, 8, 0)
                    for hf in range(2):
                        own0 = hf * HQ
                        qi = nq[0] % 2
                        nq[0] += 1
                        proj_rot(wq[hb], [f"wq{hb}"], qT[qi], f"qT{qi}", own0, 4, own0)
                        for qb in range(4):
                            def acc(m, qs):
                                a = m * 4 + qs
                                return BK[4 + a // 3][:, (a % 3) * 160:(a % 3) * 160 + 130]

                            PO = ["BK4", "BK5", "BK6"]

                            def qk(kt, hb=hb, qi=qi, qb=qb):
                                si = kt % 2
                                for m in range(2):
                                    bi = si * 2 + m
                                    P.op("pe", lambda e, m=m, kt=kt, bi=bi: e.matmul(BK[bi][:], lhsT=kT[hb][m * 64:(m + 1) * 64, kt * 128:(kt + 1) * 128], rhs=qT[qi][m * 64:(m + 1) * 64, qb * 512:(qb + 1) * 512], start=True, stop=True), reads=[f"kT{hb}", f"qT{qi}"], writes=[f"BK{bi}"])

                            qk(0)
                            for kt in range(NT):
                                si = kt % 2
                                pi = kt % 4
                                if kt + 1 < NT:
                                    qk(kt + 1)
                                for m in range(2):
                                    bi = si * 2 + m
                                    P.op("act", lambda e, bi=bi, pi=pi, m=m: e.activation(out=PT[pi][:, m, :], in_=BK[bi][:], func=AF.Exp, scale=SCALE), reads=[f"BK{bi}"], writes=[f"PT{pi}_{m}"])
                                while deferred and deferred[0][0] <= kt:
                                    deferred.pop(0)[1]()
                                for m in range(2):
                                    for qs in range(4):
                                        P.op("pe", lambda e, m=m, qs=qs, kt=kt, pi=pi, hb=hb: e.matmul(acc(m, qs), lhsT=PT[pi][:, m, qs * 128:(qs + 1) * 128], rhs=Vt[hb][:, kt, 0:130], start=(kt == 0 and (m * 4 + qs) % 3 == 0), stop=(kt == NT - 1), skip_group_check=True), reads=[f"PT{pi}_{m}", f"Vt{hb}"], writes=PO)
                            oi = no[0] % 2
                            no[0] += 1
                            for qs in range(4):
                                fk = f"fin{qs}"
                                P.op("dve", lambda e, qs=qs: e.reciprocal(out=fin[:, qs, 0:1], in_=acc(0, qs)[:, 128:129]), reads=PO, writes=[fk])
                                P.op("dve", lambda e, qs=qs: e.reciprocal(out=fin[:, qs, 1:2], in_=acc(1, qs)[:, 128:129]), reads=PO, writes=[fk])
                                P.op("dve", lambda e, qs=qs: e.tensor_tensor(out=fin[:, qs, 1:2], in0=fin[:, qs, 1:2], in1=lsc[:, 2:3], op=ALU.mult), reads=[fk, "lsc"], writes=[fk])
                                P.op("dve", lambda e, qs=qs: e.tensor_scalar(out=o1[qs][:], in0=acc(0, qs)[:, 0:128], scalar1=fin[:, qs, 0:1], scalar2=None, op0=ALU.mult), reads=PO + [fk], writes=[f"o1{qs}"])
                                P.op("dve", lambda e, qs=qs: e.scalar_tensor_tensor(out=o2[qs][:], in0=acc(1, qs)[:, 0:128], scalar=fin[:, qs, 1:2], in1=o1[qs][:], op0=ALU.mult, op1=ALU.add), reads=PO + [fk, f"o1{qs}"], writes=[f"o2{qs}"])
                            FB = [f"finb{q}" for q in range(4)]

                            def fin_b():
                                for qs in range(4):
                                    P.op("act", lambda e, qs=qs: e.activation(out=ojunk[:], in_=o2[qs][:], func=AF.Square, accum_out=fin[:, qs, 2:3]), reads=[f"o2{qs}"], writes=["ojunk", f"finb{qs}"])

                            def fin_c():
                                P.op("dve", lambda e: e.tensor_scalar(out=fin[:, :, 2], in0=fin[:, :, 2], scalar1=1.0 / 128, scalar2=EPS, op0=ALU.mult, op1=ALU.add), reads=FB, writes=FB)

                            def fin_d():
                                P.op("act", lambda e: e.activation(out=fin[:, :, 2], in_=fin[:, :, 2], func=AF.Ln), reads=FB, writes=FB)
                                P.op("act", lambda e: e.activation(out=fin[:, :, 2], in_=fin[:, :, 2], func=AF.Exp, scale=-0.5), reads=FB, writes=FB)

                            def fin_e():
                                for qs in range(4):
                                    P.op("dve", lambda e, qs=qs: e.scalar_tensor_tensor(out=ob[qs][:], in0=o2[qs][:], scalar=fin[:, qs, 2:3], in1=gsub[:], op0=ALU.mult, op1=ALU.mult), reads=[f"o2{qs}", f"finb{qs}", "gsub"], writes=[f"ob{qs}"])

                            def fin_f(oi=oi, hf=hf, h=h, qb=qb):
                                for qs in range(4):
                                    P.op("pe", lambda e, qs=qs: e.transpose(out=B7b[:, qs * 128:(qs + 1) * 128], in_=ob[qs][:], identity=idb), reads=[f"ob{qs}", "cb"], writes=["BK7"])
                                P.op("dve", lambda e, oi=oi: e.tensor_copy(out=ost[oi][:], in_=B7b[:, 0:512]), reads=["BK7"], writes=[f"ost{oi}"])
                                P.dma("sp", oTbuf[hf, h * 128:(h + 1) * 128, qb * 512:(qb + 1) * 512], ost[oi][:], reads=[f"ost{oi}"], writes=["oTbuf"])

                            deferred[:] = [(2, fin_b), (3, fin_c), (4, fin_d), (6, fin_e), (8, fin_f)]
                while deferred:
                    deferred.pop(0)[1]()
                P.flush()
        if stop_after <= 2:
            P.finish()
            return nc

        with ExitStack() as es:
            Es = es.enter_context
            wao = Es(nc.sbuf_tensor("wao", [128, 8, 1024], BF16))
            wo = Es(nc.sbuf_tensor("wo", [128, 8, 1024], BF16))
            oTb = [Es(nc.sbuf_tensor(f"oTb{i}", [128, 8, 512], BF16)) for i in range(2)]
            Gab = [Es(nc.sbuf_tensor(f"Gab{i}", [128, 8, 512], BF16)) for i in range(2)]
            Ab = [Es(nc.sbuf_tensor(f"Ab{i}", [128, 8, 512], BF16)) for i in range(2)]
            mT = [Es(nc.sbuf_tensor(f"mT{i}", [128, 8, 512], BF16)) for i in range(2)]
            tmpm = [Es(nc.sbuf_tensor(f"tmpm{i}", [128, 512], F32)) for i in range(2)]
            xt2 = [Es(nc.sbuf_tensor(f"xt2{i}", [128, D], F32)) for i in range(2)]
            x1 = [Es(nc.sbuf_tensor(f"x1{i}", [128, D], F32)) for i in range(2)]
            h2 = [Es(nc.sbuf_tensor(f"h2{i}", [128, D], BF16)) for i in range(2)]
            x1T = [Es(nc.sbuf_tensor(f"x1T{i}", [128, 8, 128], F32)) for i in range(2)]
            junk2 = Es(nc.sbuf_tensor("junk2", [128, D], BF16))
            wr = Es(nc.sbuf_tensor("wr", [128, 8, NE], F32))
            gf = Es(nc.sbuf_tensor("gf", [128, 8], F32))
            sm = [Es(nc.sbuf_tensor(f"sm{i}", [128, 8], F32)) for i in range(2)]
            lg = [Es(nc.sbuf_tensor(f"lg{i}", [128, NE], F32)) for i in range(2)]
            ex = [Es(nc.sbuf_tensor(f"ex{i}", [128, NE], F32)) for i in range(2)]
            aff = [Es(nc.sbuf_tensor(f"aff{i}", [128, NE], F32)) for i in range(2)]
            affTs = [Es(nc.sbuf_tensor(f"affTs{i}", [NE, 128], F32)) for i in range(2)]
            BK = [Es(nc.psum_tensor(f"CK{i}", [128, 512], F32)) for i in range(8)]

            def loadw(dst, key, src_ap):
                v = src_ap.rearrange("(c p) n -> p c n", p=128)
                for q in range(4):
                    P.dma("pool", dst[:, :, q * 256:(q + 1) * 256], v[:, :, q * 256:(q + 1) * 256], writes=[f"{key}_{q}"])
                return [f"{key}_{q}" for q in range(4)]

            WAO = loadw(wao, "wao", w_attn_out)
            WO = loadw(wo, "wo", w_out)
            P.dma("sp", wr[:], w_router.rearrange("(c p) e -> p c e", p=128), writes=["wr"])
            P.dma("sp", gf[:], g_ffn.rearrange("(c p) -> p c", p=128), writes=["gf"], allow_slow_non_contiguous=True)
            for c in range(8):
                P.op("dve", lambda e, c=c: e.tensor_scalar(out=wr[:, c, :], in0=wr[:, c, :], scalar1=gf[:, c:c + 1], scalar2=None, op0=ALU.mult), reads=["wr", "gf"], writes=["wr"])
            def Y(blk):
                hf, tb = blk // 4, blk % 4
                bi = blk % 2
                oTv = oTbuf[hf].rearrange("(c p) t -> p c t", p=128)
                Gav = Gabuf[hf].rearrange("(c p) t -> p c t", p=128)
                Av = Abuf[hf].rearrange("(c p) t -> p c t", p=128)
                cs = slice(tb * 512, (tb + 1) * 512)
                P.dma("sp", oTb[bi][:], oTv[:, :, cs], reads=["oTbuf"], writes=[f"oTb{bi}"])
                P.dma("sp", Gab[bi][:], Gav[:, :, cs], reads=["Gabuf"], writes=[f"Gab{bi}"])
                P.dma("sp", Ab[bi][:], Av[:, :, cs], reads=["Abuf"], writes=[f"Ab{bi}"])
                for dc in range(8):
                    pb = dc % 2
                    for c in range(8):
                        P.op("pe", lambda e, c=c, dc=dc, pb=pb, bi=bi: e.matmul(BK[pb][:], lhsT=wao[:, c, dc * 128:(dc + 1) * 128], rhs=oTb[bi][:, c, :], start=(c == 0), stop=(c == 7)), reads=WAO + [f"oTb{bi}"], writes=[f"CK{pb}"])
                    P.op("dve", lambda e, dc=dc, pb=pb, bi=bi: e.tensor_tensor(out=tmpm[pb][:], in0=BK[pb][:], in1=Gab[bi][:, dc, :], op=ALU.mult), reads=[f"CK{pb}", f"Gab{bi}"], writes=[f"tmpm{pb}"])
                    P.op("dve", lambda e, dc=dc, pb=pb, bi=bi: e.tensor_tensor(out=mT[bi][:, dc, :], in0=tmpm[pb][:], in1=Ab[bi][:, dc, :], op=ALU.add), reads=[f"tmpm{pb}", f"Ab{bi}"], writes=[f"mT{bi}"])

            def Dt(t):
                blk, j = t // 4, t % 4
                bi, xi = blk % 2, t % 2
                rows = slice(t * 128, (t + 1) * 128)
                sk = f"sm{xi}"
                P.dma("sp", xt2[xi][:], x[rows, :], writes=[f"xt2{xi}"])
                for half in range(2):
                    pb = 2 + half
                    for dk in range(8):
                        P.op("pe", lambda e, dk=dk, half=half, pb=pb, bi=bi, j=j: e.matmul(BK[pb][:], lhsT=mT[bi][:, dk, j * 128:(j + 1) * 128], rhs=wo[:, dk, half * 512:(half + 1) * 512], start=(dk == 0), stop=(dk == 7)), reads=WO + [f"mT{bi}"], writes=[f"CK{pb}"])
                    P.op("dve", lambda e, half=half, pb=pb, xi=xi: e.tensor_tensor(out=x1[xi][:, half * 512:(half + 1) * 512], in0=BK[pb][:], in1=xt2[xi][:, half * 512:(half + 1) * 512], op=ALU.add), reads=[f"CK{pb}", f"xt2{xi}"], writes=[f"x1{xi}"])
                P.dma("pool", x1buf[rows, :], x1[xi][:], reads=[f"x1{xi}"], writes=["x1buf"])
                P.op("act", lambda e, xi=xi: e.activation(out=junk2[:], in_=x1[xi][:], func=AF.Square, accum_out=sm[xi][:, 0:1]), reads=[f"x1{xi}"], writes=["junk2", sk])
                P.op("dve", lambda e, xi=xi: e.tensor_scalar(out=sm[xi][:, 0:1], in0=sm[xi][:, 0:1], scalar1=1.0 / D, scalar2=EPS, op0=ALU.mult, op1=ALU.add), reads=[sk], writes=[sk])
                P.op("act", lambda e, xi=xi: e.activation(out=sm[xi][:, 0:1], in_=sm[xi][:, 0:1], func=AF.Ln), reads=[sk], writes=[sk])
                P.op("act", lambda e, xi=xi: e.activation(out=sm[xi][:, 0:1], in_=sm[xi][:, 0:1], func=AF.Exp, scale=-0.5), reads=[sk], writes=[sk])
                P.op("act", lambda e, xi=xi: e.activation(out=h2[xi][:], in_=x1[xi][:], func=AF.Copy, scale=sm[xi][:, 0:1]), reads=[f"x1{xi}", sk], writes=[f"h2{xi}"])
                P.dma("pool", h2buf[rows, :], h2[xi][:], reads=[f"h2{xi}"], writes=["h2buf"])

            def Tt(t):
                xi = t % 2
                for dk in range(8):
                    pb = 4 + dk // 4
                    P.op("pe", lambda e, dk=dk, pb=pb, xi=xi: e.transpose(out=BK[pb][:, (dk % 4) * 128:(dk % 4 + 1) * 128], in_=x1[xi][:, dk * 128:(dk + 1) * 128], identity=idf), reads=[f"x1{xi}", "cf"], writes=[f"CK{pb}"])
                for pb in (4, 5):
                    P.op("act", lambda e, pb=pb, xi=xi: e.activation(out=x1T[xi][:, (pb - 4) * 4:(pb - 3) * 4, :], in_=BK[pb][:].rearrange("p (j n) -> p j n", j=4), func=AF.Copy), reads=[f"CK{pb}"], writes=[f"x1T{xi}"])

            def Lt(t):
                xi = t % 2
                rows = slice(t * 128, (t + 1) * 128)
                sk = f"sm{xi}"
                for dk in range(8):
                    P.op("pe", lambda e, dk=dk, xi=xi: e.matmul(BK[6][:, 0:NE], lhsT=x1T[xi][:, dk, :], rhs=wr[:, dk, :], start=(dk == 0), stop=(dk == 7)), reads=[f"x1T{xi}", "wr"], writes=["CK6"])
                P.op("dve", lambda e, xi=xi: e.tensor_scalar(out=lg[xi][:], in0=BK[6][:, 0:NE], scalar1=sm[xi][:, 0:1], scalar2=None, op0=ALU.mult), reads=["CK6", sk], writes=[f"lg{xi}"])
                P.op("dve", lambda e, xi=xi: e.reduce_max(out=sm[xi][:, 1:2], in_=lg[xi][:], axis=AX.X), reads=[f"lg{xi}"], writes=[sk + "b"])
                P.op("dve", lambda e, xi=xi: e.tensor_scalar(out=sm[xi][:, 1:2], in0=sm[xi][:, 1:2], scalar1=-1.0, scalar2=None, op0=ALU.mult), reads=[sk + "b"], writes=[sk + "b"])
                P.op("act", lambda e, xi=xi: e.activation(out=ex[xi][:], in_=lg[xi][:], func=AF.Exp, bias=sm[xi][:, 1:2], scale=1.0, accum_out=sm[xi][:, 2:3]), reads=[f"lg{xi}", sk + "b"], writes=[f"ex{xi}", sk + "c"])
                P.op("dve", lambda e, xi=xi: e.reciprocal(out=sm[xi][:, 2:3], in_=sm[xi][:, 2:3]), reads=[sk + "c"], writes=[sk + "c"])
                P.op("dve", lambda e, xi=xi: e.tensor_scalar(out=aff[xi][:], in0=ex[xi][:], scalar1=sm[xi][:, 2:3], scalar2=None, op0=ALU.mult), reads=[f"ex{xi}", sk + "c"], writes=[f"aff{xi}"])
                P.dma("pool", affbuf[rows, :], aff[xi][:], reads=[f"aff{xi}"], writes=["affbuf"])

            def At(t):
                xi = t % 2
                rows = slice(t * 128, (t + 1) * 128)
                P.op("pe", lambda e, xi=xi: e.transpose(out=BK[7][0:NE, 0:128], in_=aff[xi][:], identity=idf), reads=[f"aff{xi}", "cf"], writes=["CK7"])
                P.op("act", lambda e, xi=xi: e.activation(out=affTs[xi][:], in_=BK[7][0:NE, 0:128], func=AF.Copy), reads=["CK7"], writes=[f"affTs{xi}"])
                P.dma("pool", affTbuf[:, rows], affTs[xi][:], reads=[f"affTs{xi}"], writes=["affTbuf"])

            Y(0)
            Dt(0)
            for t in range(NT):
                if t % 4 == 1 and t // 4 + 1 < 8:
                    Y(t // 4 + 1)
                if t + 1 < NT:
                    Dt(t + 1)
                Tt(t)
                Lt(t)
                if t >= 1:
                    At(t - 1)
            At(NT - 1)
            P.flush()
        if stop_after <= 3:
            P.finish()
            return nc

        with ExitStack() as es:
            Es = es.enter_context
            affP = Es(nc.sbuf_tensor("affP", [128, 512], F32))
            afft = Es(nc.sbuf_tensor("afft", [128, NT, NE], F32))
            bjunk = Es(nc.sbuf_tensor("bjunk", [128, 512], BF16))
            bs = Es(nc.sbuf_tensor("bs", [128, 8], F32))
            taub = Es(nc.sbuf_tensor("taub", [128, NE], F32))
            mask = Es(nc.sbuf_tensor("mask", [128, NT, NE], F32))
            pa = Es(nc.sbuf_tensor("pa", [128, NT, NE], F32))
            pb_ = Es(nc.sbuf_tensor("pb_", [128, NT, NE], F32))
            slot = Es(nc.sbuf_tensor("slot", [128, NT, NE], F32))
            ra = Es(nc.sbuf_tensor("ra", [128, NT, NE], F32))
            rbf = Es(nc.sbuf_tensor("rbf", [128, NT, NE], BF16))
            R = Es(nc.sbuf_tensor("R", [128, NT, NE, 6], BF16))
            gf3 = Es(nc.sbuf_tensor("gf3", [128, 8], F32))
            oh = [Es(nc.sbuf_tensor(f"oh{i}", [128, 512], BF16)) for i in range(4)]
            idxf = [Es(nc.sbuf_tensor(f"idxf{i}", [128, 4], F32)) for i in range(2)]
            idxi = [Es(nc.sbuf_tensor(f"idxi{i}", [128, 4], I32)) for i in range(2)]
            valt = [Es(nc.sbuf_tensor(f"valt{i}", [128, 4], F32)) for i in range(2)]
            pIs = Es(nc.sbuf_tensor("pIs", [128, 32], F32))
            xs = [Es(nc.sbuf_tensor(f"xs{i}", [128, 4, D], BF16)) for i in range(2)]
            xsT = [Es(nc.sbuf_tensor(f"xsT{i}", [128, 8, 512], BF16)) for i in range(2)]
            hTm = [Es(nc.sbuf_tensor(f"hTm{i}", [128, NFC, 512], BF16)) for i in range(2)]
            yt = Es(nc.sbuf_tensor("yt", [128, 4, D], F32))
            sgt = [Es(nc.sbuf_tensor(f"sgt{i}", [128, 512], F32)) for i in range(2)]
            NGU = 3
            ND = 4
            wg = [Es(nc.sbuf_tensor(f"wg{i}", [128, 8, 256], BF16)) for i in range(NGU)]
            wu = [Es(nc.sbuf_tensor(f"wu{i}", [128, 8, 256], BF16)) for i in range(NGU)]
            wd = [Es(nc.sbuf_tensor(f"wd{i}", [128, 2, 512], BF16)) for i in range(ND)]
            MK = [Es(nc.psum_tensor(f"MK{i}", [128, 512], F32)) for i in range(8)]
            MK6b = MK[6][:].bitcast(BF16)
            MK7b = MK[7][:].bitcast(BF16)
            gsum = CF("gsum")
            P.dma("sp", affP[:], affTbuf.rearrange("e (j c) -> (e j) c", j=8), reads=["affTbuf"], writes=["affP"])
            P.dma("sp", afft[:], affbuf.rearrange("(n p) e -> p n e", p=128), reads=["affbuf"], writes=["afft"])
            P.dma("sp", gf3[:], g_ffn.rearrange("(c p) -> p c", p=128), writes=["gf3"], allow_slow_non_contiguous=True)
            NIT = 28
            P.op("dve", lambda e: e.memset(bs[:, 0:1], 0.0), writes=["bs_lo"])
            P.op("dve", lambda e: e.memset(bs[:, 1:2], 0.5), writes=["bs_mid"])
            for it in range(NIT):
                wdt = 2.0 ** -(it + 1)
                P.op("dve", lambda e: e.tensor_scalar(out=bjunk[:], in0=affP[:], scalar1=bs[:, 1:2], scalar2=0.0, op0=ALU.is_ge, op1=ALU.add, accum_out=bs[:, 2:3]), reads=["affP", "bs_mid"], writes=["bjunk", "bs_cnt"])
                P.op("pe", lambda e: e.matmul(MK[4][:, 0:1], lhsT=gsum, rhs=bs[:, 2:3], start=True, stop=True), reads=["cf", "bs_cnt"], writes=["MK4"])
                P.op("dve", lambda e: e.tensor_scalar(out=bs[:, 3:4], in0=MK[4][:, 0:1], scalar1=CAP - 0.5, scalar2=None, op0=ALU.is_ge), reads=["MK4"], writes=["bs_ge"])
                P.op("dve", lambda e, wdt=wdt: e.scalar_tensor_tensor(out=bs[:, 0:1], in0=bs[:, 3:4], scalar=wdt, in1=bs[:, 0:1], op0=ALU.mult, op1=ALU.add), reads=["bs_ge", "bs_lo"], writes=["bs_lo"])
                P.op("dve", lambda e, wdt=wdt: e.tensor_scalar(out=bs[:, 1:2], in0=bs[:, 0:1], scalar1=wdt / 2, scalar2=None, op0=ALU.add), reads=["bs_lo"], writes=["bs_mid"])
            P.dma("sp", taubuf.rearrange("(p o) -> p o", o=1), bs[:, 0:1], reads=["bs_lo"], writes=["taubuf"])
            P.dma("sp", taub[:], taubuf.rearrange("(e j) -> e j", j=8)[:, 0:1].rearrange("e o -> (e o)").partition_broadcast(128), reads=["taubuf"], writes=["taub"], allow_slow_non_contiguous=True)
            for n in range(NT):
                P.op("dve", lambda e, n=n: e.tensor_tensor(out=mask[:, n, :], in0=afft[:, n, :], in1=taub[:], op=ALU.is_ge), reads=["afft", "taub"], writes=["mask"])
            m2 = mask[:].rearrange("p n e -> p (n e)")
            P.op("pe", lambda e: e.matmul(MK[4][:], lhsT=CF("utri"), rhs=m2, start=True, stop=True), reads=["cf", "mask"], writes=["MK4"])
            P.op("pe", lambda e: e.matmul(MK[5][:], lhsT=CF("ones"), rhs=m2, start=True, stop=True), reads=["cf", "mask"], writes=["MK5"])
            P.op("dve", lambda e: e.tensor_copy(out=pa[:].rearrange("p n e -> p (n e)"), in_=MK[5][:]), reads=["MK5"], writes=["pa"])
            src, sk_, dst, dk_ = pa, "pa", pb_, "pb_"
            for sft in (1, 2, 4, 8, 16):
                P.op("dve", lambda e, src=src, dst=dst, sft=sft: e.tensor_tensor(out=dst[:, sft:, :], in0=src[:, sft:, :], in1=src[:, :NT - sft, :], op=ALU.add), reads=[sk_], writes=[dk_])
                P.op("dve", lambda e, src=src, dst=dst, sft=sft: e.tensor_copy(out=dst[:, :sft, :], in_=src[:, :sft, :]), reads=[sk_], writes=[dk_])
                src, sk_, dst, dk_ = dst, dk_, src, sk_
            P.op("dve", lambda e, src=src: e.tensor_tensor(out=slot[:].rearrange("p n e -> p (n e)"), in0=MK[4][:], in1=src[:].rearrange("p n e -> p (n e)"), op=ALU.add), reads=["MK4", sk_], writes=["slot"])
            P.op("dve", lambda e: e.tensor_tensor(out=slot[:].rearrange("p n e -> p (n e)"), in0=slot[:].rearrange("p n e -> p (n e)"), in1=MK[5][:], op=ALU.subtract), reads=["slot", "MK5"], writes=["slot"])
            P.op("dve", lambda e: e.tensor_tensor(out=slot[:], in0=slot[:], in1=mask[:], op=ALU.mult), reads=["slot", "mask"], writes=["slot"])
            P.op("dve", lambda e: e.tensor_scalar(out=slot[:], in0=slot[:], scalar1=-1.0, scalar2=None, op0=ALU.add), reads=["slot"], writes=["slot"])
            thl = CF("thl").rearrange("p (n t) -> p n t", t=2)
            P.op("dve", lambda e: e.memset(R[:], 0.0), writes=["R"])
            for ee in range(NE):
                P.op("dve", lambda e, ee=ee: e.tensor_copy(out=R[:, :, ee, 0:2], in_=thl), reads=["cf"], writes=["R"])
            P.op("dve", lambda e: e.tensor_copy(out=rbf[:], in_=afft[:]), reads=["afft"], writes=["rbf"])
            P.op("dve", lambda e: e.tensor_copy(out=R[:, :, :, 2], in_=rbf[:]), reads=["rbf"], writes=["R"])
            P.op("dve", lambda e: e.tensor_tensor(out=ra[:], in0=afft[:], in1=rbf[:], op=ALU.subtract), reads=["afft", "rbf"], writes=["ra"])
            P.op("dve", lambda e: e.tensor_copy(out=rbf[:], in_=ra[:]), reads=["ra"], writes=["rbf"])
            P.op("dve", lambda e: e.tensor_copy(out=R[:, :, :, 3], in_=rbf[:]), reads=["rbf"], writes=["R"])
            P.op("dve", lambda e: e.tensor_tensor(out=ra[:], in0=ra[:], in1=rbf[:], op=ALU.subtract), reads=["ra", "rbf"], writes=["ra"])
            P.op("dve", lambda e: e.tensor_copy(out=R[:, :, :, 4], in_=ra[:]), reads=["ra"], writes=["R"])

            gu_chunks = [(ee, pr) for ee in range(NE) for pr in range(NFC // 2)]
            d_chunks = [(ee, half, pr) for ee in range(NE) for half in range(2) for pr in range(NFC // 2)]
            gu_next = [0]
            d_next = [0]

            def emit_gu_load():
                i = gu_next[0]
                if i >= len(gu_chunks):
                    return
                gu_next[0] += 1
                ee, pr = gu_chunks[i]
                sl = i % NGU
                P.dma("pool", wg[sl][:], w_gate[ee][:, pr * 256:(pr + 1) * 256].rearrange("(c p) f -> p c f", p=128), writes=[f"wg{sl}"])
                P.dma("pool", wu[sl][:], w_up[ee][:, pr * 256:(pr + 1) * 256].rearrange("(c p) f -> p c f", p=128), writes=[f"wu{sl}"])

            def emit_d_load():
                i = d_next[0]
                if i >= len(d_chunks):
                    return
                d_next[0] += 1
                ee, half, pr = d_chunks[i]
                sl = i % ND
                P.dma("pool", wd[sl][:], w_down[ee][pr * 256:(pr + 1) * 256, half * 512:(half + 1) * 512].rearrange("(c p) d -> p c d", p=128), writes=[f"wd{sl}"])

            for _ in range(NGU):
                emit_gu_load()
            for _ in range(ND):
                emit_d_load()

            def sel_onehot(ee, n):
                oi = n % 4
                P.op("dve", lambda e, n=n, oi=oi, ee=ee: e.tensor_scalar(out=oh[oi][:], in0=CF("iota"), scalar1=slot[:, n, ee:ee + 1], scalar2=None, op0=ALU.is_equal), reads=["cf", "slot"], writes=[f"oh{oi}"])

            def sel_mm(ee, n):
                oi = n % 4
                for sb in range(4):
                    P.op("pe", lambda e, n=n, oi=oi, sb=sb, ee=ee: e.matmul(MK[4][:, sb * 8:sb * 8 + 6], lhsT=oh[oi][:, sb * 128:(sb + 1) * 128], rhs=R[:, n, ee, :], start=(n == 0 and sb == 0), stop=(n == NT - 1), skip_group_check=True), reads=[f"oh{oi}", "R"], writes=["MK4"])

            def prep_idx(ee):
                b = ee % 2
                P.op("dve", lambda e: e.tensor_copy(out=pIs[:], in_=MK[4][:, 0:32]), reads=["MK4"], writes=["pIs"])
                pI = pIs[:].rearrange("p (s k) -> p s k", k=8)
                P.op("dve", lambda e, b=b: e.scalar_tensor_tensor(out=idxf[b][:], in0=pI[:, :, 0], scalar=64.0, in1=pI[:, :, 1], op0=ALU.mult, op1=ALU.add), reads=["pIs"], writes=[f"idxf{b}"])
                P.op("dve", lambda e, b=b: e.tensor_copy(out=idxi[b][:], in_=idxf[b][:]), reads=[f"idxf{b}"], writes=[f"idxi{b}"])
                P.op("dve", lambda e, b=b: e.tensor_tensor(out=valt[b][:], in0=pI[:, :, 2], in1=pI[:, :, 3], op=ALU.add), reads=["pIs"], writes=[f"valt{b}"])
                P.op("dve", lambda e, b=b: e.tensor_tensor(out=valt[b][:], in0=valt[b][:], in1=pI[:, :, 4], op=ALU.add), reads=["pIs", f"valt{b}"], writes=[f"valt{b}"])
                for sb in range(4):
                    P.op("pool", lambda e, sb=sb, b=b: e.indirect_dma_start(out=xs[b][:, sb, :], out_offset=None, in_=h2buf, in_offset=bass.IndirectOffsetOnAxis(ap=idxi[b][:, sb:sb + 1], axis=0)), reads=[f"idxi{b}", "h2buf"], writes=[f"xs{b}_{sb}"], dma=True)

            def prep_tr(ee):
                b = ee % 2
                for dk in range(8):
                    bank, bv = (6, MK6b) if dk % 2 == 0 else (7, MK7b)
                    for sb in range(4):
                        P.op("pe", lambda e, dk=dk, sb=sb, b=b, bv=bv: e.transpose(out=bv[:, sb * 128:(sb + 1) * 128], in_=xs[b][:, sb, dk * 128:(dk + 1) * 128], identity=idb), reads=[f"xs{b}_{sb}", "cb"], writes=[f"MK{bank}"])
                    P.op("act", lambda e, dk=dk, b=b, bv=bv: e.activation(out=xsT[b][:, dk, :], in_=bv[:, 0:512], func=AF.Copy, scale=gf3[:, dk:dk + 1]), reads=[f"MK{bank}", "gf3"], writes=[f"xsT{b}"])

            gu_i = [0]
            d_i = [0]

            def gateup(ee, nxt):
                b = ee % 2
                npair = NFC // 2
                groups = [list(range(3 * pr, min(3 * pr + 3, NT))) for pr in range(npair)]
                groups[-1] = list(range(3 * (npair - 1), NT))
                for pr in range(npair):
                    i = gu_i[0]
                    gu_i[0] += 1
                    sl = i % NGU
                    for fl in range(2):
                        fc = pr * 2 + fl
                        gb = fc % 2
                        ub = 2 + fc % 2
                        for dk in range(8):
                            P.op("pe", lambda e, dk=dk, fl=fl, sl=sl, gb=gb, b=b: e.matmul(MK[gb][:], lhsT=wg[sl][:, dk, fl * 128:(fl + 1) * 128], rhs=xsT[b][:, dk, :], start=(dk == 0), stop=(dk == 7)), reads=[f"wg{sl}", f"xsT{b}"], writes=[f"MK{gb}"])
                        for dk in range(8):
                            P.op("pe", lambda e, dk=dk, fl=fl, sl=sl, ub=ub, b=b: e.matmul(MK[ub][:], lhsT=wu[sl][:, dk, fl * 128:(fl + 1) * 128], rhs=xsT[b][:, dk, :], start=(dk == 0), stop=(dk == 7)), reads=[f"wu{sl}", f"xsT{b}"], writes=[f"MK{ub}"])
                        P.op("act", lambda e, gb=gb: e.activation(out=sgt[gb][:], in_=MK[gb][:], func=AF.Silu), reads=[f"MK{gb}"], writes=[f"sgt{gb}"])
                        P.op("dve", lambda e, gb=gb, ub=ub, fc=fc, b=b: e.tensor_tensor(out=hTm[b][:, fc, :], in0=MK[ub][:], in1=sgt[gb][:], op=ALU.mult), reads=[f"MK{ub}", f"sgt{gb}"], writes=[f"hTm{b}"])
                    emit_gu_load()
                    if nxt is not None:
                        if pr >= 1:
                            for n in groups[pr - 1]:
                                sel_mm(nxt, n)
                        for n in groups[pr]:
                            sel_onehot(nxt, n)
                if nxt is not None:
                    for n in groups[npair - 1]:
                        sel_mm(nxt, n)

            def down(ee, nxt):
                b = ee % 2
                for half in range(2):
                    for pr in range(NFC // 2):
                        i = d_i[0]
                        d_i[0] += 1
                        sl = i % ND
                        for fl in range(2):
                            fc = pr * 2 + fl
                            for sb in range(4):
                                P.op("pe", lambda e, fc=fc, fl=fl, sb=sb, sl=sl, b=b: e.matmul(MK[sb][:], lhsT=hTm[b][:, fc, sb * 128:(sb + 1) * 128], rhs=wd[sl][:, fl, :], start=(fc == 0), stop=(fc == NFC - 1)), reads=[f"hTm{b}", f"wd{sl}"], writes=[f"MK{sb}"])
                        emit_d_load()
                    for sb in range(4):
                        if sb < 2:
                            P.op("act", lambda e, sb=sb, half=half, b=b: e.activation(out=yt[:, sb, half * 512:(half + 1) * 512], in_=MK[sb][:], func=AF.Copy, scale=valt[b][:, sb:sb + 1]), reads=[f"MK{sb}", f"valt{b}"], writes=[f"yt{sb}"])
                        else:
                            P.op("dve", lambda e, sb=sb, half=half, b=b: e.tensor_scalar(out=yt[:, sb, half * 512:(half + 1) * 512], in0=MK[sb][:], scalar1=valt[b][:, sb:sb + 1], scalar2=None, op0=ALU.mult), reads=[f"MK{sb}", f"valt{b}"], writes=[f"yt{sb}"])
                    if half == 0 and nxt is not None:
                        prep_tr(nxt)
                for sb in range(4):
                    P.op("pool", lambda e, sb=sb, b=b: e.indirect_dma_start(out=x1buf, out_offset=bass.IndirectOffsetOnAxis(ap=idxi[b][:, sb:sb + 1], axis=0), in_=yt[:, sb, :], in_offset=None, compute_op=ALU.add), reads=[f"idxi{b}", f"yt{sb}"] + [f"x1p{1 - b}_{q}" for q in range(4)], writes=[f"x1p{b}_{sb}"], dma=True)

            import os
            NEX = int(os.environ.get("KDBG_NEX", NE))
            for n in range(NT):
                sel_onehot(0, n)
                sel_mm(0, n)
            prep_idx(0)
            prep_tr(0)
            for ee in range(NEX):
                nxt = ee + 1 if ee + 1 < NEX else None
                gateup(ee, nxt)
                if nxt is not None:
                    prep_idx(nxt)
                down(ee, nxt)
            P.flush()
        if stop_after <= 4:
            P.finish()
            return nc

        with ExitStack() as es:
            Es = es.enter_context
            gfin = Es(nc.sbuf_tensor("gfin", [128, D], F32))
            xf = [Es(nc.sbuf_tensor(f"xf{i}", [128, D], F32)) for i in range(3)]
            of = [Es(nc.sbuf_tensor(f"of{i}", [128, D], F32)) for i in range(3)]
            fj = Es(nc.sbuf_tensor("fj", [128, D], BF16))
            fs = [Es(nc.sbuf_tensor(f"fs{i}", [128, 1], F32)) for i in range(3)]
            P.dma("sp", gfin[:], g_final.partition_broadcast(128), writes=["gfin"])
            for i in range(NT):
                a = i % 3
                rows = slice(i * 128, (i + 1) * 128)
                P.dma("sp", xf[a][:], x1buf[rows, :], reads=["x1buf"] + [f"x1p{pp}_{q}" for pp in range(2) for q in range(4)], writes=[f"xf{a}"])
                P.op("act", lambda e, a=a: e.activation(out=fj[:], in_=xf[a][:], func=AF.Square, accum_out=fs[a][:]), reads=[f"xf{a}"], writes=["fj", f"fs{a}"])
                rstd_from_ss(fs[a][:], f"fs{a}", D)
                P.op("dve", lambda e, a=a: e.scalar_tensor_tensor(out=of[a][:], in0=xf[a][:], scalar=fs[a][:], in1=gfin[:], op0=ALU.mult, op1=ALU.mult), reads=[f"xf{a}", f"fs{a}", "gfin"], writes=[f"of{a}"])
                P.dma("pool", out[rows, :], of[a][:], reads=[f"of{a}"], writes=[f"out{i}"])
            P.flush()
        P.finish()
    return nc


def make_in_maps(inputs, ncores=4, moe=False):
    cfa_np, CO, cb_np = _consts()
    x = np.asarray(inputs["x"], np.float32)
    posn = np.asarray(inputs["positions"]).astype(np.int32)
    lamv = np.concatenate([np.asarray(inputs[k], np.float32).reshape(1, 64) for k in ("lam_q1", "lam_k1", "lam_q2", "lam_k2")], axis=0)
    fixt = np.stack([_fix_tables(0), _fix_tables(1)], axis=0).astype(np.float32)
    maps = []
    for c in range(ncores):
        b = c
        xb = np.ascontiguousarray(x[b])
        xh = np.zeros((2, 256, D), np.float32)
        xh[0, 128:256] = xb[HQ:HQ + 128]
        xh[1, 0:128] = xb[HQ - 128:HQ]
        m = {
            "x": xb, "xhalo": xh, "pos": np.ascontiguousarray(posn[b]),
            "g_mix": np.asarray(inputs["g_mix"], np.float32).reshape(D),
            "w_in": np.asarray(inputs["w_in"], np.float32)[0],
            "w_pool_mix": np.asarray(inputs["w_pool_mix"], np.float32)[0],
            "pool_scale": np.asarray(inputs["pool_scale"], np.float32).reshape(D),
            "w_pool_out": np.asarray(inputs["w_pool_out"], np.float32)[0],
            "lamv": lamv,
            "g_subln": np.asarray(inputs["g_subln"], np.float32).reshape(128),
            "w_attn_out": np.asarray(inputs["w_attn_out"], np.float32)[0],
            "w_out": np.asarray(inputs["w_out"], np.float32)[0],
            "g_ffn": np.asarray(inputs["g_ffn"], np.float32).reshape(D),
            "w_router": np.asarray(inputs["w_router"], np.float32)[0],
            "w_gate": np.asarray(inputs["w_gate"], np.float32)[0],
            "w_up": np.asarray(inputs["w_up"], np.float32)[0],
            "w_down": np.asarray(inputs["w_down"], np.float32)[0],
            "g_final": np.asarray(inputs["g_final"], np.float32).reshape(D),
            "cfa": cfa_np, "cba": cb_np, "fixt": fixt,
        }
        if not moe:
            for k in ("w_gate", "w_up", "w_down"):
                m.pop(k)
        maps.append(m)
    return maps


def kernel(**inputs):
    nc = build()
    maps = make_in_maps(inputs, 4, moe=True)
    res = run_bass_kernel_spmd(nc, maps, core_ids=list(range(4)))
    return np.stack([np.asarray(r["out"], np.float32) for r in res.results], axis=0)
```

```python
import math
from contextlib import ExitStack

import numpy as np
import ml_dtypes
import concourse.bass as bass
import concourse.mybir as mybir
from concourse.bass_utils import run_bass_kernel_spmd

F32 = mybir.dt.float32
BF16 = mybir.dt.bfloat16
I32 = mybir.dt.int32
AF = mybir.ActivationFunctionType
ALU = mybir.AluOpType
AX = mybir.AxisListType

S = 4096
D = 1024
NT = 32
HQ = 2048
NH = 8
NE = 16
CAP = 512
DFF = 2816
NFC = 22
EPS = 1e-6
SCALE = 0.125
LAM_INIT = 0.8 - 0.6 * math.exp(0.0)
ROPE_THETA = 500000.0
WINS = (2, 4, 8, 16)
C_Q, C_K, C_V, C_GP, C_GA = 1024, 2048, 3072, 4096, 5120

ENGS = ("pe", "act", "dve", "pool", "sp")
DMAQ = ("sp", "act", "pool")
NDS = 14


class _Rec:
    __slots__ = ("eng", "fn", "deps", "is_dma", "sig", "sigval", "dsem", "dval", "prevwait", "stage")

    def __init__(self, eng, fn, is_dma, stage):
        self.eng = eng
        self.fn = fn
        self.deps = set()
        self.is_dma = is_dma
        self.sig = False
        self.sigval = 0
        self.dsem = None
        self.dval = 0
        self.prevwait = None
        self.stage = stage


class Prog:
    def __init__(self, nc, sems, dsems):
        self.nc = nc
        self.sems = sems
        self.dsems = dsems
        self.pending = {e: [] for e in ENGS}
        self.bufs = {}
        self.stage = 0
        self.sigc = {e: 0 for e in ENGS}
        self.dcount = {e: [0] * NDS for e in DMAQ}
        self.di = {e: 0 for e in DMAQ}
        self.waited = {e: {} for e in ENGS}

    def op(self, eng, fn, reads=(), writes=(), dma=False):
        r = _Rec(eng, fn, dma, self.stage)
        for k in reads:
            b = self.bufs.setdefault(k, [None, []])
            if b[0] is not None:
                r.deps.add(b[0])
        for k in writes:
            b = self.bufs.setdefault(k, [None, []])
            if b[0] is not None:
                d = b[0]
                if dma or d.is_dma or d.eng != eng:
                    r.deps.add(d)
            for d in b[1]:
                if dma or d.is_dma or d.eng != eng:
                    r.deps.add(d)
        for k in reads:
            self.bufs[k][1].append(r)
        for k in writes:
            self.bufs[k] = [r, []]
        r.deps.discard(r)
        self.pending[eng].append(r)
        return r

    def dma(self, eng, out, in_, reads=(), writes=(), **kw):
        return self.op(eng, lambda e: e.dma_start(out=out, in_=in_, **kw), reads, writes, dma=True)

    def flush(self):
        nc = self.nc
        st = self.stage
        allr = [r for e in ENGS for r in self.pending[e]]
        for r in allr:
            r.deps = {d for d in r.deps if d.stage == st}
            for d in r.deps:
                if not d.is_dma:
                    d.sig = True
        for e in ENGS:
            lastc = None
            for r in self.pending[e]:
                if not r.is_dma:
                    lastc = r
            if lastc is not None:
                lastc.sig = True
        bar = []
        if st > 0:
            for e in ENGS:
                if self.sigc[e] > 0:
                    bar.append((self.sems[e], self.sigc[e]))
            for e in DMAQ:
                for s in range(NDS):
                    if self.dcount[e][s] > 0:
                        bar.append((self.dsems[e][s], 16 * self.dcount[e][s]))
        for e in ENGS:
            for r in self.pending[e]:
                if r.is_dma:
                    s = self.di[e] % NDS
                    self.di[e] += 1
                    r.dsem = self.dsems[e][s]
                    if self.dcount[e][s] > 0:
                        r.prevwait = (r.dsem, 16 * self.dcount[e][s])
                    self.dcount[e][s] += 1
                    r.dval = 16 * self.dcount[e][s]
                elif r.sig:
                    self.sigc[e] += 1
                    r.sigval = self.sigc[e]
        pend = self.pending
        sems = self.sems
        waited_all = self.waited

        def run(e, engobj):
            waited = waited_all[e]

            def w(s, v):
                if waited.get(id(s), 0) >= v:
                    return
                waited[id(s)] = v
                engobj.wait_ge(s, v)

            for (s, v) in bar:
                w(s, v)
            for r in pend[e]:
                if r.prevwait is not None:
                    w(*r.prevwait)
                for d in r.deps:
                    if d.is_dma:
                        w(d.dsem, d.dval)
                    else:
                        w(sems[d.eng], d.sigval)
                ins = r.fn(engobj)
                if r.is_dma:
                    ins.then_inc(r.dsem, 16)
                elif r.sig:
                    ins.then_inc(sems[e], 1)

        with nc.Block() as block:
            @block.sync
            def _(eng):
                run("sp", eng)

            @block.scalar
            def _(eng):
                run("act", eng)

            @block.vector
            def _(eng):
                run("dve", eng)

            @block.gpsimd
            def _(eng):
                run("pool", eng)

            @block.tensor
            def _(eng):
                run("pe", eng)

        self.pending = {e: [] for e in ENGS}
        self.stage += 1

    def finish(self):
        nc = self.nc
        fin = []
        for e in DMAQ:
            for s in range(NDS):
                if self.dcount[e][s] > 0:
                    fin.append((self.dsems[e][s], 16 * self.dcount[e][s]))
        for e in ENGS:
            if self.sigc[e] > 0:
                fin.append((self.sems[e], self.sigc[e]))
        with nc.Block() as block:
            @block.sync
            def _(eng):
                for (s, v) in fin:
                    eng.wait_ge(s, v)


def _consts():
    cf = {}
    p = np.arange(128)
    cf["idf"] = np.eye(128, dtype=np.float32)
    cf["ones"] = np.ones((128, 128), np.float32)
    cf["utri"] = (p[:, None] <= p[None, :]).astype(np.float32)
    cf["gsum"] = ((p[:, None] // 8) == (p[None, :] // 8)).astype(np.float32)
    cf["iota"] = np.tile(np.arange(512, dtype=np.float32)[None, :], (128, 1))
    half = 8
    inv = ROPE_THETA ** (-np.arange(half, dtype=np.float32) * 2.0 / 16)
    rinv = np.zeros((128, 4), np.float32)
    for base in (0, 64):
        for j in range(8):
            rinv[base + j, 0] = inv[j]
            rinv[base + 8 + j, 0] = inv[j]
            rinv[base + j, 1] = -1.0
            rinv[base + 8 + j, 1] = 1.0
    cf["rinv"] = rinv
    T = np.arange(S).reshape(NT, 128).T
    thl = np.zeros((128, NT, 2), np.float32)
    thl[:, :, 0] = T // 64
    thl[:, :, 1] = T % 64
    cf["thl"] = thl.reshape(128, NT * 2)
    names = ["idf", "ones", "utri", "gsum", "iota", "rinv", "thl"]
    offs = {}
    o = 0
    for n in names:
        offs[n] = (o, cf[n].shape[1])
        o += cf[n].shape[1]
    cfa = np.concatenate([cf[n] for n in names], axis=1).astype(np.float32)
    perm = np.zeros((128, 128), np.float32)
    for base in (0, 64):
        for j in range(8):
            perm[base + j + 8, base + j] = 1.0
            perm[base + j, base + j + 8] = 1.0
    cb = np.concatenate([np.eye(128, dtype=np.float32), perm, np.ones((128, 128), np.float32)], axis=1).astype(ml_dtypes.bfloat16)
    return cfa, offs, cb


def _fix_tables(hf):
    fs = np.ones((128, 4, 8), np.float32)
    fe = np.ones((128, 4, 8), np.float32)
    for g, w in enumerate(WINS):
        for j in range(8):
            if hf == 0:
                t = j
                lo = max(t - w // 2, 0)
                hi = min(t + w // 2, S)
                fs[:, g, j] = w / (hi - lo)
            if hf == 1:
                t = S - 8 + j
                lo = max(t - w // 2, 0)
                hi = min(t + w // 2, S)
                fe[:, g, j] = w / (hi - lo)
    return np.concatenate([fs.reshape(128, 32), fe.reshape(128, 32)], axis=1)


def build(stop_after=99, dbg=False):
    nc = bass.Bass("TRN2", target_bir_lowering=False)
    cfa_np, CO, cb_np = _consts()
    NCF = cfa_np.shape[1]

    def din(name, shape, dt=F32):
        return nc.dram_tensor(name, list(shape), dt, kind="ExternalInput").ap()

    x = din("x", [S, D])
    xhalo = din("xhalo", [2, 256, D])
    pos = din("pos", [S], I32)
    g_mix = din("g_mix", [D])
    w_in = din("w_in", [D, 6144])
    w_pool_mix = din("w_pool_mix", [4, 256, 256])
    pool_scale = din("pool_scale", [D])
    w_pool_out = din("w_pool_out", [D, D])
    lamv = din("lamv", [4, 64])
    g_subln = din("g_subln", [128])
    w_attn_out = din("w_attn_out", [D, D])
    w_out = din("w_out", [D, D])
    g_ffn = din("g_ffn", [D])
    w_router = din("w_router", [D, NE])
    if stop_after >= 4:
        w_gate = din("w_gate", [NE, D, DFF])
        w_up = din("w_up", [NE, D, DFF])
        w_down = din("w_down", [NE, DFF, D])
    g_final = din("g_final", [D])
    cfa = din("cfa", [128, NCF])
    cba = din("cba", [128, 384], BF16)
    fixt = din("fixt", [2, 128, 64])
    out = nc.dram_tensor("out", [S, D], F32, kind="ExternalOutput").ap()

    def dscr(name, shape, dt):
        kind = "ExternalOutput" if dbg else "Internal"
        return nc.dram_tensor(name, list(shape), dt, kind=kind).ap()

    Abuf = dscr("Abuf", [2, D, HQ], BF16)
    Gabuf = dscr("Gabuf", [2, D, HQ], BF16)
    oTbuf = dscr("oTbuf", [2, D, HQ], BF16)
    x1buf = dscr("x1buf", [S, D], F32)
    h2buf = dscr("h2buf", [S, D], BF16)
    affbuf = dscr("affbuf", [S, NE], F32)
    affTbuf = dscr("affTbuf", [NE, S], F32)
    taubuf = dscr("taubuf", [128], F32)
    if dbg:
        hTdbg = dscr("hTdbg", [128, 8, S], BF16)
        wbdbg = dscr("wbdbg", [128, 8, 1024], BF16)

    with ExitStack() as es0:
        E0 = es0.enter_context
        sems = {e: E0(nc.semaphore("s_" + e)) for e in ENGS}
        dsems = {e: [E0(nc.semaphore(f"d_{e}{i}")) for i in range(NDS)] for e in DMAQ}
        P = Prog(nc, sems, dsems)
        cf = E0(nc.sbuf_tensor("cf", [128, NCF], F32))
        cb = E0(nc.sbuf_tensor("cb", [128, 384], BF16))
        P.dma("sp", cf[:], cfa, writes=["cf"])
        P.dma("sp", cb[:], cba, writes=["cb"])

        def CF(n):
            o, w = CO[n]
            return cf[:, o:o + w]

        idb = cb[:, 0:128]
        permb = cb[:, 128:256]
        onesb = cb[:, 256:384]
        idf = CF("idf")

        def rstd_from_ss(ss, tag, scale_n):
            P.op("dve", lambda e: e.tensor_scalar(out=ss, in0=ss, scalar1=1.0 / scale_n, scalar2=EPS, op0=ALU.mult, op1=ALU.add), reads=[tag], writes=[tag])
            P.op("act", lambda e: e.activation(out=ss, in_=ss, func=AF.Ln), reads=[tag], writes=[tag])
            P.op("act", lambda e: e.activation(out=ss, in_=ss, func=AF.Exp, scale=-0.5), reads=[tag], writes=[tag])

        with ExitStack() as esA:
            EA = esA.enter_context
            hT = EA(nc.sbuf_tensor("hT", [128, 8, S], BF16))
            hTh = EA(nc.sbuf_tensor("hTh", [128, 8, 512], BF16))
            with ExitStack() as es:
                Es = es.enter_context
                xt = [Es(nc.sbuf_tensor(f"xt{i}", [128, D], F32)) for i in range(6)]
                xn = [Es(nc.sbuf_tensor(f"xn{i}", [128, D], BF16)) for i in range(4)]
                junk = Es(nc.sbuf_tensor("junk0", [128, D], BF16))
                ssq = [Es(nc.sbuf_tensor(f"ss{i}", [128, 1], F32)) for i in range(6)]
                gm = Es(nc.sbuf_tensor("gm", [128, 8], F32))
                gexp = Es(nc.sbuf_tensor("gexp", [128, 8, 128], F32))
                pT = [Es(nc.psum_tensor(f"pT{i}", [128, 8, 128], BF16)) for i in range(4)]
                P.dma("sp", gm[:], g_mix.rearrange("(c p) -> p c", p=128), writes=["gm"], allow_slow_non_contiguous=True)
                for c in range(8):
                    P.op("dve", lambda e, c=c: e.tensor_scalar(out=gexp[:, c, :], in0=CF("ones"), scalar1=gm[:, c:c + 1], scalar2=None, op0=ALU.mult), reads=["cf", "gm"], writes=["gexp"])
                def s0_info(i):
                    if i < NT:
                        return x[i * 128:(i + 1) * 128, :], hT[:, :, i * 128:(i + 1) * 128], "hT"
                    j = i - NT
                    return xhalo[j // 2, (j % 2) * 128:(j % 2 + 1) * 128, :], hTh[:, :, j * 128:(j + 1) * 128], "hTh"

                def s0_a(i):
                    a = i % 6
                    src, dst, dkey = s0_info(i)
                    P.dma("sp", xt[a][:], src, writes=[f"xt{a}"])
                    P.op("act", lambda e, a=a: e.activation(out=junk[:], in_=xt[a][:], func=AF.Square, accum_out=ssq[a][:]), reads=[f"xt{a}"], writes=["junk0", f"ss{a}"])
                    P.op("dve", lambda e, a=a: e.tensor_scalar(out=ssq[a][:], in0=ssq[a][:], scalar1=1.0 / D, scalar2=EPS, op0=ALU.mult, op1=ALU.add), reads=[f"ss{a}"], writes=[f"ss{a}"])

                def s0_b(i):
                    a, b2, c2 = i % 6, i % 4, i % 4
                    src, dst, dkey = s0_info(i)
                    P.op("act", lambda e, a=a: e.activation(out=ssq[a][:], in_=ssq[a][:], func=AF.Ln), reads=[f"ss{a}"], writes=[f"ss{a}"])
                    P.op("act", lambda e, a=a: e.activation(out=ssq[a][:], in_=ssq[a][:], func=AF.Exp, scale=-0.5), reads=[f"ss{a}"], writes=[f"ss{a}"])
                    P.op("act", lambda e, a=a, b2=b2: e.activation(out=xn[b2][:], in_=xt[a][:], func=AF.Copy, scale=ssq[a][:]), reads=[f"xt{a}", f"ss{a}"], writes=[f"xn{b2}"])
                    for c in range(8):
                        P.op("pe", lambda e, c=c, b2=b2, c2=c2: e.transpose(out=pT[c2][:, c, :], in_=xn[b2][:, c * 128:(c + 1) * 128], identity=idb), reads=[f"xn{b2}", "cb"], writes=[f"pT{c2}"])
                    P.op("dve", lambda e, c2=c2, dst=dst: e.tensor_tensor(out=dst, in0=pT[c2][:], in1=gexp[:], op=ALU.mult), reads=[f"pT{c2}", "gexp"], writes=[dkey])

                s0_a(0)
                s0_a(1)
                for i in range(NT + 4):
                    if i + 2 < NT + 4:
                        s0_a(i + 2)
                    s0_b(i)
                if dbg:
                    for c in range(8):
                        P.dma("sp", hTdbg[:, c, :], hT[:, c, :], reads=["hT"])
                P.flush()
            if stop_after <= 0:
                P.finish()
                return nc

            with ExitStack() as es:
                Es = es.enter_context
                wb = [Es(nc.sbuf_tensor(f"wb{i}", [128, 8, 1024], BF16)) for i in range(2)]
                wmix = Es(nc.sbuf_tensor("wmix", [128, 8, 256], BF16))
                psc = Es(nc.sbuf_tensor("psc", [128, 8], F32))
                fx = Es(nc.sbuf_tensor("fx", [128, 64], F32))
                u = [Es(nc.sbuf_tensor(f"u{i}", [128, 2304], F32)) for i in range(2)]
                wa = [Es(nc.sbuf_tensor(f"wa{i}", [128, 2304], F32)) for i in range(2)]
                pooled = [Es(nc.sbuf_tensor(f"pooled{i}", [128, 2, HQ], BF16)) for i in range(2)]
                pmT = Es(nc.sbuf_tensor("pmT", [128, 8, HQ], BF16))
                sg = [Es(nc.sbuf_tensor(f"sg{i}", [128, 512], F32)) for i in range(2)]
                ast = [Es(nc.sbuf_tensor(f"ast{i}", [128, 512], BF16)) for i in range(3)]
                pu = [Es(nc.psum_tensor(f"pu{i}", [128, 512], F32)) for i in range(4)]
                P.dma("sp", psc[:], pool_scale.rearrange("(c p) -> p c", p=128), writes=["psc"], allow_slow_non_contiguous=True)
                for g in range(4):
                    P.dma("pool", wmix[:, 2 * g:2 * g + 2, :], w_pool_mix[g].rearrange("(c p) d -> p c d", p=128), writes=[f"wmix{g}"])

                def load_w(i, src_ap, key):
                    v = src_ap.rearrange("(c p) n -> p c n", p=128)
                    for q in range(4):
                        P.dma("pool", wb[i][:, :, q * 256:(q + 1) * 256], v[:, :, q * 256:(q + 1) * 256], writes=[f"{key}_{q}"])

                WK0 = [f"wb0_{q}" for q in range(4)]
                WK1 = [f"wb1_{q}" for q in range(4)]
                npu = [0]

                def next_pu():
                    i = npu[0] % 4
                    npu[0] += 1
                    return i

                nst = [0]
                for hf in range(2):
                    own0 = hf * HQ
                    P.dma("sp", fx[:], fixt[hf], writes=["fx"])
                    load_w(0, w_in[:, 0:1024], "wb0")
                    if dbg and hf == 0:
                        for c in range(8):
                            P.dma("sp", wbdbg[:, c, :], wb[0][:, c, :], reads=WK0)
                    load_w(1, w_pool_out, "wb1")
                    blocks = [(0, 128, lambda dk, hf=hf: hTh[:, dk, (2 * hf) * 128:(2 * hf + 1) * 128], "hTh")]
                    for tb in range(4):
                        blocks.append((128 + tb * 512, 512, lambda dk, tb=tb, own0=own0: hT[:, dk, own0 + tb * 512:own0 + (tb + 1) * 512], "hT"))
                    blocks.append((128 + HQ, 128, lambda dk, hf=hf: hTh[:, dk, (2 * hf + 1) * 128:(2 * hf + 2) * 128], "hTh"))
                    pend_mix = []
                    for cc in range(8):
                        g = cc // 2
                        w = WINS[g]
                        ub = u[cc % 2]
                        uk = f"u{cc % 2}"
                        for (c0, n, rf, rk) in blocks:
                            pi = next_pu()
                            for dk in range(8):
                                P.op("pe", lambda e, pi=pi, dk=dk, n=n, rf=rf, cc=cc: e.matmul(pu[pi][:, 0:n], lhsT=wb[0][:, dk, cc * 128:(cc + 1) * 128], rhs=rf(dk), start=(dk == 0), stop=(dk == 7)), reads=WK0 + [rk], writes=[f"pu{pi}"])
                            P.op("act", lambda e, pi=pi, c0=c0, n=n, ub=ub: e.activation(out=ub[:, c0:c0 + n], in_=pu[pi][:, 0:n], func=AF.Copy), reads=[f"pu{pi}"], writes=[uk])
                        if pend_mix:
                            pend_mix.pop(0)()
                        cur, ck = ub, uk
                        lvl = [(1, 2304, 1, 0), (2, 2303, 1, -1), (4, 2301, 2, -2), (8, 2297, 4, -4)]
                        eng_alt = ["dve", "dve"]
                        for li in range(g + 1):
                            lo, hi, sp_, sm = lvl[li]
                            dstt = wa[li % 2]
                            dk_ = f"wa{li % 2}"
                            if li == 0:
                                P.op("dve", lambda e, dstt=dstt, cur=cur, lo=lo, hi=hi: e.tensor_tensor(out=dstt[:, lo:hi], in0=cur[:, lo - 1:hi - 1], in1=cur[:, lo:hi], op=ALU.add), reads=[ck], writes=[dk_])
                            else:
                                P.op(eng_alt[li % 2], lambda e, dstt=dstt, cur=cur, lo=lo, hi=hi, sp_=sp_: e.tensor_tensor(out=dstt[:, lo:hi], in0=cur[:, lo - sp_:hi - sp_], in1=cur[:, lo + sp_:hi + sp_], op=ALU.add), reads=[ck], writes=[dk_])
                            cur, ck = dstt, dk_
                        P.op("dve", lambda e, cur=cur, g=g: e.tensor_tensor(out=cur[:, 128:136], in0=cur[:, 128:136], in1=fx[:, g * 8:(g + 1) * 8], op=ALU.mult), reads=[ck, "fx"], writes=[ck])
                        P.op("dve", lambda e, cur=cur, g=g: e.tensor_tensor(out=cur[:, 128 + HQ - 8:128 + HQ], in0=cur[:, 128 + HQ - 8:128 + HQ], in1=fx[:, 32 + g * 8:32 + (g + 1) * 8], op=ALU.mult), reads=[ck, "fx"], writes=[ck])
                        pk = f"pooled{g % 2}"
                        P.op("dve", lambda e, cur=cur, ub=ub, g=g, cc=cc, w=w: e.scalar_tensor_tensor(out=pooled[g % 2][:, cc % 2, :], in0=cur[:, 128:128 + HQ], scalar=1.0 / w, in1=ub[:, 128:128 + HQ], op0=ALU.mult, op1=ALU.subtract), reads=[ck, uk], writes=[pk])
                        if cc % 2 == 1:
                            def _mix(g=g, pk=pk):
                                for dc in range(2):
                                    for tb in range(4):
                                        pi = next_pu()
                                        for c in range(2):
                                            P.op("pe", lambda e, pi=pi, c=c, g=g, dc=dc, tb=tb: e.matmul(pu[pi][:], lhsT=wmix[:, 2 * g + c, dc * 128:(dc + 1) * 128], rhs=pooled[g % 2][:, c, tb * 512:(tb + 1) * 512], start=(c == 0), stop=(c == 1)), reads=[f"wmix{g}", pk], writes=[f"pu{pi}"])
                                        P.op("act", lambda e, pi=pi, g=g, dc=dc, tb=tb: e.activation(out=pmT[:, 2 * g + dc, tb * 512:(tb + 1) * 512], in_=pu[pi][:], func=AF.Copy, scale=psc[:, 2 * g + dc:2 * g + dc + 1]), reads=[f"pu{pi}", "psc"], writes=["pmT"])
                            pend_mix.append(_mix)
                    while pend_mix:
                        pend_mix.pop(0)()
                    load_w(0, w_in[:, C_GP:C_GP + 1024], "wb0")
                    for dc in range(8):
                        for tb in range(4):
                            p1 = next_pu()
                            for c in range(8):
                                P.op("pe", lambda e, p1=p1, c=c, dc=dc, tb=tb: e.matmul(pu[p1][:], lhsT=wb[1][:, c, dc * 128:(dc + 1) * 128], rhs=pmT[:, c, tb * 512:(tb + 1) * 512], start=(c == 0), stop=(c == 7)), reads=WK1 + ["pmT"], writes=[f"pu{p1}"])
                            p2 = next_pu()
                            for dk in range(8):
                                P.op("pe", lambda e, p2=p2, dk=dk, dc=dc, tb=tb, own0=own0: e.matmul(pu[p2][:], lhsT=wb[0][:, dk, dc * 128:(dc + 1) * 128], rhs=hT[:, dk, own0 + tb * 512:own0 + (tb + 1) * 512], start=(dk == 0), stop=(dk == 7)), reads=WK0 + ["hT"], writes=[f"pu{p2}"])
                            si = nst[0] % 2
                            ai = nst[0] % 3
                            nst[0] += 1
                            P.op("act", lambda e, p2=p2, si=si: e.activation(out=sg[si][:], in_=pu[p2][:], func=AF.Sigmoid), reads=[f"pu{p2}"], writes=[f"sg{si}"])
                            P.op("dve", lambda e, p1=p1, si=si, ai=ai: e.tensor_tensor(out=ast[ai][:], in0=pu[p1][:], in1=sg[si][:], op=ALU.mult), reads=[f"pu{p1}", f"sg{si}"], writes=[f"ast{ai}"])
                            P.dma("sp", Abuf[hf, dc * 128:(dc + 1) * 128, tb * 512:(tb + 1) * 512], ast[ai][:], reads=[f"ast{ai}"], writes=["Abuf"])
                    load_w(1, w_in[:, C_GA:C_GA + 1024], "wb1")
                    for dc in range(8):
                        for tb in range(4):
                            p2 = next_pu()
                            for dk in range(8):
                                P.op("pe", lambda e, p2=p2, dk=dk, dc=dc, tb=tb, own0=own0: e.matmul(pu[p2][:], lhsT=wb[1][:, dk, dc * 128:(dc + 1) * 128], rhs=hT[:, dk, own0 + tb * 512:own0 + (tb + 1) * 512], start=(dk == 0), stop=(dk == 7)), reads=WK1 + ["hT"], writes=[f"pu{p2}"])
                            ai = nst[0] % 3
                            nst[0] += 1
                            P.op("act", lambda e, p2=p2, ai=ai: e.activation(out=ast[ai][:], in_=pu[p2][:], func=AF.Sigmoid), reads=[f"pu{p2}"], writes=[f"ast{ai}"])
                            P.dma("sp", Gabuf[hf, dc * 128:(dc + 1) * 128, tb * 512:(tb + 1) * 512], ast[ai][:], reads=[f"ast{ai}"], writes=["Gabuf"])
                P.flush()
            if stop_after <= 1:
                P.finish()
                return nc

            with ExitStack() as es:
                Es = es.enter_context
                Ct = Es(nc.sbuf_tensor("Ct", [128, S], F32))
                St = Es(nc.sbuf_tensor("St", [128, S], F32))
                tmpi = Es(nc.sbuf_tensor("tmpi", [128, 1024], I32))
                tmpa = Es(nc.sbuf_tensor("tmpa", [128, 1024], F32))
                tmpb = Es(nc.sbuf_tensor("tmpb", [128, 1024], F32))
                kT = [Es(nc.sbuf_tensor(f"kT{i}", [128, S], BF16)) for i in range(2)]
                Vt = [Es(nc.sbuf_tensor(f"Vt{i}", [128, NT, 130], BF16)) for i in range(2)]
                qT = [Es(nc.sbuf_tensor(f"qT{i}", [128, HQ], BF16)) for i in range(2)]
                wq = [Es(nc.sbuf_tensor(f"wq{i}", [128, 8, 128], BF16)) for i in range(2)]
                wk = [Es(nc.sbuf_tensor(f"wk{i}", [128, 8, 128], BF16)) for i in range(2)]
                wv = [Es(nc.sbuf_tensor(f"wv{i}", [128, 8, 128], BF16)) for i in range(2)]
                zb = [Es(nc.sbuf_tensor(f"zb{i}", [128, 512], BF16)) for i in range(2)]
                t1 = [Es(nc.sbuf_tensor(f"t1{i}", [128, 512], F32)) for i in range(2)]
                t2 = [Es(nc.sbuf_tensor(f"t2{i}", [128, 512], F32)) for i in range(2)]
                PT = [Es(nc.sbuf_tensor(f"PT{i}", [128, 2, 512], BF16)) for i in range(3)]
                lamt = Es(nc.sbuf_tensor("lamt", [128, 256], F32))
                lamp = Es(nc.sbuf_tensor("lamp", [128, 128], F32))
                lsc = Es(nc.sbuf_tensor("lsc", [128, 4], F32))
                gsub = Es(nc.sbuf_tensor("gsub", [128, 128], F32))
                fin = Es(nc.sbuf_tensor("fin", [128, 4, 4], F32))
                o1 = [Es(nc.sbuf_tensor(f"o1{i}", [128, 128], F32)) for i in range(4)]
                o2 = [Es(nc.sbuf_tensor(f"o2{i}", [128, 128], F32)) for i in range(4)]
                ob = [Es(nc.sbuf_tensor(f"ob{i}", [128, 128], BF16)) for i in range(4)]
                ojunk = Es(nc.sbuf_tensor("ojunk", [128, 128], F32))
                ost = [Es(nc.sbuf_tensor(f"ost{i}", [128, 512], BF16)) for i in range(2)]
                pS2 = [Es(nc.psum_tensor(f"pS2_{i}", [128, 2, 512], F32)) for i in range(2)]

                class _V:
                    def __init__(self, t, m):
                        self.t, self.m = t, m

                    def __getitem__(self, idx):
                        if idx == slice(None):
                            return self.t[:, self.m, :]
                        return self.t[:, self.m, :][idx]

                BK = [_V(pS2[0], 0), _V(pS2[0], 1), _V(pS2[1], 0), _V(pS2[1], 1)] + [Es(nc.psum_tensor(f"BK{i}", [128, 512], F32)) for i in range(4, 8)]
                B7b = BK[7][:].bitcast(BF16)
                rinv = CF("rinv")
                for q4 in range(4):
                    cs = slice(q4 * 1024, (q4 + 1) * 1024)
                    P.dma("sp", tmpi[:], pos[q4 * 1024:(q4 + 1) * 1024].partition_broadcast(128), writes=["tmpi"])
                    P.op("dve", lambda e: e.tensor_copy(out=tmpa[:], in_=tmpi[:]), reads=["tmpi"], writes=["tmpa"])
                    for which in range(2):
                        dstT = St if which == 0 else Ct
                        dkey = f"St{q4}" if which == 0 else f"Ct{q4}"
                        P.op("dve", lambda e, which=which: e.tensor_scalar(out=tmpb[:], in0=tmpa[:], scalar1=rinv[:, 0:1], scalar2=(0.0 if which == 0 else math.pi / 2), op0=ALU.mult, op1=ALU.add), reads=["tmpa", "cf"], writes=["tmpb"])
                        P.op("dve", lambda e: e.tensor_scalar(out=tmpi[:], in0=tmpb[:], scalar1=1.0 / (2 * math.pi), scalar2=None, op0=ALU.mult), reads=["tmpb"], writes=["tmpi"])
                        P.op("dve", lambda e, dstT=dstT, cs=cs: e.tensor_copy(out=dstT[:, cs], in_=tmpi[:]), reads=["tmpi"], writes=[dkey])
                        P.op("dve", lambda e, dstT=dstT, cs=cs: e.scalar_tensor_tensor(out=tmpb[:], in0=dstT[:, cs], scalar=-2 * math.pi, in1=tmpb[:], op0=ALU.mult, op1=ALU.add), reads=[dkey, "tmpb"], writes=["tmpb"])
                        P.op("dve", lambda e, dstT=dstT, cs=cs: e.tensor_scalar(out=dstT[:, cs], in0=tmpb[:], scalar1=math.pi, scalar2=-2 * math.pi, op0=ALU.is_gt, op1=ALU.mult), reads=["tmpb"], writes=[dkey])
                        P.op("dve", lambda e, dstT=dstT, cs=cs: e.tensor_tensor(out=tmpb[:], in0=tmpb[:], in1=dstT[:, cs], op=ALU.add), reads=["tmpb", dkey], writes=["tmpb"])
                        P.op("act", lambda e, dstT=dstT, cs=cs: e.activation(out=dstT[:, cs], in_=tmpb[:], func=AF.Sin), reads=["tmpb"], writes=[dkey])
                        if which == 0:
                            P.op("dve", lambda e, cs=cs: e.tensor_scalar(out=St[:, cs], in0=St[:, cs], scalar1=rinv[:, 1:2], scalar2=None, op0=ALU.mult), reads=[f"St{q4}", "cf"], writes=[f"St{q4}"])
                P.dma("sp", lamt[:], lamv.rearrange("a b -> (a b)").partition_broadcast(128), writes=["lamt"])
                P.dma("sp", gsub[:], g_subln.partition_broadcast(128), writes=["gsub"])
                P.op("dve", lambda e: e.tensor_tensor(out=lamp[:, 0:64], in0=lamt[:, 0:64], in1=lamt[:, 64:128], op=ALU.mult), reads=["lamt"], writes=["lamp"])
                P.op("dve", lambda e: e.tensor_tensor(out=lamp[:, 64:128], in0=lamt[:, 128:192], in1=lamt[:, 192:256], op=ALU.mult), reads=["lamt"], writes=["lamp"])
                P.op("dve", lambda e: e.reduce_sum(out=lsc[:, 0:1], in_=lamp[:, 0:64], axis=AX.X), reads=["lamp"], writes=["lsc"])
                P.op("dve", lambda e: e.reduce_sum(out=lsc[:, 1:2], in_=lamp[:, 64:128], axis=AX.X), reads=["lamp"], writes=["lsc"])
                P.op("act", lambda e: e.activation(out=lsc[:, 0:2], in_=lsc[:, 0:2], func=AF.Exp), reads=["lsc"], writes=["lsc"])
                P.op("dve", lambda e: e.tensor_tensor(out=lsc[:, 2:3], in0=lsc[:, 1:2], in1=lsc[:, 0:1], op=ALU.subtract), reads=["lsc"], writes=["lsc"])
                P.op("dve", lambda e: e.tensor_scalar(out=lsc[:, 2:3], in0=lsc[:, 2:3], scalar1=-LAM_INIT, scalar2=None, op0=ALU.add), reads=["lsc"], writes=["lsc"])
                P.op("dve", lambda e: e.tensor_scalar(out=gsub[:], in0=gsub[:], scalar1=(1.0 - LAM_INIT), scalar2=None, op0=ALU.mult), reads=["gsub"], writes=["gsub"])
                for i in range(2):
                    P.op("dve", lambda e, i=i: e.memset(Vt[i][:, :, 128:130], 1.0), writes=[f"Vt{i}"])

                deferred = []
                ZB = [0, 4]
                ZPB = [1, 5]

                def proj_rot(wt, wkeys, dstt, dkey, hcol0, nblk, tcol0):
                    def mm(tb):
                        zb_ = ZB[tb % 2]
                        for dk in range(8):
                            P.op("pe", lambda e, dk=dk, tb=tb, zb_=zb_: e.matmul(BK[zb_][:], lhsT=wt[:, dk, :], rhs=hT[:, dk, hcol0 + tb * 512:hcol0 + (tb + 1) * 512], start=(dk == 0), stop=(dk == 7)), reads=wkeys + ["hT"], writes=[f"BK{zb_}"])

                    mm(0)
                    for tb in range(nblk):
                        zi = tb % 2
                        zb_ = ZB[tb % 2]
                        zp_ = ZPB[tb % 2]
                        tq = (tcol0 + tb * 512) // 1024
                        if tb + 1 < nblk:
                            mm(tb + 1)
                        P.op("act", lambda e, zi=zi, zb_=zb_: e.activation(out=zb[zi][:], in_=BK[zb_][:], func=AF.Copy), reads=[f"BK{zb_}"], writes=[f"zb{zi}"])
                        P.op("dve", lambda e, zi=zi, tb=tb: e.tensor_tensor(out=t1[zi][:], in0=zb[zi][:], in1=Ct[:, tcol0 + tb * 512:tcol0 + (tb + 1) * 512], op=ALU.mult), reads=[f"zb{zi}", f"Ct{tq}"], writes=[f"t1{zi}"])
                        P.op("pe", lambda e, zi=zi, zp_=zp_: e.matmul(BK[zp_][:], lhsT=permb, rhs=zb[zi][:], start=True, stop=True), reads=[f"zb{zi}", "cb"], writes=[f"BK{zp_}"])
                        P.op("dve", lambda e, zi=zi, tb=tb, zp_=zp_: e.tensor_tensor(out=t2[zi][:], in0=BK[zp_][:], in1=St[:, tcol0 + tb * 512:tcol0 + (tb + 1) * 512], op=ALU.mult), reads=[f"BK{zp_}", f"St{tq}"], writes=[f"t2{zi}"])
                        P.op("dve", lambda e, zi=zi, tb=tb: e.tensor_tensor(out=dstt[:, tb * 512:(tb + 1) * 512], in0=t1[zi][:], in1=t2[zi][:], op=ALU.add), reads=[f"t1{zi}", f"t2{zi}"], writes=[dkey])
                        if deferred:
                            deferred.pop(0)[1]()

                import os
                nq = [0]
                no = [0]
                for h in range(int(os.environ.get("KDBG_NH", NH))):
                    hb = h % 2
                    for (wt, nm_, c0) in ((wq, "wq", C_Q), (wk, "wk", C_K), (wv, "wv", C_V)):
                        P.dma("pool", wt[hb][:], w_in[:, c0 + h * 128:c0 + (h + 1) * 128].rearrange("(c p) n -> p c n", p=128), writes=[f"{nm_}{hb}"])
                    proj_rot(wk[hb], [f"wk{hb}"], kT[hb], f"kT{hb}", 0, 8, 0)
                    for tg in range(NT // 4):
                        bi = 2 + tg % 2
                        for j in range(4):
                            ti = tg * 4 + j
                            for dk in range(8):
                                P.op("pe", lambda e, dk=dk, ti=ti, j=j, bi=bi, hb=hb: e.matmul(BK[bi][:, j * 128:(j + 1) * 128], lhsT=hT[:, dk, ti * 128:(ti + 1) * 128], rhs=wv[hb][:, dk, :], start=(dk == 0), stop=(dk == 7)), reads=[f"wv{hb}", "hT"], writes=[f"BK{bi}"])
                        P.op("dve", lambda e, tg=tg, bi=bi, hb=hb: e.tensor_copy(out=Vt[hb][:, tg * 4:(tg + 1) * 4, 0:128], in_=BK[bi][:].rearrange("p (j n) -> p j n", j=4)), reads=[f"BK{bi}"], writes=[f"Vt{hb}"])
                    for hf in range(2):
                        own0 = hf * HQ
                        qi = nq[0] % 2
                        nq[0] += 1
                        proj_rot(wq[hb], [f"wq{hb}"], qT[qi], f"qT{qi}", own0, 4, own0)
                        for qb in range(4):
                            def acc(m, qs):
                                a = m * 4 + qs
                                return BK[4 + a // 3][:, (a % 3) * 160:(a % 3) * 160 + 130]

                            PO = ["BK4", "BK5", "BK6"]

                            def qk(kt, hb=hb, qi=qi, qb=qb):
                                si = kt % 2
                                for m in range(2):
                                    bi = si * 2 + m
                                    P.op("pe", lambda e, m=m, kt=kt, bi=bi: e.matmul(BK[bi][:], lhsT=kT[hb][m * 64:(m + 1) * 64, kt * 128:(kt + 1) * 128], rhs=qT[qi][m * 64:(m + 1) * 64, qb * 512:(qb + 1) * 512], start=True, stop=True), reads=[f"kT{hb}", f"qT{qi}"], writes=[f"BK{bi}"])

                            qk(0)
                            for kt in range(NT):
                                si = kt % 2
                                pi = kt % 3
                                if kt + 1 < NT:
                                    qk(kt + 1)
                                for m in range(2):
                                    bi = si * 2 + m
                                    P.op("act", lambda e, bi=bi, pi=pi, m=m: e.activation(out=PT[pi][:, m, :], in_=BK[bi][:], func=AF.Exp, scale=SCALE), reads=[f"BK{bi}"], writes=[f"PT{pi}_{m}"])
                                while deferred and deferred[0][0] <= kt:
                                    deferred.pop(0)[1]()
                                for m in range(2):
                                    for qs in range(4):
                                        P.op("pe", lambda e, m=m, qs=qs, kt=kt, pi=pi, hb=hb: e.matmul(acc(m, qs), lhsT=PT[pi][:, m, qs * 128:(qs + 1) * 128], rhs=Vt[hb][:, kt, 0:130], start=(kt == 0 and (m * 4 + qs) % 3 == 0), stop=(kt == NT - 1), skip_group_check=True), reads=[f"PT{pi}_{m}", f"Vt{hb}"], writes=PO)
                            oi = no[0] % 2
                            no[0] += 1
                            for qs in range(4):
                                fk = f"fin{qs}"
                                P.op("dve", lambda e, qs=qs: e.reciprocal(out=fin[:, qs, 0:1], in_=acc(0, qs)[:, 128:129]), reads=PO, writes=[fk])
                                P.op("dve", lambda e, qs=qs: e.reciprocal(out=fin[:, qs, 1:2], in_=acc(1, qs)[:, 128:129]), reads=PO, writes=[fk])
                                P.op("dve", lambda e, qs=qs: e.tensor_tensor(out=fin[:, qs, 1:2], in0=fin[:, qs, 1:2], in1=lsc[:, 2:3], op=ALU.mult), reads=[fk, "lsc"], writes=[fk])
                                P.op("dve", lambda e, qs=qs: e.tensor_scalar(out=o1[qs][:], in0=acc(0, qs)[:, 0:128], scalar1=fin[:, qs, 0:1], scalar2=None, op0=ALU.mult), reads=PO + [fk], writes=[f"o1{qs}"])
                                P.op("dve", lambda e, qs=qs: e.scalar_tensor_tensor(out=o2[qs][:], in0=acc(1, qs)[:, 0:128], scalar=fin[:, qs, 1:2], in1=o1[qs][:], op0=ALU.mult, op1=ALU.add), reads=PO + [fk, f"o1{qs}"], writes=[f"o2{qs}"])
                            FB = [f"finb{q}" for q in range(4)]

                            def fin_b():
                                for qs in range(4):
                                    P.op("act", lambda e, qs=qs: e.activation(out=ojunk[:], in_=o2[qs][:], func=AF.Square, accum_out=fin[:, qs, 2:3]), reads=[f"o2{qs}"], writes=["ojunk", f"finb{qs}"])

                            def fin_c():
                                P.op("dve", lambda e: e.tensor_scalar(out=fin[:, :, 2], in0=fin[:, :, 2], scalar1=1.0 / 128, scalar2=EPS, op0=ALU.mult, op1=ALU.add), reads=FB, writes=FB)

                            def fin_d():
                                P.op("act", lambda e: e.activation(out=fin[:, :, 2], in_=fin[:, :, 2], func=AF.Ln), reads=FB, writes=FB)
                                P.op("act", lambda e: e.activation(out=fin[:, :, 2], in_=fin[:, :, 2], func=AF.Exp, scale=-0.5), reads=FB, writes=FB)

                            def fin_e():
                                for qs in range(4):
                                    P.op("dve", lambda e, qs=qs: e.scalar_tensor_tensor(out=ob[qs][:], in0=o2[qs][:], scalar=fin[:, qs, 2:3], in1=gsub[:], op0=ALU.mult, op1=ALU.mult), reads=[f"o2{qs}", f"finb{qs}", "gsub"], writes=[f"ob{qs}"])

                            def fin_f(oi=oi, hf=hf, h=h, qb=qb):
                                for qs in range(4):
                                    P.op("pe", lambda e, qs=qs: e.transpose(out=B7b[:, qs * 128:(qs + 1) * 128], in_=ob[qs][:], identity=idb), reads=[f"ob{qs}", "cb"], writes=["BK7"])
                                P.op("dve", lambda e, oi=oi: e.tensor_copy(out=ost[oi][:], in_=B7b[:, 0:512]), reads=["BK7"], writes=[f"ost{oi}"])
                                P.dma("sp", oTbuf[hf, h * 128:(h + 1) * 128, qb * 512:(qb + 1) * 512], ost[oi][:], reads=[f"ost{oi}"], writes=["oTbuf"])

                            deferred[:] = [(2, fin_b), (3, fin_c), (4, fin_d), (6, fin_e), (8, fin_f)]
                while deferred:
                    deferred.pop(0)[1]()
                P.flush()
        if stop_after <= 2:
            P.finish()
            return nc

        with ExitStack() as es:
            Es = es.enter_context
            wao = Es(nc.sbuf_tensor("wao", [128, 8, 1024], BF16))
            wo = Es(nc.sbuf_tensor("wo", [128, 8, 1024], BF16))
            oTb = [Es(nc.sbuf_tensor(f"oTb{i}", [128, 8, 512], BF16)) for i in range(2)]
            Gab = [Es(nc.sbuf_tensor(f"Gab{i}", [128, 8, 512], BF16)) for i in range(2)]
            Ab = [Es(nc.sbuf_tensor(f"Ab{i}", [128, 8, 512], BF16)) for i in range(2)]
            mT = [Es(nc.sbuf_tensor(f"mT{i}", [128, 8, 512], BF16)) for i in range(2)]
            tmpm = [Es(nc.sbuf_tensor(f"tmpm{i}", [128, 512], F32)) for i in range(2)]
            xt2 = [Es(nc.sbuf_tensor(f"xt2{i}", [128, D], F32)) for i in range(2)]
            x1 = [Es(nc.sbuf_tensor(f"x1{i}", [128, D], F32)) for i in range(2)]
            h2 = [Es(nc.sbuf_tensor(f"h2{i}", [128, D], BF16)) for i in range(2)]
            x1T = [Es(nc.sbuf_tensor(f"x1T{i}", [128, 8, 128], F32)) for i in range(2)]
            junk2 = Es(nc.sbuf_tensor("junk2", [128, D], BF16))
            wr = Es(nc.sbuf_tensor("wr", [128, 8, NE], F32))
            gf = Es(nc.sbuf_tensor("gf", [128, 8], F32))
            sm = [Es(nc.sbuf_tensor(f"sm{i}", [128, 8], F32)) for i in range(2)]
            lg = [Es(nc.sbuf_tensor(f"lg{i}", [128, NE], F32)) for i in range(2)]
            ex = [Es(nc.sbuf_tensor(f"ex{i}", [128, NE], F32)) for i in range(2)]
            aff = [Es(nc.sbuf_tensor(f"aff{i}", [128, NE], F32)) for i in range(2)]
            affTs = [Es(nc.sbuf_tensor(f"affTs{i}", [NE, 128], F32)) for i in range(2)]
            BK = [Es(nc.psum_tensor(f"CK{i}", [128, 512], F32)) for i in range(8)]

            def loadw(dst, key, src_ap):
                v = src_ap.rearrange("(c p) n -> p c n", p=128)
                for q in range(4):
                    P.dma("pool", dst[:, :, q * 256:(q + 1) * 256], v[:, :, q * 256:(q + 1) * 256], writes=[f"{key}_{q}"])
                return [f"{key}_{q}" for q in range(4)]

            WAO = loadw(wao, "wao", w_attn_out)
            WO = loadw(wo, "wo", w_out)
            P.dma("sp", wr[:], w_router.rearrange("(c p) e -> p c e", p=128), writes=["wr"])
            P.dma("sp", gf[:], g_ffn.rearrange("(c p) -> p c", p=128), writes=["gf"], allow_slow_non_contiguous=True)
            for c in range(8):
                P.op("dve", lambda e, c=c: e.tensor_scalar(out=wr[:, c, :], in0=wr[:, c, :], scalar1=gf[:, c:c + 1], scalar2=None, op0=ALU.mult), reads=["wr", "gf"], writes=["wr"])
            def Y(blk):
                hf, tb = blk // 4, blk % 4
                bi = blk % 2
                oTv = oTbuf[hf].rearrange("(c p) t -> p c t", p=128)
                Gav = Gabuf[hf].rearrange("(c p) t -> p c t", p=128)
                Av = Abuf[hf].rearrange("(c p) t -> p c t", p=128)
                cs = slice(tb * 512, (tb + 1) * 512)
                P.dma("sp", oTb[bi][:], oTv[:, :, cs], reads=["oTbuf"], writes=[f"oTb{bi}"])
                P.dma("sp", Gab[bi][:], Gav[:, :, cs], reads=["Gabuf"], writes=[f"Gab{bi}"])
                P.dma("sp", Ab[bi][:], Av[:, :, cs], reads=["Abuf"], writes=[f"Ab{bi}"])
                for dc in range(8):
                    pb = dc % 2
                    for c in range(8):
                        P.op("pe", lambda e, c=c, dc=dc, pb=pb, bi=bi: e.matmul(BK[pb][:], lhsT=wao[:, c, dc * 128:(dc + 1) * 128], rhs=oTb[bi][:, c, :], start=(c == 0), stop=(c == 7)), reads=WAO + [f"oTb{bi}"], writes=[f"CK{pb}"])
                    P.op("dve", lambda e, dc=dc, pb=pb, bi=bi: e.tensor_tensor(out=tmpm[pb][:], in0=BK[pb][:], in1=Gab[bi][:, dc, :], op=ALU.mult), reads=[f"CK{pb}", f"Gab{bi}"], writes=[f"tmpm{pb}"])
                    P.op("dve", lambda e, dc=dc, pb=pb, bi=bi: e.tensor_tensor(out=mT[bi][:, dc, :], in0=tmpm[pb][:], in1=Ab[bi][:, dc, :], op=ALU.add), reads=[f"tmpm{pb}", f"Ab{bi}"], writes=[f"mT{bi}"])

            def Dt(t):
                blk, j = t // 4, t % 4
                bi, xi = blk % 2, t % 2
                rows = slice(t * 128, (t + 1) * 128)
                sk = f"sm{xi}"
                P.dma("sp", xt2[xi][:], x[rows, :], writes=[f"xt2{xi}"])
                for half in range(2):
                    pb = 2 + half
                    for dk in range(8):
                        P.op("pe", lambda e, dk=dk, half=half, pb=pb, bi=bi, j=j: e.matmul(BK[pb][:], lhsT=mT[bi][:, dk, j * 128:(j + 1) * 128], rhs=wo[:, dk, half * 512:(half + 1) * 512], start=(dk == 0), stop=(dk == 7)), reads=WO + [f"mT{bi}"], writes=[f"CK{pb}"])
                    P.op("dve", lambda e, half=half, pb=pb, xi=xi: e.tensor_tensor(out=x1[xi][:, half * 512:(half + 1) * 512], in0=BK[pb][:], in1=xt2[xi][:, half * 512:(half + 1) * 512], op=ALU.add), reads=[f"CK{pb}", f"xt2{xi}"], writes=[f"x1{xi}"])
                P.dma("pool", x1buf[rows, :], x1[xi][:], reads=[f"x1{xi}"], writes=["x1buf"])
                P.op("act", lambda e, xi=xi: e.activation(out=junk2[:], in_=x1[xi][:], func=AF.Square, accum_out=sm[xi][:, 0:1]), reads=[f"x1{xi}"], writes=["junk2", sk])
                P.op("dve", lambda e, xi=xi: e.tensor_scalar(out=sm[xi][:, 0:1], in0=sm[xi][:, 0:1], scalar1=1.0 / D, scalar2=EPS, op0=ALU.mult, op1=ALU.add), reads=[sk], writes=[sk])
                P.op("act", lambda e, xi=xi: e.activation(out=sm[xi][:, 0:1], in_=sm[xi][:, 0:1], func=AF.Ln), reads=[sk], writes=[sk])
                P.op("act", lambda e, xi=xi: e.activation(out=sm[xi][:, 0:1], in_=sm[xi][:, 0:1], func=AF.Exp, scale=-0.5), reads=[sk], writes=[sk])
                P.op("act", lambda e, xi=xi: e.activation(out=h2[xi][:], in_=x1[xi][:], func=AF.Copy, scale=sm[xi][:, 0:1]), reads=[f"x1{xi}", sk], writes=[f"h2{xi}"])
                P.dma("pool", h2buf[rows, :], h2[xi][:], reads=[f"h2{xi}"], writes=["h2buf"])

            def Tt(t):
                xi = t % 2
                for dk in range(8):
                    pb = 4 + dk // 4
                    P.op("pe", lambda e, dk=dk, pb=pb, xi=xi: e.transpose(out=BK[pb][:, (dk % 4) * 128:(dk % 4 + 1) * 128], in_=x1[xi][:, dk * 128:(dk + 1) * 128], identity=idf), reads=[f"x1{xi}", "cf"], writes=[f"CK{pb}"])
                for pb in (4, 5):
                    P.op("act", lambda e, pb=pb, xi=xi: e.activation(out=x1T[xi][:, (pb - 4) * 4:(pb - 3) * 4, :], in_=BK[pb][:].rearrange("p (j n) -> p j n", j=4), func=AF.Copy), reads=[f"CK{pb}"], writes=[f"x1T{xi}"])

            def Lt(t):
                xi = t % 2
                rows = slice(t * 128, (t + 1) * 128)
                sk = f"sm{xi}"
                for dk in range(8):
                    P.op("pe", lambda e, dk=dk, xi=xi: e.matmul(BK[6][:, 0:NE], lhsT=x1T[xi][:, dk, :], rhs=wr[:, dk, :], start=(dk == 0), stop=(dk == 7)), reads=[f"x1T{xi}", "wr"], writes=["CK6"])
                P.op("dve", lambda e, xi=xi: e.tensor_scalar(out=lg[xi][:], in0=BK[6][:, 0:NE], scalar1=sm[xi][:, 0:1], scalar2=None, op0=ALU.mult), reads=["CK6", sk], writes=[f"lg{xi}"])
                P.op("dve", lambda e, xi=xi: e.reduce_max(out=sm[xi][:, 1:2], in_=lg[xi][:], axis=AX.X), reads=[f"lg{xi}"], writes=[sk + "b"])
                P.op("dve", lambda e, xi=xi: e.tensor_scalar(out=sm[xi][:, 1:2], in0=sm[xi][:, 1:2], scalar1=-1.0, scalar2=None, op0=ALU.mult), reads=[sk + "b"], writes=[sk + "b"])
                P.op("act", lambda e, xi=xi: e.activation(out=ex[xi][:], in_=lg[xi][:], func=AF.Exp, bias=sm[xi][:, 1:2], scale=1.0, accum_out=sm[xi][:, 2:3]), reads=[f"lg{xi}", sk + "b"], writes=[f"ex{xi}", sk + "c"])
                P.op("dve", lambda e, xi=xi: e.reciprocal(out=sm[xi][:, 2:3], in_=sm[xi][:, 2:3]), reads=[sk + "c"], writes=[sk + "c"])
                P.op("dve", lambda e, xi=xi: e.tensor_scalar(out=aff[xi][:], in0=ex[xi][:], scalar1=sm[xi][:, 2:3], scalar2=None, op0=ALU.mult), reads=[f"ex{xi}", sk + "c"], writes=[f"aff{xi}"])
                P.dma("pool", affbuf[rows, :], aff[xi][:], reads=[f"aff{xi}"], writes=["affbuf"])

            def At(t):
                xi = t % 2
                rows = slice(t * 128, (t + 1) * 128)
                P.op("pe", lambda e, xi=xi: e.transpose(out=BK[7][0:NE, 0:128], in_=aff[xi][:], identity=idf), reads=[f"aff{xi}", "cf"], writes=["CK7"])
                P.op("act", lambda e, xi=xi: e.activation(out=affTs[xi][:], in_=BK[7][0:NE, 0:128], func=AF.Copy), reads=["CK7"], writes=[f"affTs{xi}"])
                P.dma("pool", affTbuf[:, rows], affTs[xi][:], reads=[f"affTs{xi}"], writes=["affTbuf"])

            Y(0)
            Dt(0)
            for t in range(NT):
                if t % 4 == 1 and t // 4 + 1 < 8:
                    Y(t // 4 + 1)
                if t + 1 < NT:
                    Dt(t + 1)
                Tt(t)
                Lt(t)
                if t >= 1:
                    At(t - 1)
            At(NT - 1)
            P.flush()
        if stop_after <= 3:
            P.finish()
            return nc

        with ExitStack() as es:
            Es = es.enter_context
            affP = Es(nc.sbuf_tensor("affP", [128, 512], F32))
            afft = Es(nc.sbuf_tensor("afft", [128, NT, NE], F32))
            bjunk = Es(nc.sbuf_tensor("bjunk", [128, 512], BF16))
            bs = Es(nc.sbuf_tensor("bs", [128, 8], F32))
            taub = Es(nc.sbuf_tensor("taub", [128, NE], F32))
            mask = Es(nc.sbuf_tensor("mask", [128, NT, NE], F32))
            pa = Es(nc.sbuf_tensor("pa", [128, NT, NE], F32))
            pb_ = Es(nc.sbuf_tensor("pb_", [128, NT, NE], F32))
            slot = Es(nc.sbuf_tensor("slot", [128, NT, NE], F32))
            ra = Es(nc.sbuf_tensor("ra", [128, NT, NE], F32))
            rbf = Es(nc.sbuf_tensor("rbf", [128, NT, NE], BF16))
            R = Es(nc.sbuf_tensor("R", [128, NT, NE, 6], BF16))
            gf3 = Es(nc.sbuf_tensor("gf3", [128, 8], F32))
            oh = [Es(nc.sbuf_tensor(f"oh{i}", [128, 512], BF16)) for i in range(4)]
            idxf = [Es(nc.sbuf_tensor(f"idxf{i}", [128, 4], F32)) for i in range(2)]
            idxi = [Es(nc.sbuf_tensor(f"idxi{i}", [128, 4], I32)) for i in range(2)]
            valt = [Es(nc.sbuf_tensor(f"valt{i}", [128, 4], F32)) for i in range(2)]
            pIs = Es(nc.sbuf_tensor("pIs", [128, 32], F32))
            xs = [Es(nc.sbuf_tensor(f"xs{i}", [128, 4, D], BF16)) for i in range(2)]
            xsT = [Es(nc.sbuf_tensor(f"xsT{i}", [128, 8, 512], BF16)) for i in range(2)]
            hTm = [Es(nc.sbuf_tensor(f"hTm{i}", [128, NFC, 512], BF16)) for i in range(2)]
            yt = Es(nc.sbuf_tensor("yt", [128, 4, D], F32))
            sgt = [Es(nc.sbuf_tensor(f"sgt{i}", [128, 512], F32)) for i in range(2)]
            NGU = 3
            ND = 4
            wg = [Es(nc.sbuf_tensor(f"wg{i}", [128, 8, 256], BF16)) for i in range(NGU)]
            wu = [Es(nc.sbuf_tensor(f"wu{i}", [128, 8, 256], BF16)) for i in range(NGU)]
            wd = [Es(nc.sbuf_tensor(f"wd{i}", [128, 2, 512], BF16)) for i in range(ND)]
            MK = [Es(nc.psum_tensor(f"MK{i}", [128, 512], F32)) for i in range(8)]
            MK6b = MK[6][:].bitcast(BF16)
            MK7b = MK[7][:].bitcast(BF16)
            gsum = CF("gsum")
            P.dma("sp", affP[:], affTbuf.rearrange("e (j c) -> (e j) c", j=8), reads=["affTbuf"], writes=["affP"])
            P.dma("sp", afft[:], affbuf.rearrange("(n p) e -> p n e", p=128), reads=["affbuf"], writes=["afft"])
            P.dma("sp", gf3[:], g_ffn.rearrange("(c p) -> p c", p=128), writes=["gf3"], allow_slow_non_contiguous=True)
            NIT = 28
            P.op("dve", lambda e: e.memset(bs[:, 0:1], 0.0), writes=["bs_lo"])
            P.op("dve", lambda e: e.memset(bs[:, 1:2], 0.5), writes=["bs_mid"])
            for it in range(NIT):
                wdt = 2.0 ** -(it + 1)
                P.op("dve", lambda e: e.tensor_scalar(out=bjunk[:], in0=affP[:], scalar1=bs[:, 1:2], scalar2=0.0, op0=ALU.is_ge, op1=ALU.add, accum_out=bs[:, 2:3]), reads=["affP", "bs_mid"], writes=["bjunk", "bs_cnt"])
                P.op("pe", lambda e: e.matmul(MK[4][:, 0:1], lhsT=gsum, rhs=bs[:, 2:3], start=True, stop=True), reads=["cf", "bs_cnt"], writes=["MK4"])
                P.op("dve", lambda e: e.tensor_scalar(out=bs[:, 3:4], in0=MK[4][:, 0:1], scalar1=CAP - 0.5, scalar2=None, op0=ALU.is_ge), reads=["MK4"], writes=["bs_ge"])
                P.op("dve", lambda e, wdt=wdt: e.scalar_tensor_tensor(out=bs[:, 0:1], in0=bs[:, 3:4], scalar=wdt, in1=bs[:, 0:1], op0=ALU.mult, op1=ALU.add), reads=["bs_ge", "bs_lo"], writes=["bs_lo"])
                P.op("dve", lambda e, wdt=wdt: e.tensor_scalar(out=bs[:, 1:2], in0=bs[:, 0:1], scalar1=wdt / 2, scalar2=None, op0=ALU.add), reads=["bs_lo"], writes=["bs_mid"])
            P.dma("sp", taubuf.rearrange("(p o) -> p o", o=1), bs[:, 0:1], reads=["bs_lo"], writes=["taubuf"])
            P.dma("sp", taub[:], taubuf.rearrange("(e j) -> e j", j=8)[:, 0:1].rearrange("e o -> (e o)").partition_broadcast(128), reads=["taubuf"], writes=["taub"], allow_slow_non_contiguous=True)
            for n in range(NT):
                P.op("dve", lambda e, n=n: e.tensor_tensor(out=mask[:, n, :], in0=afft[:, n, :], in1=taub[:], op=ALU.is_ge), reads=["afft", "taub"], writes=["mask"])
            m2 = mask[:].rearrange("p n e -> p (n e)")
            P.op("pe", lambda e: e.matmul(MK[4][:], lhsT=CF("utri"), rhs=m2, start=True, stop=True), reads=["cf", "mask"], writes=["MK4"])
            P.op("pe", lambda e: e.matmul(MK[5][:], lhsT=CF("ones"), rhs=m2, start=True, stop=True), reads=["cf", "mask"], writes=["MK5"])
            P.op("dve", lambda e: e.tensor_copy(out=pa[:].rearrange("p n e -> p (n e)"), in_=MK[5][:]), reads=["MK5"], writes=["pa"])
            src, sk_, dst, dk_ = pa, "pa", pb_, "pb_"
            for sft in (1, 2, 4, 8, 16):
                P.op("dve", lambda e, src=src, dst=dst, sft=sft: e.tensor_tensor(out=dst[:, sft:, :], in0=src[:, sft:, :], in1=src[:, :NT - sft, :], op=ALU.add), reads=[sk_], writes=[dk_])
                P.op("dve", lambda e, src=src, dst=dst, sft=sft: e.tensor_copy(out=dst[:, :sft, :], in_=src[:, :sft, :]), reads=[sk_], writes=[dk_])
                src, sk_, dst, dk_ = dst, dk_, src, sk_
            P.op("dve", lambda e, src=src: e.tensor_tensor(out=slot[:].rearrange("p n e -> p (n e)"), in0=MK[4][:], in1=src[:].rearrange("p n e -> p (n e)"), op=ALU.add), reads=["MK4", sk_], writes=["slot"])
            P.op("dve", lambda e: e.tensor_tensor(out=slot[:].rearrange("p n e -> p (n e)"), in0=slot[:].rearrange("p n e -> p (n e)"), in1=MK[5][:], op=ALU.subtract), reads=["slot", "MK5"], writes=["slot"])
            P.op("dve", lambda e: e.tensor_tensor(out=slot[:], in0=slot[:], in1=mask[:], op=ALU.mult), reads=["slot", "mask"], writes=["slot"])
            P.op("dve", lambda e: e.tensor_scalar(out=slot[:], in0=slot[:], scalar1=-1.0, scalar2=None, op0=ALU.add), reads=["slot"], writes=["slot"])
            thl = CF("thl").rearrange("p (n t) -> p n t", t=2)
            P.op("dve", lambda e: e.memset(R[:], 0.0), writes=["R"])
            for ee in range(NE):
                P.op("dve", lambda e, ee=ee: e.tensor_copy(out=R[:, :, ee, 0:2], in_=thl), reads=["cf"], writes=["R"])
            P.op("dve", lambda e: e.tensor_copy(out=rbf[:], in_=afft[:]), reads=["afft"], writes=["rbf"])
            P.op("dve", lambda e: e.tensor_copy(out=R[:, :, :, 2], in_=rbf[:]), reads=["rbf"], writes=["R"])
            P.op("dve", lambda e: e.tensor_tensor(out=ra[:], in0=afft[:], in1=rbf[:], op=ALU.subtract), reads=["afft", "rbf"], writes=["ra"])
            P.op("dve", lambda e: e.tensor_copy(out=rbf[:], in_=ra[:]), reads=["ra"], writes=["rbf"])
            P.op("dve", lambda e: e.tensor_copy(out=R[:, :, :, 3], in_=rbf[:]), reads=["rbf"], writes=["R"])
            P.op("dve", lambda e: e.tensor_tensor(out=ra[:], in0=ra[:], in1=rbf[:], op=ALU.subtract), reads=["ra", "rbf"], writes=["ra"])
            P.op("dve", lambda e: e.tensor_copy(out=R[:, :, :, 4], in_=ra[:]), reads=["ra"], writes=["R"])

            gu_chunks = [(ee, pr) for ee in range(NE) for pr in range(NFC // 2)]
            d_chunks = [(ee, half, pr) for ee in range(NE) for half in range(2) for pr in range(NFC // 2)]
            gu_next = [0]
            d_next = [0]

            def emit_gu_load():
                i = gu_next[0]
                if i >= len(gu_chunks):
                    return
                gu_next[0] += 1
                ee, pr = gu_chunks[i]
                sl = i % NGU
                P.dma("pool", wg[sl][:], w_gate[ee][:, pr * 256:(pr + 1) * 256].rearrange("(c p) f -> p c f", p=128), writes=[f"wg{sl}"])
                P.dma("pool", wu[sl][:], w_up[ee][:, pr * 256:(pr + 1) * 256].rearrange("(c p) f -> p c f", p=128), writes=[f"wu{sl}"])

            def emit_d_load():
                i = d_next[0]
                if i >= len(d_chunks):
                    return
                d_next[0] += 1
                ee, half, pr = d_chunks[i]
                sl = i % ND
                P.dma("pool", wd[sl][:], w_down[ee][pr * 256:(pr + 1) * 256, half * 512:(half + 1) * 512].rearrange("(c p) d -> p c d", p=128), writes=[f"wd{sl}"])

            for _ in range(NGU):
                emit_gu_load()
            for _ in range(ND):
                emit_d_load()

            def sel_onehot(ee, n):
                oi = n % 4
                P.op("dve", lambda e, n=n, oi=oi, ee=ee: e.tensor_scalar(out=oh[oi][:], in0=CF("iota"), scalar1=slot[:, n, ee:ee + 1], scalar2=None, op0=ALU.is_equal), reads=["cf", "slot"], writes=[f"oh{oi}"])

            def sel_mm(ee, n):
                oi = n % 4
                for sb in range(4):
                    P.op("pe", lambda e, n=n, oi=oi, sb=sb, ee=ee: e.matmul(MK[4][:, sb * 8:sb * 8 + 6], lhsT=oh[oi][:, sb * 128:(sb + 1) * 128], rhs=R[:, n, ee, :], start=(n == 0 and sb == 0), stop=(n == NT - 1), skip_group_check=True), reads=[f"oh{oi}", "R"], writes=["MK4"])

            def prep_idx(ee):
                b = ee % 2
                P.op("dve", lambda e: e.tensor_copy(out=pIs[:], in_=MK[4][:, 0:32]), reads=["MK4"], writes=["pIs"])
                pI = pIs[:].rearrange("p (s k) -> p s k", k=8)
                P.op("dve", lambda e, b=b: e.scalar_tensor_tensor(out=idxf[b][:], in0=pI[:, :, 0], scalar=64.0, in1=pI[:, :, 1], op0=ALU.mult, op1=ALU.add), reads=["pIs"], writes=[f"idxf{b}"])
                P.op("dve", lambda e, b=b: e.tensor_copy(out=idxi[b][:], in_=idxf[b][:]), reads=[f"idxf{b}"], writes=[f"idxi{b}"])
                P.op("dve", lambda e, b=b: e.tensor_tensor(out=valt[b][:], in0=pI[:, :, 2], in1=pI[:, :, 3], op=ALU.add), reads=["pIs"], writes=[f"valt{b}"])
                P.op("dve", lambda e, b=b: e.tensor_tensor(out=valt[b][:], in0=valt[b][:], in1=pI[:, :, 4], op=ALU.add), reads=["pIs", f"valt{b}"], writes=[f"valt{b}"])
                for sb in range(4):
                    P.op("pool", lambda e, sb=sb, b=b: e.indirect_dma_start(out=xs[b][:, sb, :], out_offset=None, in_=h2buf, in_offset=bass.IndirectOffsetOnAxis(ap=idxi[b][:, sb:sb + 1], axis=0)), reads=[f"idxi{b}", "h2buf"], writes=[f"xs{b}_{sb}"], dma=True)

            def prep_tr(ee):
                b = ee % 2
                for dk in range(8):
                    bank, bv = (6, MK6b) if dk % 2 == 0 else (7, MK7b)
                    for sb in range(4):
                        P.op("pe", lambda e, dk=dk, sb=sb, b=b, bv=bv: e.transpose(out=bv[:, sb * 128:(sb + 1) * 128], in_=xs[b][:, sb, dk * 128:(dk + 1) * 128], identity=idb), reads=[f"xs{b}_{sb}", "cb"], writes=[f"MK{bank}"])
                    P.op("act", lambda e, dk=dk, b=b, bv=bv: e.activation(out=xsT[b][:, dk, :], in_=bv[:, 0:512], func=AF.Copy, scale=gf3[:, dk:dk + 1]), reads=[f"MK{bank}", "gf3"], writes=[f"xsT{b}"])

            gu_i = [0]
            d_i = [0]

            def gateup(ee, nxt):
                b = ee % 2
                npair = NFC // 2
                groups = [list(range(3 * pr, min(3 * pr + 3, NT))) for pr in range(npair)]
                groups[-1] = list(range(3 * (npair - 1), NT))
                for pr in range(npair):
                    i = gu_i[0]
                    gu_i[0] += 1
                    sl = i % NGU
                    for fl in range(2):
                        fc = pr * 2 + fl
                        gb = fc % 2
                        ub = 2 + fc % 2
                        for dk in range(8):
                            P.op("pe", lambda e, dk=dk, fl=fl, sl=sl, gb=gb, b=b: e.matmul(MK[gb][:], lhsT=wg[sl][:, dk, fl * 128:(fl + 1) * 128], rhs=xsT[b][:, dk, :], start=(dk == 0), stop=(dk == 7)), reads=[f"wg{sl}", f"xsT{b}"], writes=[f"MK{gb}"])
                        for dk in range(8):
                            P.op("pe", lambda e, dk=dk, fl=fl, sl=sl, ub=ub, b=b: e.matmul(MK[ub][:], lhsT=wu[sl][:, dk, fl * 128:(fl + 1) * 128], rhs=xsT[b][:, dk, :], start=(dk == 0), stop=(dk == 7)), reads=[f"wu{sl}", f"xsT{b}"], writes=[f"MK{ub}"])
                        P.op("act", lambda e, gb=gb: e.activation(out=sgt[gb][:], in_=MK[gb][:], func=AF.Silu), reads=[f"MK{gb}"], writes=[f"sgt{gb}"])
                        P.op("dve", lambda e, gb=gb, ub=ub, fc=fc, b=b: e.tensor_tensor(out=hTm[b][:, fc, :], in0=MK[ub][:], in1=sgt[gb][:], op=ALU.mult), reads=[f"MK{ub}", f"sgt{gb}"], writes=[f"hTm{b}"])
                    emit_gu_load()
                    if nxt is not None:
                        if pr >= 1:
                            for n in groups[pr - 1]:
                                sel_mm(nxt, n)
                        for n in groups[pr]:
                            sel_onehot(nxt, n)
                if nxt is not None:
                    for n in groups[npair - 1]:
                        sel_mm(nxt, n)

            def down(ee, nxt):
                b = ee % 2
                for half in range(2):
                    for pr in range(NFC // 2):
                        i = d_i[0]
                        d_i[0] += 1
                        sl = i % ND
                        for fl in range(2):
                            fc = pr * 2 + fl
                            for sb in range(4):
                                P.op("pe", lambda e, fc=fc, fl=fl, sb=sb, sl=sl, b=b: e.matmul(MK[sb][:], lhsT=hTm[b][:, fc, sb * 128:(sb + 1) * 128], rhs=wd[sl][:, fl, :], start=(fc == 0), stop=(fc == NFC - 1)), reads=[f"hTm{b}", f"wd{sl}"], writes=[f"MK{sb}"])
                        emit_d_load()
                    for sb in range(4):
                        if sb < 2:
                            P.op("act", lambda e, sb=sb, half=half, b=b: e.activation(out=yt[:, sb, half * 512:(half + 1) * 512], in_=MK[sb][:], func=AF.Copy, scale=valt[b][:, sb:sb + 1]), reads=[f"MK{sb}", f"valt{b}"], writes=[f"yt{sb}"])
                        else:
                            P.op("dve", lambda e, sb=sb, half=half, b=b: e.tensor_scalar(out=yt[:, sb, half * 512:(half + 1) * 512], in0=MK[sb][:], scalar1=valt[b][:, sb:sb + 1], scalar2=None, op0=ALU.mult), reads=[f"MK{sb}", f"valt{b}"], writes=[f"yt{sb}"])
                    if half == 0 and nxt is not None:
                        prep_tr(nxt)
                for sb in range(4):
                    P.op("pool", lambda e, sb=sb, b=b: e.indirect_dma_start(out=x1buf, out_offset=bass.IndirectOffsetOnAxis(ap=idxi[b][:, sb:sb + 1], axis=0), in_=yt[:, sb, :], in_offset=None, compute_op=ALU.add), reads=[f"idxi{b}", f"yt{sb}"] + [f"x1p{1 - b}_{q}" for q in range(4)], writes=[f"x1p{b}_{sb}"], dma=True)

            import os
            NEX = int(os.environ.get("KDBG_NEX", NE))
            for n in range(NT):
                sel_onehot(0, n)
                sel_mm(0, n)
            prep_idx(0)
            prep_tr(0)
            for ee in range(NEX):
                nxt = ee + 1 if ee + 1 < NEX else None
                gateup(ee, nxt)
                if nxt is not None:
                    prep_idx(nxt)
                down(ee, nxt)
            P.flush()
        if stop_after <= 4:
            P.finish()
            return nc

        with ExitStack() as es:
            Es = es.enter_context
            gfin = Es(nc.sbuf_tensor("gfin", [128, D], F32))
            xf = [Es(nc.sbuf_tensor(f"xf{i}", [128, D], F32)) for i in range(3)]
            of = [Es(nc.sbuf_tensor(f"of{i}", [128, D], F32)) for i in range(3)]
            fj = Es(nc.sbuf_tensor("fj", [128, D], BF16))
            fs = [Es(nc.sbuf_tensor(f"fs{i}", [128, 1], F32)) for i in range(3)]
            P.dma("sp", gfin[:], g_final.partition_broadcast(128), writes=["gfin"])
            def s4_a(i):
                a = i % 3
                rows = slice(i * 128, (i + 1) * 128)
                P.dma("sp", xf[a][:], x1buf[rows, :], reads=["x1buf"] + [f"x1p{pp}_{q}" for pp in range(2) for q in range(4)], writes=[f"xf{a}"])
                P.op("act", lambda e, a=a: e.activation(out=fj[:], in_=xf[a][:], func=AF.Square, accum_out=fs[a][:]), reads=[f"xf{a}"], writes=["fj", f"fs{a}"])
                P.op("dve", lambda e, a=a: e.tensor_scalar(out=fs[a][:], in0=fs[a][:], scalar1=1.0 / D, scalar2=EPS, op0=ALU.mult, op1=ALU.add), reads=[f"fs{a}"], writes=[f"fs{a}"])

            def s4_b(i):
                a = i % 3
                rows = slice(i * 128, (i + 1) * 128)
                P.op("act", lambda e, a=a: e.activation(out=fs[a][:], in_=fs[a][:], func=AF.Ln), reads=[f"fs{a}"], writes=[f"fs{a}"])
                P.op("act", lambda e, a=a: e.activation(out=fs[a][:], in_=fs[a][:], func=AF.Exp, scale=-0.5), reads=[f"fs{a}"], writes=[f"fs{a}"])
                P.op("dve", lambda e, a=a: e.scalar_tensor_tensor(out=of[a][:], in0=xf[a][:], scalar=fs[a][:], in1=gfin[:], op0=ALU.mult, op1=ALU.mult), reads=[f"xf{a}", f"fs{a}", "gfin"], writes=[f"of{a}"])
                P.dma("pool", out[rows, :], of[a][:], reads=[f"of{a}"], writes=[f"out{i}"])

            s4_a(0)
            for i in range(NT):
                if i + 1 < NT:
                    s4_a(i + 1)
                s4_b(i)
            P.flush()
        P.finish()
    return nc


def make_in_maps(inputs, ncores=4, moe=False):
    cfa_np, CO, cb_np = _consts()
    x = np.asarray(inputs["x"], np.float32)
    posn = np.asarray(inputs["positions"]).astype(np.int32)
    lamv = np.concatenate([np.asarray(inputs[k], np.float32).reshape(1, 64) for k in ("lam_q1", "lam_k1", "lam_q2", "lam_k2")], axis=0)
    fixt = np.stack([_fix_tables(0), _fix_tables(1)], axis=0).astype(np.float32)
    maps = []
    for c in range(ncores):
        b = c
        xb = np.ascontiguousarray(x[b])
        xh = np.zeros((2, 256, D), np.float32)
        xh[0, 128:256] = xb[HQ:HQ + 128]
        xh[1, 0:128] = xb[HQ - 128:HQ]
        m = {
            "x": xb, "xhalo": xh, "pos": np.ascontiguousarray(posn[b]),
            "g_mix": np.asarray(inputs["g_mix"], np.float32).reshape(D),
            "w_in": np.asarray(inputs["w_in"], np.float32)[0],
            "w_pool_mix": np.asarray(inputs["w_pool_mix"], np.float32)[0],
            "pool_scale": np.asarray(inputs["pool_scale"], np.float32).reshape(D),
            "w_pool_out": np.asarray(inputs["w_pool_out"], np.float32)[0],
            "lamv": lamv,
            "g_subln": np.asarray(inputs["g_subln"], np.float32).reshape(128),
            "w_attn_out": np.asarray(inputs["w_attn_out"], np.float32)[0],
            "w_out": np.asarray(inputs["w_out"], np.float32)[0],
            "g_ffn": np.asarray(inputs["g_ffn"], np.float32).reshape(D),
            "w_router": np.asarray(inputs["w_router"], np.float32)[0],
            "w_gate": np.asarray(inputs["w_gate"], np.float32)[0],
            "w_up": np.asarray(inputs["w_up"], np.float32)[0],
            "w_down": np.asarray(inputs["w_down"], np.float32)[0],
            "g_final": np.asarray(inputs["g_final"], np.float32).reshape(D),
            "cfa": cfa_np, "cba": cb_np, "fixt": fixt,
        }
        if not moe:
            for k in ("w_gate", "w_up", "w_down"):
                m.pop(k)
        maps.append(m)
    return maps


def kernel(**inputs):
    nc = build()
    maps = make_in_maps(inputs, 4, moe=True)
    res = run_bass_kernel_spmd(nc, maps, core_ids=list(range(4)))
    return np.stack([np.asarray(r["out"], np.float32) for r in res.results], axis=0)
```

```python
import math
from contextlib import ExitStack

import numpy as np
import ml_dtypes
import concourse.bass as bass
import concourse.mybir as mybir
from concourse.bass_utils import run_bass_kernel_spmd

F32 = mybir.dt.float32
BF16 = mybir.dt.bfloat16
I32 = mybir.dt.int32
AF = mybir.ActivationFunctionType
ALU = mybir.AluOpType
AX = mybir.AxisListType

S = 4096
D = 1024
NT = 32
HQ = 2048
NH = 8
NE = 16
CAP = 512
DFF = 2816
NFC = 22
EPS = 1e-6
SCALE = 0.125
LAM_INIT = 0.8 - 0.6 * math.exp(0.0)
ROPE_THETA = 500000.0
WINS = (2, 4, 8, 16)
C_Q, C_K, C_V, C_GP, C_GA = 1024, 2048, 3072, 4096, 5120

ENGS = ("pe", "act", "dve", "pool", "sp")
DMAQ = ("sp", "act", "pool")
NDS = 14


class _Rec:
    __slots__ = ("eng", "fn", "deps", "is_dma", "sig", "sigval", "dsem", "dval", "prevwait", "stage")

    def __init__(self, eng, fn, is_dma, stage):
        self.eng = eng
        self.fn = fn
        self.deps = set()
        self.is_dma = is_dma
        self.sig = False
        self.sigval = 0
        self.dsem = None
        self.dval = 0
        self.prevwait = None
        self.stage = stage


class Prog:
    def __init__(self, nc, sems, dsems):
        self.nc = nc
        self.sems = sems
        self.dsems = dsems
        self.pending = {e: [] for e in ENGS}
        self.bufs = {}
        self.stage = 0
        self.sigc = {e: 0 for e in ENGS}
        self.dcount = {e: [0] * NDS for e in DMAQ}
        self.di = {e: 0 for e in DMAQ}
        self.waited = {e: {} for e in ENGS}

    def op(self, eng, fn, reads=(), writes=(), dma=False):
        r = _Rec(eng, fn, dma, self.stage)
        for k in reads:
            b = self.bufs.setdefault(k, [None, []])
            if b[0] is not None:
                r.deps.add(b[0])
        for k in writes:
            b = self.bufs.setdefault(k, [None, []])
            if b[0] is not None:
                d = b[0]
                if dma or d.is_dma or d.eng != eng:
                    r.deps.add(d)
            for d in b[1]:
                if dma or d.is_dma or d.eng != eng:
                    r.deps.add(d)
        for k in reads:
            self.bufs[k][1].append(r)
        for k in writes:
            self.bufs[k] = [r, []]
        r.deps.discard(r)
        self.pending[eng].append(r)
        return r

    def dma(self, eng, out, in_, reads=(), writes=(), **kw):
        return self.op(eng, lambda e: e.dma_start(out=out, in_=in_, **kw), reads, writes, dma=True)

    def flush(self):
        nc = self.nc
        st = self.stage
        allr = [r for e in ENGS for r in self.pending[e]]
        for r in allr:
            r.deps = {d for d in r.deps if d.stage == st}
            for d in r.deps:
                if not d.is_dma:
                    d.sig = True
        for e in ENGS:
            lastc = None
            for r in self.pending[e]:
                if not r.is_dma:
                    lastc = r
            if lastc is not None:
                lastc.sig = True
        bar = []
        if st > 0:
            for e in ENGS:
                if self.sigc[e] > 0:
                    bar.append((self.sems[e], self.sigc[e]))
            for e in DMAQ:
                for s in range(NDS):
                    if self.dcount[e][s] > 0:
                        bar.append((self.dsems[e][s], 16 * self.dcount[e][s]))
        for e in ENGS:
            for r in self.pending[e]:
                if r.is_dma:
                    s = self.di[e] % NDS
                    self.di[e] += 1
                    r.dsem = self.dsems[e][s]
                    if self.dcount[e][s] > 0:
                        r.prevwait = (r.dsem, 16 * self.dcount[e][s])
                    self.dcount[e][s] += 1
                    r.dval = 16 * self.dcount[e][s]
                elif r.sig:
                    self.sigc[e] += 1
                    r.sigval = self.sigc[e]
        pend = self.pending
        sems = self.sems
        waited_all = self.waited

        def run(e, engobj):
            waited = waited_all[e]

            def w(s, v):
                if waited.get(id(s), 0) >= v:
                    return
                waited[id(s)] = v
                engobj.wait_ge(s, v)

            for (s, v) in bar:
                w(s, v)
            for r in pend[e]:
                if r.prevwait is not None:
                    w(*r.prevwait)
                for d in r.deps:
                    if d.is_dma:
                        w(d.dsem, d.dval)
                    else:
                        w(sems[d.eng], d.sigval)
                ins = r.fn(engobj)
                if r.is_dma:
                    ins.then_inc(r.dsem, 16)
                elif r.sig:
                    ins.then_inc(sems[e], 1)

        with nc.Block() as block:
            @block.sync
            def _(eng):
                run("sp", eng)

            @block.scalar
            def _(eng):
                run("act", eng)

            @block.vector
            def _(eng):
                run("dve", eng)

            @block.gpsimd
            def _(eng):
                run("pool", eng)

            @block.tensor
            def _(eng):
                run("pe", eng)

        self.pending = {e: [] for e in ENGS}
        self.stage += 1

    def finish(self):
        nc = self.nc
        fin = []
        for e in DMAQ:
            for s in range(NDS):
                if self.dcount[e][s] > 0:
                    fin.append((self.dsems[e][s], 16 * self.dcount[e][s]))
        for e in ENGS:
            if self.sigc[e] > 0:
                fin.append((self.sems[e], self.sigc[e]))
        with nc.Block() as block:
            @block.sync
            def _(eng):
                for (s, v) in fin:
                    eng.wait_ge(s, v)


def _consts():
    cf = {}
    p = np.arange(128)
    cf["idf"] = np.eye(128, dtype=np.float32)
    cf["ones"] = np.ones((128, 128), np.float32)
    cf["utri"] = (p[:, None] <= p[None, :]).astype(np.float32)
    cf["gsum"] = ((p[:, None] // 8) == (p[None, :] // 8)).astype(np.float32)
    cf["iota"] = np.tile(np.arange(512, dtype=np.float32)[None, :], (128, 1))
    half = 8
    inv = ROPE_THETA ** (-np.arange(half, dtype=np.float32) * 2.0 / 16)
    rinv = np.zeros((128, 4), np.float32)
    for base in (0, 64):
        for j in range(8):
            rinv[base + j, 0] = inv[j]
            rinv[base + 8 + j, 0] = inv[j]
            rinv[base + j, 1] = -1.0
            rinv[base + 8 + j, 1] = 1.0
    cf["rinv"] = rinv
    T = np.arange(S).reshape(NT, 128).T
    thl = np.zeros((128, NT, 2), np.float32)
    thl[:, :, 0] = T // 64
    thl[:, :, 1] = T % 64
    cf["thl"] = thl.reshape(128, NT * 2)
    names = ["idf", "ones", "utri", "gsum", "iota", "rinv", "thl"]
    offs = {}
    o = 0
    for n in names:
        offs[n] = (o, cf[n].shape[1])
        o += cf[n].shape[1]
    cfa = np.concatenate([cf[n] for n in names], axis=1).astype(np.float32)
    perm = np.zeros((128, 128), np.float32)
    for base in (0, 64):
        for j in range(8):
            perm[base + j + 8, base + j] = 1.0
            perm[base + j, base + j + 8] = 1.0
    cb = np.concatenate([np.eye(128, dtype=np.float32), perm, np.ones((128, 128), np.float32)], axis=1).astype(ml_dtypes.bfloat16)
    return cfa, offs, cb


def _fix_tables(hf):
    fs = np.ones((128, 4, 8), np.float32)
    fe = np.ones((128, 4, 8), np.float32)
    for g, w in enumerate(WINS):
        for j in range(8):
            if hf == 0:
                t = j
                lo = max(t - w // 2, 0)
                hi = min(t + w // 2, S)
                fs[:, g, j] = w / (hi - lo)
            if hf == 1:
                t = S - 8 + j
                lo = max(t - w // 2, 0)
                hi = min(t + w // 2, S)
                fe[:, g, j] = w / (hi - lo)
    return np.concatenate([fs.reshape(128, 32), fe.reshape(128, 32)], axis=1)


def build(stop_after=99, dbg=False):
    nc = bass.Bass("TRN2", target_bir_lowering=False)
    cfa_np, CO, cb_np = _consts()
    NCF = cfa_np.shape[1]

    def din(name, shape, dt=F32):
        return nc.dram_tensor(name, list(shape), dt, kind="ExternalInput").ap()

    x = din("x", [S, D])
    xhalo = din("xhalo", [2, 256, D])
    pos = din("pos", [S], I32)
    g_mix = din("g_mix", [D])
    w_in = din("w_in", [D, 6144])
    w_pool_mix = din("w_pool_mix", [4, 256, 256])
    pool_scale = din("pool_scale", [D])
    w_pool_out = din("w_pool_out", [D, D])
    lamv = din("lamv", [4, 64])
    g_subln = din("g_subln", [128])
    w_attn_out = din("w_attn_out", [D, D])
    w_out = din("w_out", [D, D])
    g_ffn = din("g_ffn", [D])
    w_router = din("w_router", [D, NE])
    if stop_after >= 4:
        w_gate = din("w_gate", [NE, D, DFF])
        w_up = din("w_up", [NE, D, DFF])
        w_down = din("w_down", [NE, DFF, D])
    g_final = din("g_final", [D])
    cfa = din("cfa", [128, NCF])
    cba = din("cba", [128, 384], BF16)
    fixt = din("fixt", [2, 128, 64])
    out = nc.dram_tensor("out", [S, D], F32, kind="ExternalOutput").ap()

    def dscr(name, shape, dt):
        kind = "ExternalOutput" if dbg else "Internal"
        return nc.dram_tensor(name, list(shape), dt, kind=kind).ap()

    Abuf = dscr("Abuf", [2, D, HQ], BF16)
    Gabuf = dscr("Gabuf", [2, D, HQ], BF16)
    oTbuf = dscr("oTbuf", [2, D, HQ], BF16)
    x1buf = dscr("x1buf", [S, D], F32)
    h2buf = dscr("h2buf", [S, D], BF16)
    affbuf = dscr("affbuf", [S, NE], F32)
    affTbuf = dscr("affTbuf", [NE, S], F32)
    taubuf = dscr("taubuf", [128], F32)
    if dbg:
        hTdbg = dscr("hTdbg", [128, 8, S], BF16)
        wbdbg = dscr("wbdbg", [128, 8, 1024], BF16)

    with ExitStack() as es0:
        E0 = es0.enter_context
        sems = {e: E0(nc.semaphore("s_" + e)) for e in ENGS}
        dsems = {e: [E0(nc.semaphore(f"d_{e}{i}")) for i in range(NDS)] for e in DMAQ}
        P = Prog(nc, sems, dsems)
        cf = E0(nc.sbuf_tensor("cf", [128, NCF], F32))
        cb = E0(nc.sbuf_tensor("cb", [128, 384], BF16))
        P.dma("sp", cf[:], cfa, writes=["cf"])
        P.dma("sp", cb[:], cba, writes=["cb"])

        def CF(n):
            o, w = CO[n]
            return cf[:, o:o + w]

        idb = cb[:, 0:128]
        permb = cb[:, 128:256]
        onesb = cb[:, 256:384]
        idf = CF("idf")

        def rstd_from_ss(ss, tag, scale_n):
            P.op("dve", lambda e: e.tensor_scalar(out=ss, in0=ss, scalar1=1.0 / scale_n, scalar2=EPS, op0=ALU.mult, op1=ALU.add), reads=[tag], writes=[tag])
            P.op("act", lambda e: e.activation(out=ss, in_=ss, func=AF.Ln), reads=[tag], writes=[tag])
            P.op("act", lambda e: e.activation(out=ss, in_=ss, func=AF.Exp, scale=-0.5), reads=[tag], writes=[tag])

        with ExitStack() as esA:
            EA = esA.enter_context
            hT = EA(nc.sbuf_tensor("hT", [128, 8, S], BF16))
            hTh = EA(nc.sbuf_tensor("hTh", [128, 8, 512], BF16))
            with ExitStack() as es:
                Es = es.enter_context
                xt = [Es(nc.sbuf_tensor(f"xt{i}", [128, D], F32)) for i in range(6)]
                xn = [Es(nc.sbuf_tensor(f"xn{i}", [128, D], BF16)) for i in range(4)]
                junk = Es(nc.sbuf_tensor("junk0", [128, D], BF16))
                ssq = [Es(nc.sbuf_tensor(f"ss{i}", [128, 1], F32)) for i in range(6)]
                gm = Es(nc.sbuf_tensor("gm", [128, 8], F32))
                gexp = Es(nc.sbuf_tensor("gexp", [128, 8, 128], F32))
                pT = [Es(nc.psum_tensor(f"pT{i}", [128, 8, 128], BF16)) for i in range(4)]
                P.dma("sp", gm[:], g_mix.rearrange("(c p) -> p c", p=128), writes=["gm"], allow_slow_non_contiguous=True)
                for c in range(8):
                    P.op("dve", lambda e, c=c: e.tensor_scalar(out=gexp[:, c, :], in0=CF("ones"), scalar1=gm[:, c:c + 1], scalar2=None, op0=ALU.mult), reads=["cf", "gm"], writes=["gexp"])
                def s0_info(i):
                    if i < NT:
                        return x[i * 128:(i + 1) * 128, :], hT[:, :, i * 128:(i + 1) * 128], "hT"
                    j = i - NT
                    return xhalo[j // 2, (j % 2) * 128:(j % 2 + 1) * 128, :], hTh[:, :, j * 128:(j + 1) * 128], "hTh"

                def s0_a(i):
                    a = i % 6
                    src, dst, dkey = s0_info(i)
                    P.dma("sp", xt[a][:], src, writes=[f"xt{a}"])
                    P.op("act", lambda e, a=a: e.activation(out=junk[:], in_=xt[a][:], func=AF.Square, accum_out=ssq[a][:]), reads=[f"xt{a}"], writes=["junk0", f"ss{a}"])
                    P.op("dve", lambda e, a=a: e.tensor_scalar(out=ssq[a][:], in0=ssq[a][:], scalar1=1.0 / D, scalar2=EPS, op0=ALU.mult, op1=ALU.add), reads=[f"ss{a}"], writes=[f"ss{a}"])

                def s0_b(i):
                    a, b2, c2 = i % 6, i % 4, i % 4
                    src, dst, dkey = s0_info(i)
                    P.op("act", lambda e, a=a: e.activation(out=ssq[a][:], in_=ssq[a][:], func=AF.Ln), reads=[f"ss{a}"], writes=[f"ss{a}"])
                    P.op("act", lambda e, a=a: e.activation(out=ssq[a][:], in_=ssq[a][:], func=AF.Exp, scale=-0.5), reads=[f"ss{a}"], writes=[f"ss{a}"])
                    P.op("act", lambda e, a=a, b2=b2: e.activation(out=xn[b2][:], in_=xt[a][:], func=AF.Copy, scale=ssq[a][:]), reads=[f"xt{a}", f"ss{a}"], writes=[f"xn{b2}"])
                    for c in range(8):
                        P.op("pe", lambda e, c=c, b2=b2, c2=c2: e.transpose(out=pT[c2][:, c, :], in_=xn[b2][:, c * 128:(c + 1) * 128], identity=idb), reads=[f"xn{b2}", "cb"], writes=[f"pT{c2}"])
                    P.op("dve", lambda e, c2=c2, dst=dst: e.tensor_tensor(out=dst, in0=pT[c2][:], in1=gexp[:], op=ALU.mult), reads=[f"pT{c2}", "gexp"], writes=[dkey])

                s0_a(0)
                s0_a(1)
                for i in range(NT + 4):
                    if i + 2 < NT + 4:
                        s0_a(i + 2)
                    s0_b(i)
                if dbg:
                    for c in range(8):
                        P.dma("sp", hTdbg[:, c, :], hT[:, c, :], reads=["hT"])
                P.flush()
            if stop_after <= 0:
                P.finish()
                return nc

            with ExitStack() as es:
                Es = es.enter_context
                wb = [Es(nc.sbuf_tensor(f"wb{i}", [128, 8, 1024], BF16)) for i in range(2)]
                wmix = Es(nc.sbuf_tensor("wmix", [128, 8, 256], BF16))
                psc = Es(nc.sbuf_tensor("psc", [128, 8], F32))
                fx = Es(nc.sbuf_tensor("fx", [128, 64], F32))
                u = [Es(nc.sbuf_tensor(f"u{i}", [128, 2304], F32)) for i in range(2)]
                wa = [Es(nc.sbuf_tensor(f"wa{i}", [128, 2304], F32)) for i in range(2)]
                pooled = [Es(nc.sbuf_tensor(f"pooled{i}", [128, 2, HQ], BF16)) for i in range(2)]
                pmT = Es(nc.sbuf_tensor("pmT", [128, 8, HQ], BF16))
                sg = [Es(nc.sbuf_tensor(f"sg{i}", [128, 512], F32)) for i in range(2)]
                ast = [Es(nc.sbuf_tensor(f"ast{i}", [128, 512], BF16)) for i in range(3)]
                pu = [Es(nc.psum_tensor(f"pu{i}", [128, 512], F32)) for i in range(4)]
                P.dma("sp", psc[:], pool_scale.rearrange("(c p) -> p c", p=128), writes=["psc"], allow_slow_non_contiguous=True)
                for g in range(4):
                    P.dma("pool", wmix[:, 2 * g:2 * g + 2, :], w_pool_mix[g].rearrange("(c p) d -> p c d", p=128), writes=[f"wmix{g}"])

                def load_w(i, src_ap, key):
                    v = src_ap.rearrange("(c p) n -> p c n", p=128)
                    for q in range(4):
                        P.dma("pool", wb[i][:, :, q * 256:(q + 1) * 256], v[:, :, q * 256:(q + 1) * 256], writes=[f"{key}_{q}"])

                WK0 = [f"wb0_{q}" for q in range(4)]
                WK1 = [f"wb1_{q}" for q in range(4)]
                npu = [0]

                def next_pu():
                    i = npu[0] % 4
                    npu[0] += 1
                    return i

                nst = [0]
                for hf in range(2):
                    own0 = hf * HQ
                    P.dma("sp", fx[:], fixt[hf], writes=["fx"])
                    load_w(0, w_in[:, 0:1024], "wb0")
                    if dbg and hf == 0:
                        for c in range(8):
                            P.dma("sp", wbdbg[:, c, :], wb[0][:, c, :], reads=WK0)
                    load_w(1, w_pool_out, "wb1")
                    blocks = [(0, 128, lambda dk, hf=hf: hTh[:, dk, (2 * hf) * 128:(2 * hf + 1) * 128], "hTh")]
                    for tb in range(4):
                        blocks.append((128 + tb * 512, 512, lambda dk, tb=tb, own0=own0: hT[:, dk, own0 + tb * 512:own0 + (tb + 1) * 512], "hT"))
                    blocks.append((128 + HQ, 128, lambda dk, hf=hf: hTh[:, dk, (2 * hf + 1) * 128:(2 * hf + 2) * 128], "hTh"))
                    pend_mix = []
                    for cc in range(8):
                        g = cc // 2
                        w = WINS[g]
                        ub = u[cc % 2]
                        uk = f"u{cc % 2}"
                        for (c0, n, rf, rk) in blocks:
                            pi = next_pu()
                            for dk in range(8):
                                P.op("pe", lambda e, pi=pi, dk=dk, n=n, rf=rf, cc=cc: e.matmul(pu[pi][:, 0:n], lhsT=wb[0][:, dk, cc * 128:(cc + 1) * 128], rhs=rf(dk), start=(dk == 0), stop=(dk == 7)), reads=WK0 + [rk], writes=[f"pu{pi}"])
                            P.op("act", lambda e, pi=pi, c0=c0, n=n, ub=ub: e.activation(out=ub[:, c0:c0 + n], in_=pu[pi][:, 0:n], func=AF.Copy), reads=[f"pu{pi}"], writes=[uk])
                        if pend_mix:
                            pend_mix.pop(0)()
                        cur, ck = ub, uk
                        lvl = [(1, 2304, 1, 0), (2, 2303, 1, -1), (4, 2301, 2, -2), (8, 2297, 4, -4)]
                        eng_alt = ["dve", "dve"]
                        for li in range(g + 1):
                            lo, hi, sp_, sm = lvl[li]
                            dstt = wa[li % 2]
                            dk_ = f"wa{li % 2}"
                            if li == 0:
                                P.op("dve", lambda e, dstt=dstt, cur=cur, lo=lo, hi=hi: e.tensor_tensor(out=dstt[:, lo:hi], in0=cur[:, lo - 1:hi - 1], in1=cur[:, lo:hi], op=ALU.add), reads=[ck], writes=[dk_])
                            else:
                                P.op(eng_alt[li % 2], lambda e, dstt=dstt, cur=cur, lo=lo, hi=hi, sp_=sp_: e.tensor_tensor(out=dstt[:, lo:hi], in0=cur[:, lo - sp_:hi - sp_], in1=cur[:, lo + sp_:hi + sp_], op=ALU.add), reads=[ck], writes=[dk_])
                            cur, ck = dstt, dk_
                        P.op("dve", lambda e, cur=cur, g=g: e.tensor_tensor(out=cur[:, 128:136], in0=cur[:, 128:136], in1=fx[:, g * 8:(g + 1) * 8], op=ALU.mult), reads=[ck, "fx"], writes=[ck])
                        P.op("dve", lambda e, cur=cur, g=g: e.tensor_tensor(out=cur[:, 128 + HQ - 8:128 + HQ], in0=cur[:, 128 + HQ - 8:128 + HQ], in1=fx[:, 32 + g * 8:32 + (g + 1) * 8], op=ALU.mult), reads=[ck, "fx"], writes=[ck])
                        pk = f"pooled{g % 2}"
                        P.op("dve", lambda e, cur=cur, ub=ub, g=g, cc=cc, w=w: e.scalar_tensor_tensor(out=pooled[g % 2][:, cc % 2, :], in0=cur[:, 128:128 + HQ], scalar=1.0 / w, in1=ub[:, 128:128 + HQ], op0=ALU.mult, op1=ALU.subtract), reads=[ck, uk], writes=[pk])
                        if cc % 2 == 1:
                            def _mix(g=g, pk=pk):
                                for dc in range(2):
                                    for tb in range(4):
                                        pi = next_pu()
                                        for c in range(2):
                                            P.op("pe", lambda e, pi=pi, c=c, g=g, dc=dc, tb=tb: e.matmul(pu[pi][:], lhsT=wmix[:, 2 * g + c, dc * 128:(dc + 1) * 128], rhs=pooled[g % 2][:, c, tb * 512:(tb + 1) * 512], start=(c == 0), stop=(c == 1)), reads=[f"wmix{g}", pk], writes=[f"pu{pi}"])
                                        P.op("act", lambda e, pi=pi, g=g, dc=dc, tb=tb: e.activation(out=pmT[:, 2 * g + dc, tb * 512:(tb + 1) * 512], in_=pu[pi][:], func=AF.Copy, scale=psc[:, 2 * g + dc:2 * g + dc + 1]), reads=[f"pu{pi}", "psc"], writes=["pmT"])
                            pend_mix.append(_mix)
                    while pend_mix:
                        pend_mix.pop(0)()
                    load_w(0, w_in[:, C_GP:C_GP + 1024], "wb0")
                    for dc in range(8):
                        for tb in range(4):
                            p1 = next_pu()
                            for c in range(8):
                                P.op("pe", lambda e, p1=p1, c=c, dc=dc, tb=tb: e.matmul(pu[p1][:], lhsT=wb[1][:, c, dc * 128:(dc + 1) * 128], rhs=pmT[:, c, tb * 512:(tb + 1) * 512], start=(c == 0), stop=(c == 7)), reads=WK1 + ["pmT"], writes=[f"pu{p1}"])
                            p2 = next_pu()
                            for dk in range(8):
                                P.op("pe", lambda e, p2=p2, dk=dk, dc=dc, tb=tb, own0=own0: e.matmul(pu[p2][:], lhsT=wb[0][:, dk, dc * 128:(dc + 1) * 128], rhs=hT[:, dk, own0 + tb * 512:own0 + (tb + 1) * 512], start=(dk == 0), stop=(dk == 7)), reads=WK0 + ["hT"], writes=[f"pu{p2}"])
                            si = nst[0] % 2
                            ai = nst[0] % 3
                            nst[0] += 1
                            P.op("act", lambda e, p2=p2, si=si: e.activation(out=sg[si][:], in_=pu[p2][:], func=AF.Sigmoid), reads=[f"pu{p2}"], writes=[f"sg{si}"])
                            P.op("dve", lambda e, p1=p1, si=si, ai=ai: e.tensor_tensor(out=ast[ai][:], in0=pu[p1][:], in1=sg[si][:], op=ALU.mult), reads=[f"pu{p1}", f"sg{si}"], writes=[f"ast{ai}"])
                            P.dma("sp", Abuf[hf, dc * 128:(dc + 1) * 128, tb * 512:(tb + 1) * 512], ast[ai][:], reads=[f"ast{ai}"], writes=["Abuf"])
                    load_w(1, w_in[:, C_GA:C_GA + 1024], "wb1")
                    for dc in range(8):
                        for tb in range(4):
                            p2 = next_pu()
                            for dk in range(8):
                                P.op("pe", lambda e, p2=p2, dk=dk, dc=dc, tb=tb, own0=own0: e.matmul(pu[p2][:], lhsT=wb[1][:, dk, dc * 128:(dc + 1) * 128], rhs=hT[:, dk, own0 + tb * 512:own0 + (tb + 1) * 512], start=(dk == 0), stop=(dk == 7)), reads=WK1 + ["hT"], writes=[f"pu{p2}"])
                            ai = nst[0] % 3
                            nst[0] += 1
                            P.op("act", lambda e, p2=p2, ai=ai: e.activation(out=ast[ai][:], in_=pu[p2][:], func=AF.Sigmoid), reads=[f"pu{p2}"], writes=[f"ast{ai}"])
                            P.dma("sp", Gabuf[hf, dc * 128:(dc + 1) * 128, tb * 512:(tb + 1) * 512], ast[ai][:], reads=[f"ast{ai}"], writes=["Gabuf"])
                P.flush()
            if stop_after <= 1:
                P.finish()
                return nc

            with ExitStack() as es:
                Es = es.enter_context
                Ct = Es(nc.sbuf_tensor("Ct", [128, S], F32))
                St = Es(nc.sbuf_tensor("St", [128, S], F32))
                tmpi = Es(nc.sbuf_tensor("tmpi", [128, 1024], I32))
                tmpa = Es(nc.sbuf_tensor("tmpa", [128, 1024], F32))
                tmpb = Es(nc.sbuf_tensor("tmpb", [128, 1024], F32))
                kT = [Es(nc.sbuf_tensor(f"kT{i}", [128, S], BF16)) for i in range(2)]
                Vt = [Es(nc.sbuf_tensor(f"Vt{i}", [128, NT, 130], BF16)) for i in range(2)]
                qT = [Es(nc.sbuf_tensor(f"qT{i}", [128, HQ], BF16)) for i in range(2)]
                wq = [Es(nc.sbuf_tensor(f"wq{i}", [128, 8, 128], BF16)) for i in range(2)]
                wk = [Es(nc.sbuf_tensor(f"wk{i}", [128, 8, 128], BF16)) for i in range(2)]
                wv = [Es(nc.sbuf_tensor(f"wv{i}", [128, 8, 128], BF16)) for i in range(2)]
                zb = [Es(nc.sbuf_tensor(f"zb{i}", [128, 512], BF16)) for i in range(2)]
                t1 = [Es(nc.sbuf_tensor(f"t1{i}", [128, 512], F32)) for i in range(2)]
                t2 = [Es(nc.sbuf_tensor(f"t2{i}", [128, 512], F32)) for i in range(2)]
                PT = [Es(nc.sbuf_tensor(f"PT{i}", [128, 2, 512], BF16)) for i in range(3)]
                lamt = Es(nc.sbuf_tensor("lamt", [128, 256], F32))
                lamp = Es(nc.sbuf_tensor("lamp", [128, 128], F32))
                lsc = Es(nc.sbuf_tensor("lsc", [128, 4], F32))
                gsub = Es(nc.sbuf_tensor("gsub", [128, 128], F32))
                fin = Es(nc.sbuf_tensor("fin", [128, 4, 4], F32))
                o1 = [Es(nc.sbuf_tensor(f"o1{i}", [128, 128], F32)) for i in range(4)]
                o2 = [Es(nc.sbuf_tensor(f"o2{i}", [128, 128], F32)) for i in range(4)]
                ob = [Es(nc.sbuf_tensor(f"ob{i}", [128, 128], BF16)) for i in range(4)]
                ojunk = Es(nc.sbuf_tensor("ojunk", [128, 128], F32))
                ost = [Es(nc.sbuf_tensor(f"ost{i}", [128, 512], BF16)) for i in range(2)]
                pS2 = [Es(nc.psum_tensor(f"pS2_{i}", [128, 2, 512], F32)) for i in range(2)]

                class _V:
                    def __init__(self, t, m):
                        self.t, self.m = t, m

                    def __getitem__(self, idx):
                        if idx == slice(None):
                            return self.t[:, self.m, :]
                        return self.t[:, self.m, :][idx]

                BK = [_V(pS2[0], 0), _V(pS2[0], 1), _V(pS2[1], 0), _V(pS2[1], 1)] + [Es(nc.psum_tensor(f"BK{i}", [128, 512], F32)) for i in range(4, 8)]
                B7b = BK[7][:].bitcast(BF16)
                rinv = CF("rinv")
                for q4 in range(4):
                    cs = slice(q4 * 1024, (q4 + 1) * 1024)
                    P.dma("sp", tmpi[:], pos[q4 * 1024:(q4 + 1) * 1024].partition_broadcast(128), writes=["tmpi"])
                    P.op("dve", lambda e: e.tensor_copy(out=tmpa[:], in_=tmpi[:]), reads=["tmpi"], writes=["tmpa"])
                    for which in range(2):
                        dstT = St if which == 0 else Ct
                        dkey = f"St{q4}" if which == 0 else f"Ct{q4}"
                        P.op("dve", lambda e, which=which: e.tensor_scalar(out=tmpb[:], in0=tmpa[:], scalar1=rinv[:, 0:1], scalar2=(0.0 if which == 0 else math.pi / 2), op0=ALU.mult, op1=ALU.add), reads=["tmpa", "cf"], writes=["tmpb"])
                        P.op("dve", lambda e: e.tensor_scalar(out=tmpi[:], in0=tmpb[:], scalar1=1.0 / (2 * math.pi), scalar2=None, op0=ALU.mult), reads=["tmpb"], writes=["tmpi"])
                        P.op("dve", lambda e, dstT=dstT, cs=cs: e.tensor_copy(out=dstT[:, cs], in_=tmpi[:]), reads=["tmpi"], writes=[dkey])
                        P.op("dve", lambda e, dstT=dstT, cs=cs: e.scalar_tensor_tensor(out=tmpb[:], in0=dstT[:, cs], scalar=-2 * math.pi, in1=tmpb[:], op0=ALU.mult, op1=ALU.add), reads=[dkey, "tmpb"], writes=["tmpb"])
                        P.op("dve", lambda e, dstT=dstT, cs=cs: e.tensor_scalar(out=dstT[:, cs], in0=tmpb[:], scalar1=math.pi, scalar2=-2 * math.pi, op0=ALU.is_gt, op1=ALU.mult), reads=["tmpb"], writes=[dkey])
                        P.op("dve", lambda e, dstT=dstT, cs=cs: e.tensor_tensor(out=tmpb[:], in0=tmpb[:], in1=dstT[:, cs], op=ALU.add), reads=["tmpb", dkey], writes=["tmpb"])
                        P.op("act", lambda e, dstT=dstT, cs=cs: e.activation(out=dstT[:, cs], in_=tmpb[:], func=AF.Sin), reads=["tmpb"], writes=[dkey])
                        if which == 0:
                            P.op("dve", lambda e, cs=cs: e.tensor_scalar(out=St[:, cs], in0=St[:, cs], scalar1=rinv[:, 1:2], scalar2=None, op0=ALU.mult), reads=[f"St{q4}", "cf"], writes=[f"St{q4}"])
                P.dma("sp", lamt[:], lamv.rearrange("a b -> (a b)").partition_broadcast(128), writes=["lamt"])
                P.dma("sp", gsub[:], g_subln.partition_broadcast(128), writes=["gsub"])
                P.op("dve", lambda e: e.tensor_tensor(out=lamp[:, 0:64], in0=lamt[:, 0:64], in1=lamt[:, 64:128], op=ALU.mult), reads=["lamt"], writes=["lamp"])
                P.op("dve", lambda e: e.tensor_tensor(out=lamp[:, 64:128], in0=lamt[:, 128:192], in1=lamt[:, 192:256], op=ALU.mult), reads=["lamt"], writes=["lamp"])
                P.op("dve", lambda e: e.reduce_sum(out=lsc[:, 0:1], in_=lamp[:, 0:64], axis=AX.X), reads=["lamp"], writes=["lsc"])
                P.op("dve", lambda e: e.reduce_sum(out=lsc[:, 1:2], in_=lamp[:, 64:128], axis=AX.X), reads=["lamp"], writes=["lsc"])
                P.op("act", lambda e: e.activation(out=lsc[:, 0:2], in_=lsc[:, 0:2], func=AF.Exp), reads=["lsc"], writes=["lsc"])
                P.op("dve", lambda e: e.tensor_tensor(out=lsc[:, 2:3], in0=lsc[:, 1:2], in1=lsc[:, 0:1], op=ALU.subtract), reads=["lsc"], writes=["lsc"])
                P.op("dve", lambda e: e.tensor_scalar(out=lsc[:, 2:3], in0=lsc[:, 2:3], scalar1=-LAM_INIT, scalar2=None, op0=ALU.add), reads=["lsc"], writes=["lsc"])
                P.op("dve", lambda e: e.tensor_scalar(out=gsub[:], in0=gsub[:], scalar1=(1.0 - LAM_INIT), scalar2=None, op0=ALU.mult), reads=["gsub"], writes=["gsub"])
                for i in range(2):
                    P.op("dve", lambda e, i=i: e.memset(Vt[i][:, :, 128:130], 1.0), writes=[f"Vt{i}"])

                deferred = []
                ZB = [0, 4]
                ZPB = [1, 5]

                def proj_rot(wt, wkeys, dstt, dkey, hcol0, nblk, tcol0):
                    def mm(tb):
                        zb_ = ZB[tb % 2]
                        for dk in range(8):
                            P.op("pe", lambda e, dk=dk, tb=tb, zb_=zb_: e.matmul(BK[zb_][:], lhsT=wt[:, dk, :], rhs=hT[:, dk, hcol0 + tb * 512:hcol0 + (tb + 1) * 512], start=(dk == 0), stop=(dk == 7)), reads=wkeys + ["hT"], writes=[f"BK{zb_}"])

                    mm(0)
                    for tb in range(nblk):
                        zi = tb % 2
                        zb_ = ZB[tb % 2]
                        zp_ = ZPB[tb % 2]
                        tq = (tcol0 + tb * 512) // 1024
                        if tb + 1 < nblk:
                            mm(tb + 1)
                        P.op("act", lambda e, zi=zi, zb_=zb_: e.activation(out=zb[zi][:], in_=BK[zb_][:], func=AF.Copy), reads=[f"BK{zb_}"], writes=[f"zb{zi}"])
                        P.op("dve", lambda e, zi=zi, tb=tb: e.tensor_tensor(out=t1[zi][:], in0=zb[zi][:], in1=Ct[:, tcol0 + tb * 512:tcol0 + (tb + 1) * 512], op=ALU.mult), reads=[f"zb{zi}", f"Ct{tq}"], writes=[f"t1{zi}"])
                        P.op("pe", lambda e, zi=zi, zp_=zp_: e.matmul(BK[zp_][:], lhsT=permb, rhs=zb[zi][:], start=True, stop=True), reads=[f"zb{zi}", "cb"], writes=[f"BK{zp_}"])
                        P.op("dve", lambda e, zi=zi, tb=tb, zp_=zp_: e.tensor_tensor(out=t2[zi][:], in0=BK[zp_][:], in1=St[:, tcol0 + tb * 512:tcol0 + (tb + 1) * 512], op=ALU.mult), reads=[f"BK{zp_}", f"St{tq}"], writes=[f"t2{zi}"])
                        P.op("dve", lambda e, zi=zi, tb=tb: e.tensor_tensor(out=dstt[:, tb * 512:(tb + 1) * 512], in0=t1[zi][:], in1=t2[zi][:], op=ALU.add), reads=[f"t1{zi}", f"t2{zi}"], writes=[dkey])
                        if deferred:
                            deferred.pop(0)[1]()

                import os
                nq = [0]
                no = [0]
                for h in range(int(os.environ.get("KDBG_NH", NH))):
                    hb = h % 2
                    for (wt, nm_, c0) in ((wq, "wq", C_Q), (wk, "wk", C_K), (wv, "wv", C_V)):
                        P.dma("pool", wt[hb][:], w_in[:, c0 + h * 128:c0 + (h + 1) * 128].rearrange("(c p) n -> p c n", p=128), writes=[f"{nm_}{hb}"])
                    proj_rot(wk[hb], [f"wk{hb}"], kT[hb], f"kT{hb}", 0, 8, 0)
                    for tg in range(NT // 4):
                        bi = 2 + tg % 2
                        for j in range(4):
                            ti = tg * 4 + j
                            for dk in range(8):
                                P.op("pe", lambda e, dk=dk, ti=ti, j=j, bi=bi, hb=hb: e.matmul(BK[bi][:, j * 128:(j + 1) * 128], lhsT=hT[:, dk, ti * 128:(ti + 1) * 128], rhs=wv[hb][:, dk, :], start=(dk == 0), stop=(dk == 7)), reads=[f"wv{hb}", "hT"], writes=[f"BK{bi}"])
                        P.op("dve", lambda e, tg=tg, bi=bi, hb=hb: e.tensor_copy(out=Vt[hb][:, tg * 4:(tg + 1) * 4, 0:128], in_=BK[bi][:].rearrange("p (j n) -> p j n", j=4)), reads=[f"BK{bi}"], writes=[f"Vt{hb}"])
                    for hf in range(2):
                        own0 = hf * HQ
                        qi = nq[0] % 2
                        nq[0] += 1
                        proj_rot(wq[hb], [f"wq{hb}"], qT[qi], f"qT{qi}", own0, 4, own0)
                        for qb in range(4):
                            def acc(m, qs):
                                a = m * 4 + qs
                                return BK[4 + a // 3][:, (a % 3) * 160:(a % 3) * 160 + 130]

                            PO = ["BK4", "BK5", "BK6"]

                            def qk(kt, hb=hb, qi=qi, qb=qb):
                                si = kt % 2
                                for m in range(2):
                                    bi = si * 2 + m
                                    P.op("pe", lambda e, m=m, kt=kt, bi=bi: e.matmul(BK[bi][:], lhsT=kT[hb][m * 64:(m + 1) * 64, kt * 128:(kt + 1) * 128], rhs=qT[qi][m * 64:(m + 1) * 64, qb * 512:(qb + 1) * 512], start=True, stop=True), reads=[f"kT{hb}", f"qT{qi}"], writes=[f"BK{bi}"])

                            qk(0)
                            for kt in range(NT):
                                si = kt % 2
                                pi = kt % 3
                                if kt + 1 < NT:
                                    qk(kt + 1)
                                for m in range(2):
                                    bi = si * 2 + m
                                    P.op("act", lambda e, bi=bi, pi=pi, m=m: e.activation(out=PT[pi][:, m, :], in_=BK[bi][:], func=AF.Exp, scale=SCALE), reads=[f"BK{bi}"], writes=[f"PT{pi}_{m}"])
                                while deferred and deferred[0][0] <= kt:
                                    deferred.pop(0)[1]()
                                for m in range(2):
                                    for qs in range(4):
                                        P.op("pe", lambda e, m=m, qs=qs, kt=kt, pi=pi, hb=hb: e.matmul(acc(m, qs), lhsT=PT[pi][:, m, qs * 128:(qs + 1) * 128], rhs=Vt[hb][:, kt, 0:130], start=(kt == 0 and (m * 4 + qs) % 3 == 0), stop=(kt == NT - 1), skip_group_check=True), reads=[f"PT{pi}_{m}", f"Vt{hb}"], writes=PO)
                            oi = no[0] % 2
                            no[0] += 1
                            for qs in range(4):
                                fk = f"fin{qs}"
                                P.op("dve", lambda e, qs=qs: e.reciprocal(out=fin[:, qs, 0:1], in_=acc(0, qs)[:, 128:129]), reads=PO, writes=[fk])
                                P.op("dve", lambda e, qs=qs: e.reciprocal(out=fin[:, qs, 1:2], in_=acc(1, qs)[:, 128:129]), reads=PO, writes=[fk])
                                P.op("dve", lambda e, qs=qs: e.tensor_tensor(out=fin[:, qs, 1:2], in0=fin[:, qs, 1:2], in1=lsc[:, 2:3], op=ALU.mult), reads=[fk, "lsc"], writes=[fk])
                                P.op("dve", lambda e, qs=qs: e.tensor_scalar(out=o1[qs][:], in0=acc(0, qs)[:, 0:128], scalar1=fin[:, qs, 0:1], scalar2=None, op0=ALU.mult), reads=PO + [fk], writes=[f"o1{qs}"])
                                P.op("dve", lambda e, qs=qs: e.scalar_tensor_tensor(out=o2[qs][:], in0=acc(1, qs)[:, 0:128], scalar=fin[:, qs, 1:2], in1=o1[qs][:], op0=ALU.mult, op1=ALU.add), reads=PO + [fk, f"o1{qs}"], writes=[f"o2{qs}"])
                            FB = [f"finb{q}" for q in range(4)]

                            def fin_b():
                                for qs in range(4):
                                    P.op("act", lambda e, qs=qs: e.activation(out=ojunk[:], in_=o2[qs][:], func=AF.Square, accum_out=fin[:, qs, 2:3]), reads=[f"o2{qs}"], writes=["ojunk", f"finb{qs}"])

                            def fin_c():
                                P.op("dve", lambda e: e.tensor_scalar(out=fin[:, :, 2], in0=fin[:, :, 2], scalar1=1.0 / 128, scalar2=EPS, op0=ALU.mult, op1=ALU.add), reads=FB, writes=FB)

                            def fin_d():
                                P.op("act", lambda e: e.activation(out=fin[:, :, 2], in_=fin[:, :, 2], func=AF.Ln), reads=FB, writes=FB)
                                P.op("act", lambda e: e.activation(out=fin[:, :, 2], in_=fin[:, :, 2], func=AF.Exp, scale=-0.5), reads=FB, writes=FB)

                            def fin_e():
                                for qs in range(4):
                                    P.op("dve", lambda e, qs=qs: e.scalar_tensor_tensor(out=ob[qs][:], in0=o2[qs][:], scalar=fin[:, qs, 2:3], in1=gsub[:], op0=ALU.mult, op1=ALU.mult), reads=[f"o2{qs}", f"finb{qs}", "gsub"], writes=[f"ob{qs}"])

                            def fin_f(oi=oi, hf=hf, h=h, qb=qb):
                                for qs in range(4):
                                    P.op("pe", lambda e, qs=qs: e.transpose(out=B7b[:, qs * 128:(qs + 1) * 128], in_=ob[qs][:], identity=idb), reads=[f"ob{qs}", "cb"], writes=["BK7"])
                                P.op("dve", lambda e, oi=oi: e.tensor_copy(out=ost[oi][:], in_=B7b[:, 0:512]), reads=["BK7"], writes=[f"ost{oi}"])
                                P.dma("sp", oTbuf[hf, h * 128:(h + 1) * 128, qb * 512:(qb + 1) * 512], ost[oi][:], reads=[f"ost{oi}"], writes=["oTbuf"])

                            deferred[:] = [(2, fin_b), (3, fin_c), (4, fin_d), (6, fin_e), (8, fin_f)]
                while deferred:
                    deferred.pop(0)[1]()
                P.flush()
        if stop_after <= 2:
            P.finish()
            return nc

        with ExitStack() as es:
            Es = es.enter_context
            wao = Es(nc.sbuf_tensor("wao", [128, 8, 1024], BF16))
            wo = Es(nc.sbuf_tensor("wo", [128, 8, 1024], BF16))
            oTb = [Es(nc.sbuf_tensor(f"oTb{i}", [128, 8, 512], BF16)) for i in range(2)]
            Gab = [Es(nc.sbuf_tensor(f"Gab{i}", [128, 8, 512], BF16)) for i in range(2)]
            Ab = [Es(nc.sbuf_tensor(f"Ab{i}", [128, 8, 512], BF16)) for i in range(2)]
            mT = [Es(nc.sbuf_tensor(f"mT{i}", [128, 8, 512], BF16)) for i in range(2)]
            tmpm = [Es(nc.sbuf_tensor(f"tmpm{i}", [128, 512], F32)) for i in range(2)]
            xt2 = [Es(nc.sbuf_tensor(f"xt2{i}", [128, D], F32)) for i in range(2)]
            x1 = [Es(nc.sbuf_tensor(f"x1{i}", [128, D], F32)) for i in range(2)]
            h2 = [Es(nc.sbuf_tensor(f"h2{i}", [128, D], BF16)) for i in range(2)]
            x1T = [Es(nc.sbuf_tensor(f"x1T{i}", [128, 8, 128], F32)) for i in range(2)]
            junk2 = Es(nc.sbuf_tensor("junk2", [128, D], BF16))
            wr = Es(nc.sbuf_tensor("wr", [128, 8, NE], F32))
            gf = Es(nc.sbuf_tensor("gf", [128, 8], F32))
            sm = [Es(nc.sbuf_tensor(f"sm{i}", [128, 8], F32)) for i in range(3)]
            lg = [Es(nc.sbuf_tensor(f"lg{i}", [128, NE], F32)) for i in range(2)]
            ex = [Es(nc.sbuf_tensor(f"ex{i}", [128, NE], F32)) for i in range(2)]
            aff = [Es(nc.sbuf_tensor(f"aff{i}", [128, NE], F32)) for i in range(2)]
            affTs = [Es(nc.sbuf_tensor(f"affTs{i}", [NE, 128], F32)) for i in range(2)]
            BK = [Es(nc.psum_tensor(f"CK{i}", [128, 512], F32)) for i in range(8)]

            def loadw(dst, key, src_ap):
                v = src_ap.rearrange("(c p) n -> p c n", p=128)
                for q in range(4):
                    P.dma("pool", dst[:, :, q * 256:(q + 1) * 256], v[:, :, q * 256:(q + 1) * 256], writes=[f"{key}_{q}"])
                return [f"{key}_{q}" for q in range(4)]

            WAO = loadw(wao, "wao", w_attn_out)
            WO = loadw(wo, "wo", w_out)
            P.dma("sp", wr[:], w_router.rearrange("(c p) e -> p c e", p=128), writes=["wr"])
            P.dma("sp", gf[:], g_ffn.rearrange("(c p) -> p c", p=128), writes=["gf"], allow_slow_non_contiguous=True)
            for c in range(8):
                P.op("dve", lambda e, c=c: e.tensor_scalar(out=wr[:, c, :], in0=wr[:, c, :], scalar1=gf[:, c:c + 1], scalar2=None, op0=ALU.mult), reads=["wr", "gf"], writes=["wr"])
            def Y(blk):
                hf, tb = blk // 4, blk % 4
                bi = blk % 2
                oTv = oTbuf[hf].rearrange("(c p) t -> p c t", p=128)
                Gav = Gabuf[hf].rearrange("(c p) t -> p c t", p=128)
                Av = Abuf[hf].rearrange("(c p) t -> p c t", p=128)
                cs = slice(tb * 512, (tb + 1) * 512)
                P.dma("sp", oTb[bi][:], oTv[:, :, cs], reads=["oTbuf"], writes=[f"oTb{bi}"])
                P.dma("sp", Gab[bi][:], Gav[:, :, cs], reads=["Gabuf"], writes=[f"Gab{bi}"])
                P.dma("sp", Ab[bi][:], Av[:, :, cs], reads=["Abuf"], writes=[f"Ab{bi}"])
                for dc in range(8):
                    pb = dc % 2
                    for c in range(8):
                        P.op("pe", lambda e, c=c, dc=dc, pb=pb, bi=bi: e.matmul(BK[pb][:], lhsT=wao[:, c, dc * 128:(dc + 1) * 128], rhs=oTb[bi][:, c, :], start=(c == 0), stop=(c == 7)), reads=WAO + [f"oTb{bi}"], writes=[f"CK{pb}"])
                    P.op("dve", lambda e, dc=dc, pb=pb, bi=bi: e.tensor_tensor(out=tmpm[pb][:], in0=BK[pb][:], in1=Gab[bi][:, dc, :], op=ALU.mult), reads=[f"CK{pb}", f"Gab{bi}"], writes=[f"tmpm{pb}"])
                    P.op("dve", lambda e, dc=dc, pb=pb, bi=bi: e.tensor_tensor(out=mT[bi][:, dc, :], in0=tmpm[pb][:], in1=Ab[bi][:, dc, :], op=ALU.add), reads=[f"tmpm{pb}", f"Ab{bi}"], writes=[f"mT{bi}"])

            def Dt(t):
                blk, j = t // 4, t % 4
                bi, xi = blk % 2, t % 2
                rows = slice(t * 128, (t + 1) * 128)
                si3 = t % 3
                sk = f"sm{si3}"
                P.dma("sp", xt2[xi][:], x[rows, :], writes=[f"xt2{xi}"])
                for half in range(2):
                    pb = 2 + half
                    for dk in range(8):
                        P.op("pe", lambda e, dk=dk, half=half, pb=pb, bi=bi, j=j: e.matmul(BK[pb][:], lhsT=mT[bi][:, dk, j * 128:(j + 1) * 128], rhs=wo[:, dk, half * 512:(half + 1) * 512], start=(dk == 0), stop=(dk == 7)), reads=WO + [f"mT{bi}"], writes=[f"CK{pb}"])
                    P.op("dve", lambda e, half=half, pb=pb, xi=xi: e.tensor_tensor(out=x1[xi][:, half * 512:(half + 1) * 512], in0=BK[pb][:], in1=xt2[xi][:, half * 512:(half + 1) * 512], op=ALU.add), reads=[f"CK{pb}", f"xt2{xi}"], writes=[f"x1{xi}"])
                P.dma("pool", x1buf[rows, :], x1[xi][:], reads=[f"x1{xi}"], writes=["x1buf"])
                P.op("act", lambda e, xi=xi, si3=si3: e.activation(out=junk2[:], in_=x1[xi][:], func=AF.Square, accum_out=sm[si3][:, 0:1]), reads=[f"x1{xi}"], writes=["junk2", sk])
                P.op("dve", lambda e, si3=si3: e.tensor_scalar(out=sm[si3][:, 0:1], in0=sm[si3][:, 0:1], scalar1=1.0 / D, scalar2=EPS, op0=ALU.mult, op1=ALU.add), reads=[sk], writes=[sk])
                P.op("act", lambda e, si3=si3: e.activation(out=sm[si3][:, 0:1], in_=sm[si3][:, 0:1], func=AF.Ln), reads=[sk], writes=[sk])
                P.op("act", lambda e, si3=si3: e.activation(out=sm[si3][:, 0:1], in_=sm[si3][:, 0:1], func=AF.Exp, scale=-0.5), reads=[sk], writes=[sk])
                P.op("act", lambda e, xi=xi, si3=si3: e.activation(out=h2[xi][:], in_=x1[xi][:], func=AF.Copy, scale=sm[si3][:, 0:1]), reads=[f"x1{xi}", sk], writes=[f"h2{xi}"])
                P.dma("pool", h2buf[rows, :], h2[xi][:], reads=[f"h2{xi}"], writes=["h2buf"])

            def Tt(t):
                xi = t % 2
                for dk in range(8):
                    pb = 4 + dk // 4
                    P.op("pe", lambda e, dk=dk, pb=pb, xi=xi: e.transpose(out=BK[pb][:, (dk % 4) * 128:(dk % 4 + 1) * 128], in_=x1[xi][:, dk * 128:(dk + 1) * 128], identity=idf), reads=[f"x1{xi}", "cf"], writes=[f"CK{pb}"])
                for pb in (4, 5):
                    P.op("act", lambda e, pb=pb, xi=xi: e.activation(out=x1T[xi][:, (pb - 4) * 4:(pb - 3) * 4, :], in_=BK[pb][:].rearrange("p (j n) -> p j n", j=4), func=AF.Copy), reads=[f"CK{pb}"], writes=[f"x1T{xi}"])

            def Lt(t):
                xi = t % 2
                rows = slice(t * 128, (t + 1) * 128)
                si3 = t % 3
                sk = f"sm{si3}"
                for dk in range(8):
                    P.op("pe", lambda e, dk=dk, xi=xi: e.matmul(BK[6][:, 0:NE], lhsT=x1T[xi][:, dk, :], rhs=wr[:, dk, :], start=(dk == 0), stop=(dk == 7)), reads=[f"x1T{xi}", "wr"], writes=["CK6"])
                P.op("dve", lambda e, xi=xi, si3=si3: e.tensor_scalar(out=lg[xi][:], in0=BK[6][:, 0:NE], scalar1=sm[si3][:, 0:1], scalar2=None, op0=ALU.mult), reads=["CK6", sk], writes=[f"lg{xi}"])
                P.op("dve", lambda e, xi=xi, si3=si3: e.reduce_max(out=sm[si3][:, 1:2], in_=lg[xi][:], axis=AX.X), reads=[f"lg{xi}"], writes=[sk + "b"])
                P.op("dve", lambda e, xi=xi, si3=si3: e.tensor_scalar(out=sm[si3][:, 1:2], in0=sm[si3][:, 1:2], scalar1=-1.0, scalar2=None, op0=ALU.mult), reads=[sk + "b"], writes=[sk + "b"])
                P.op("act", lambda e, xi=xi, si3=si3: e.activation(out=ex[xi][:], in_=lg[xi][:], func=AF.Exp, bias=sm[si3][:, 1:2], scale=1.0, accum_out=sm[si3][:, 2:3]), reads=[f"lg{xi}", sk + "b"], writes=[f"ex{xi}", sk + "c"])
                P.op("dve", lambda e, xi=xi, si3=si3: e.reciprocal(out=sm[si3][:, 2:3], in_=sm[si3][:, 2:3]), reads=[sk + "c"], writes=[sk + "c"])
                P.op("dve", lambda e, xi=xi, si3=si3: e.tensor_scalar(out=aff[xi][:], in0=ex[xi][:], scalar1=sm[si3][:, 2:3], scalar2=None, op0=ALU.mult), reads=[f"ex{xi}", sk + "c"], writes=[f"aff{xi}"])
                P.dma("pool", affbuf[rows, :], aff[xi][:], reads=[f"aff{xi}"], writes=["affbuf"])

            def At(t):
                xi = t % 2
                rows = slice(t * 128, (t + 1) * 128)
                P.op("pe", lambda e, xi=xi: e.transpose(out=BK[7][0:NE, 0:128], in_=aff[xi][:], identity=idf), reads=[f"aff{xi}", "cf"], writes=["CK7"])
                P.op("act", lambda e, xi=xi: e.activation(out=affTs[xi][:], in_=BK[7][0:NE, 0:128], func=AF.Copy), reads=["CK7"], writes=[f"affTs{xi}"])
                P.dma("pool", affTbuf[:, rows], affTs[xi][:], reads=[f"affTs{xi}"], writes=["affTbuf"])

            Y(0)
            Dt(0)
            for t in range(NT):
                if t % 4 == 1 and t // 4 + 1 < 8:
                    Y(t // 4 + 1)
                if t + 1 < NT:
                    Dt(t + 1)
                Tt(t)
                if t >= 1:
                    Lt(t - 1)
                if t >= 2:
                    At(t - 2)
            Lt(NT - 1)
            At(NT - 2)
            At(NT - 1)
            P.flush()
        if stop_after <= 3:
            P.finish()
            return nc

        with ExitStack() as es:
            Es = es.enter_context
            affP = Es(nc.sbuf_tensor("affP", [128, 512], F32))
            afft = Es(nc.sbuf_tensor("afft", [128, NT, NE], F32))
            bjunk = Es(nc.sbuf_tensor("bjunk", [128, 512], BF16))
            bs = Es(nc.sbuf_tensor("bs", [128, 8], F32))
            taub = Es(nc.sbuf_tensor("taub", [128, NE], F32))
            mask = Es(nc.sbuf_tensor("mask", [128, NT, NE], F32))
            pa = Es(nc.sbuf_tensor("pa", [128, NT, NE], F32))
            pb_ = Es(nc.sbuf_tensor("pb_", [128, NT, NE], F32))
            slot = Es(nc.sbuf_tensor("slot", [128, NT, NE], F32))
            ra = Es(nc.sbuf_tensor("ra", [128, NT, NE], F32))
            rbf = Es(nc.sbuf_tensor("rbf", [128, NT, NE], BF16))
            R = Es(nc.sbuf_tensor("R", [128, NT, NE, 6], BF16))
            gf3 = Es(nc.sbuf_tensor("gf3", [128, 8], F32))
            oh = [Es(nc.sbuf_tensor(f"oh{i}", [128, 512], BF16)) for i in range(4)]
            idxf = [Es(nc.sbuf_tensor(f"idxf{i}", [128, 4], F32)) for i in range(2)]
            idxi = [Es(nc.sbuf_tensor(f"idxi{i}", [128, 4], I32)) for i in range(2)]
            valt = [Es(nc.sbuf_tensor(f"valt{i}", [128, 4], F32)) for i in range(2)]
            pIs = Es(nc.sbuf_tensor("pIs", [128, 32], F32))
            xs = [Es(nc.sbuf_tensor(f"xs{i}", [128, 4, D], BF16)) for i in range(2)]
            xsT = [Es(nc.sbuf_tensor(f"xsT{i}", [128, 8, 512], BF16)) for i in range(2)]
            hTm = [Es(nc.sbuf_tensor(f"hTm{i}", [128, NFC, 512], BF16)) for i in range(2)]
            yt = Es(nc.sbuf_tensor("yt", [128, 4, D], F32))
            sgt = [Es(nc.sbuf_tensor(f"sgt{i}", [128, 512], F32)) for i in range(2)]
            NGU = 3
            ND = 4
            wg = [Es(nc.sbuf_tensor(f"wg{i}", [128, 8, 256], BF16)) for i in range(NGU)]
            wu = [Es(nc.sbuf_tensor(f"wu{i}", [128, 8, 256], BF16)) for i in range(NGU)]
            wd = [Es(nc.sbuf_tensor(f"wd{i}", [128, 2, 512], BF16)) for i in range(ND)]
            MK = [Es(nc.psum_tensor(f"MK{i}", [128, 512], F32)) for i in range(8)]
            MK6b = MK[6][:].bitcast(BF16)
            MK7b = MK[7][:].bitcast(BF16)
            gsum = CF("gsum")
            P.dma("sp", affP[:], affTbuf.rearrange("e (j c) -> (e j) c", j=8), reads=["affTbuf"], writes=["affP"])
            P.dma("sp", afft[:], affbuf.rearrange("(n p) e -> p n e", p=128), reads=["affbuf"], writes=["afft"])
            P.dma("sp", gf3[:], g_ffn.rearrange("(c p) -> p c", p=128), writes=["gf3"], allow_slow_non_contiguous=True)
            NIT = 28
            P.op("dve", lambda e: e.memset(bs[:, 0:1], 0.0), writes=["bs_lo"])
            P.op("dve", lambda e: e.memset(bs[:, 1:2], 0.5), writes=["bs_mid"])
            for it in range(NIT):
                wdt = 2.0 ** -(it + 1)
                P.op("dve", lambda e: e.tensor_scalar(out=bjunk[:], in0=affP[:], scalar1=bs[:, 1:2], scalar2=0.0, op0=ALU.is_ge, op1=ALU.add, accum_out=bs[:, 2:3]), reads=["affP", "bs_mid"], writes=["bjunk", "bs_cnt"])
                P.op("pe", lambda e: e.matmul(MK[4][:, 0:1], lhsT=gsum, rhs=bs[:, 2:3], start=True, stop=True), reads=["cf", "bs_cnt"], writes=["MK4"])
                P.op("dve", lambda e: e.tensor_scalar(out=bs[:, 3:4], in0=MK[4][:, 0:1], scalar1=CAP - 0.5, scalar2=None, op0=ALU.is_ge), reads=["MK4"], writes=["bs_ge"])
                P.op("dve", lambda e, wdt=wdt: e.scalar_tensor_tensor(out=bs[:, 0:1], in0=bs[:, 3:4], scalar=wdt, in1=bs[:, 0:1], op0=ALU.mult, op1=ALU.add), reads=["bs_ge", "bs_lo"], writes=["bs_lo"])
                P.op("dve", lambda e, wdt=wdt: e.tensor_scalar(out=bs[:, 1:2], in0=bs[:, 0:1], scalar1=wdt / 2, scalar2=None, op0=ALU.add), reads=["bs_lo"], writes=["bs_mid"])
            P.dma("sp", taubuf.rearrange("(p o) -> p o", o=1), bs[:, 0:1], reads=["bs_lo"], writes=["taubuf"])
            P.dma("sp", taub[:], taubuf.rearrange("(e j) -> e j", j=8)[:, 0:1].rearrange("e o -> (e o)").partition_broadcast(128), reads=["taubuf"], writes=["taub"], allow_slow_non_contiguous=True)
            for n in range(NT):
                P.op("dve", lambda e, n=n: e.tensor_tensor(out=mask[:, n, :], in0=afft[:, n, :], in1=taub[:], op=ALU.is_ge), reads=["afft", "taub"], writes=["mask"])
            m2 = mask[:].rearrange("p n e -> p (n e)")
            P.op("pe", lambda e: e.matmul(MK[4][:], lhsT=CF("utri"), rhs=m2, start=True, stop=True), reads=["cf", "mask"], writes=["MK4"])
            P.op("pe", lambda e: e.matmul(MK[5][:], lhsT=CF("ones"), rhs=m2, start=True, stop=True), reads=["cf", "mask"], writes=["MK5"])
            P.op("dve", lambda e: e.tensor_copy(out=pa[:].rearrange("p n e -> p (n e)"), in_=MK[5][:]), reads=["MK5"], writes=["pa"])
            src, sk_, dst, dk_ = pa, "pa", pb_, "pb_"
            for sft in (1, 2, 4, 8, 16):
                P.op("dve", lambda e, src=src, dst=dst, sft=sft: e.tensor_tensor(out=dst[:, sft:, :], in0=src[:, sft:, :], in1=src[:, :NT - sft, :], op=ALU.add), reads=[sk_], writes=[dk_])
                P.op("dve", lambda e, src=src, dst=dst, sft=sft: e.tensor_copy(out=dst[:, :sft, :], in_=src[:, :sft, :]), reads=[sk_], writes=[dk_])
                src, sk_, dst, dk_ = dst, dk_, src, sk_
            P.op("dve", lambda e, src=src: e.tensor_tensor(out=slot[:].rearrange("p n e -> p (n e)"), in0=MK[4][:], in1=src[:].rearrange("p n e -> p (n e)"), op=ALU.add), reads=["MK4", sk_], writes=["slot"])
            P.op("dve", lambda e: e.tensor_tensor(out=slot[:].rearrange("p n e -> p (n e)"), in0=slot[:].rearrange("p n e -> p (n e)"), in1=MK[5][:], op=ALU.subtract), reads=["slot", "MK5"], writes=["slot"])
            P.op("dve", lambda e: e.tensor_tensor(out=slot[:], in0=slot[:], in1=mask[:], op=ALU.mult), reads=["slot", "mask"], writes=["slot"])
            P.op("dve", lambda e: e.tensor_scalar(out=slot[:], in0=slot[:], scalar1=-1.0, scalar2=None, op0=ALU.add), reads=["slot"], writes=["slot"])
            thl = CF("thl").rearrange("p (n t) -> p n t", t=2)
            P.op("dve", lambda e: e.memset(R[:], 0.0), writes=["R"])
            for ee in range(NE):
                P.op("dve", lambda e, ee=ee: e.tensor_copy(out=R[:, :, ee, 0:2], in_=thl), reads=["cf"], writes=["R"])
            P.op("dve", lambda e: e.tensor_copy(out=rbf[:], in_=afft[:]), reads=["afft"], writes=["rbf"])
            P.op("dve", lambda e: e.tensor_copy(out=R[:, :, :, 2], in_=rbf[:]), reads=["rbf"], writes=["R"])
            P.op("dve", lambda e: e.tensor_tensor(out=ra[:], in0=afft[:], in1=rbf[:], op=ALU.subtract), reads=["afft", "rbf"], writes=["ra"])
            P.op("dve", lambda e: e.tensor_copy(out=rbf[:], in_=ra[:]), reads=["ra"], writes=["rbf"])
            P.op("dve", lambda e: e.tensor_copy(out=R[:, :, :, 3], in_=rbf[:]), reads=["rbf"], writes=["R"])
            P.op("dve", lambda e: e.tensor_tensor(out=ra[:], in0=ra[:], in1=rbf[:], op=ALU.subtract), reads=["ra", "rbf"], writes=["ra"])
            P.op("dve", lambda e: e.tensor_copy(out=R[:, :, :, 4], in_=ra[:]), reads=["ra"], writes=["R"])

            gu_chunks = [(ee, pr) for ee in range(NE) for pr in range(NFC // 2)]
            d_chunks = [(ee, half, pr) for ee in range(NE) for half in range(2) for pr in range(NFC // 2)]
            gu_next = [0]
            d_next = [0]

            def emit_gu_load():
                i = gu_next[0]
                if i >= len(gu_chunks):
                    return
                gu_next[0] += 1
                ee, pr = gu_chunks[i]
                sl = i % NGU
                P.dma("pool", wg[sl][:], w_gate[ee][:, pr * 256:(pr + 1) * 256].rearrange("(c p) f -> p c f", p=128), writes=[f"wg{sl}"])
                P.dma("pool", wu[sl][:], w_up[ee][:, pr * 256:(pr + 1) * 256].rearrange("(c p) f -> p c f", p=128), writes=[f"wu{sl}"])

            def emit_d_load():
                i = d_next[0]
                if i >= len(d_chunks):
                    return
                d_next[0] += 1
                ee, half, pr = d_chunks[i]
                sl = i % ND
                P.dma("pool", wd[sl][:], w_down[ee][pr * 256:(pr + 1) * 256, half * 512:(half + 1) * 512].rearrange("(c p) d -> p c d", p=128), writes=[f"wd{sl}"])

            for _ in range(NGU):
                emit_gu_load()
            for _ in range(ND):
                emit_d_load()

            def sel_onehot(ee, n):
                oi = n % 4
                P.op("dve", lambda e, n=n, oi=oi, ee=ee: e.tensor_scalar(out=oh[oi][:], in0=CF("iota"), scalar1=slot[:, n, ee:ee + 1], scalar2=None, op0=ALU.is_equal), reads=["cf", "slot"], writes=[f"oh{oi}"])

            def sel_mm(ee, n):
                oi = n % 4
                for sb in range(4):
                    P.op("pe", lambda e, n=n, oi=oi, sb=sb, ee=ee: e.matmul(MK[4][:, sb * 8:sb * 8 + 6], lhsT=oh[oi][:, sb * 128:(sb + 1) * 128], rhs=R[:, n, ee, :], start=(n == 0 and sb == 0), stop=(n == NT - 1), skip_group_check=True), reads=[f"oh{oi}", "R"], writes=["MK4"])

            def prep_idx(ee):
                b = ee % 2
                P.op("dve", lambda e: e.tensor_copy(out=pIs[:], in_=MK[4][:, 0:32]), reads=["MK4"], writes=["pIs"])
                pI = pIs[:].rearrange("p (s k) -> p s k", k=8)
                P.op("dve", lambda e, b=b: e.scalar_tensor_tensor(out=idxf[b][:], in0=pI[:, :, 0], scalar=64.0, in1=pI[:, :, 1], op0=ALU.mult, op1=ALU.add), reads=["pIs"], writes=[f"idxf{b}"])
                P.op("dve", lambda e, b=b: e.tensor_copy(out=idxi[b][:], in_=idxf[b][:]), reads=[f"idxf{b}"], writes=[f"idxi{b}"])
                P.op("dve", lambda e, b=b: e.tensor_tensor(out=valt[b][:], in0=pI[:, :, 2], in1=pI[:, :, 3], op=ALU.add), reads=["pIs"], writes=[f"valt{b}"])
                P.op("dve", lambda e, b=b: e.tensor_tensor(out=valt[b][:], in0=valt[b][:], in1=pI[:, :, 4], op=ALU.add), reads=["pIs", f"valt{b}"], writes=[f"valt{b}"])
                for sb in range(4):
                    P.op("pool", lambda e, sb=sb, b=b: e.indirect_dma_start(out=xs[b][:, sb, :], out_offset=None, in_=h2buf, in_offset=bass.IndirectOffsetOnAxis(ap=idxi[b][:, sb:sb + 1], axis=0)), reads=[f"idxi{b}", "h2buf"], writes=[f"xs{b}_{sb}"], dma=True)

            def prep_tr(ee):
                b = ee % 2
                for dk in range(8):
                    bank, bv = (6, MK6b) if dk % 2 == 0 else (7, MK7b)
                    for sb in range(4):
                        P.op("pe", lambda e, dk=dk, sb=sb, b=b, bv=bv: e.transpose(out=bv[:, sb * 128:(sb + 1) * 128], in_=xs[b][:, sb, dk * 128:(dk + 1) * 128], identity=idb), reads=[f"xs{b}_{sb}", "cb"], writes=[f"MK{bank}"])
                    P.op("act", lambda e, dk=dk, b=b, bv=bv: e.activation(out=xsT[b][:, dk, :], in_=bv[:, 0:512], func=AF.Copy, scale=gf3[:, dk:dk + 1]), reads=[f"MK{bank}", "gf3"], writes=[f"xsT{b}"])

            gu_i = [0]
            d_i = [0]

            def gateup(ee, nxt):
                b = ee % 2
                npair = NFC // 2
                groups = [list(range(3 * pr, min(3 * pr + 3, NT))) for pr in range(npair)]
                groups[-1] = list(range(3 * (npair - 1), NT))
                for pr in range(npair):
                    i = gu_i[0]
                    gu_i[0] += 1
                    sl = i % NGU
                    for fl in range(2):
                        fc = pr * 2 + fl
                        gb = fc % 2
                        ub = 2 + fc % 2
                        for dk in range(8):
                            P.op("pe", lambda e, dk=dk, fl=fl, sl=sl, gb=gb, b=b: e.matmul(MK[gb][:], lhsT=wg[sl][:, dk, fl * 128:(fl + 1) * 128], rhs=xsT[b][:, dk, :], start=(dk == 0), stop=(dk == 7)), reads=[f"wg{sl}", f"xsT{b}"], writes=[f"MK{gb}"])
                        for dk in range(8):
                            P.op("pe", lambda e, dk=dk, fl=fl, sl=sl, ub=ub, b=b: e.matmul(MK[ub][:], lhsT=wu[sl][:, dk, fl * 128:(fl + 1) * 128], rhs=xsT[b][:, dk, :], start=(dk == 0), stop=(dk == 7)), reads=[f"wu{sl}", f"xsT{b}"], writes=[f"MK{ub}"])
                        P.op("act", lambda e, gb=gb: e.activation(out=sgt[gb][:], in_=MK[gb][:], func=AF.Silu), reads=[f"MK{gb}"], writes=[f"sgt{gb}"])
                        P.op("dve", lambda e, gb=gb, ub=ub, fc=fc, b=b: e.tensor_tensor(out=hTm[b][:, fc, :], in0=MK[ub][:], in1=sgt[gb][:], op=ALU.mult), reads=[f"MK{ub}", f"sgt{gb}"], writes=[f"hTm{b}"])
                    emit_gu_load()
                    if nxt is not None:
                        if pr >= 1:
                            for n in groups[pr - 1]:
                                sel_mm(nxt, n)
                        for n in groups[pr]:
                            sel_onehot(nxt, n)
                if nxt is not None:
                    for n in groups[npair - 1]:
                        sel_mm(nxt, n)

            def down(ee, nxt):
                b = ee % 2
                for half in range(2):
                    for pr in range(NFC // 2):
                        i = d_i[0]
                        d_i[0] += 1
                        sl = i % ND
                        for fl in range(2):
                            fc = pr * 2 + fl
                            for sb in range(4):
                                P.op("pe", lambda e, fc=fc, fl=fl, sb=sb, sl=sl, b=b: e.matmul(MK[sb][:], lhsT=hTm[b][:, fc, sb * 128:(sb + 1) * 128], rhs=wd[sl][:, fl, :], start=(fc == 0), stop=(fc == NFC - 1)), reads=[f"hTm{b}", f"wd{sl}"], writes=[f"MK{sb}"])
                        emit_d_load()
                    for sb in range(4):
                        if sb < 2:
                            P.op("act", lambda e, sb=sb, half=half, b=b: e.activation(out=yt[:, sb, half * 512:(half + 1) * 512], in_=MK[sb][:], func=AF.Copy, scale=valt[b][:, sb:sb + 1]), reads=[f"MK{sb}", f"valt{b}"], writes=[f"yt{sb}"])
                        else:
                            P.op("dve", lambda e, sb=sb, half=half, b=b: e.tensor_scalar(out=yt[:, sb, half * 512:(half + 1) * 512], in0=MK[sb][:], scalar1=valt[b][:, sb:sb + 1], scalar2=None, op0=ALU.mult), reads=[f"MK{sb}", f"valt{b}"], writes=[f"yt{sb}"])
                    if half == 0 and nxt is not None:
                        prep_tr(nxt)
                for sb in range(4):
                    P.op("pool", lambda e, sb=sb, b=b: e.indirect_dma_start(out=x1buf, out_offset=bass.IndirectOffsetOnAxis(ap=idxi[b][:, sb:sb + 1], axis=0), in_=yt[:, sb, :], in_offset=None, compute_op=ALU.add), reads=[f"idxi{b}", f"yt{sb}"] + [f"x1p{1 - b}_{q}" for q in range(4)], writes=[f"x1p{b}_{sb}"], dma=True)

            import os
            NEX = int(os.environ.get("KDBG_NEX", NE))
            for n in range(NT):
                sel_onehot(0, n)
                sel_mm(0, n)
            prep_idx(0)
            prep_tr(0)
            for ee in range(NEX):
                nxt = ee + 1 if ee + 1 < NEX else None
                gateup(ee, nxt)
                if nxt is not None:
                    prep_idx(nxt)
                down(ee, nxt)
            P.flush()
        if stop_after <= 4:
            P.finish()
            return nc

        with ExitStack() as es:
            Es = es.enter_context
            gfin = Es(nc.sbuf_tensor("gfin", [128, D], F32))
            xf = [Es(nc.sbuf_tensor(f"xf{i}", [128, D], F32)) for i in range(3)]
            of = [Es(nc.sbuf_tensor(f"of{i}", [128, D], F32)) for i in range(3)]
            fj = Es(nc.sbuf_tensor("fj", [128, D], BF16))
            fs = [Es(nc.sbuf_tensor(f"fs{i}", [128, 1], F32)) for i in range(3)]
            P.dma("sp", gfin[:], g_final.partition_broadcast(128), writes=["gfin"])
            for i in range(NT):
                a = i % 3
                rows = slice(i * 128, (i + 1) * 128)
                P.dma("sp", xf[a][:], x1buf[rows, :], reads=["x1buf"] + [f"x1p{pp}_{q}" for pp in range(2) for q in range(4)], writes=[f"xf{a}"])
                P.op("act", lambda e, a=a: e.activation(out=fj[:], in_=xf[a][:], func=AF.Square, accum_out=fs[a][:]), reads=[f"xf{a}"], writes=["fj", f"fs{a}"])
                rstd_from_ss(fs[a][:], f"fs{a}", D)
                P.op("dve", lambda e, a=a: e.scalar_tensor_tensor(out=of[a][:], in0=xf[a][:], scalar=fs[a][:], in1=gfin[:], op0=ALU.mult, op1=ALU.mult), reads=[f"xf{a}", f"fs{a}", "gfin"], writes=[f"of{a}"])
                P.dma("pool", out[rows, :], of[a][:], reads=[f"of{a}"], writes=[f"out{i}"])
            P.flush()
        P.finish()
    return nc


def make_in_maps(inputs, ncores=4, moe=False):
    cfa_np, CO, cb_np = _consts()
    x = np.asarray(inputs["x"], np.float32)
    posn = np.asarray(inputs["positions"]).astype(np.int32)
    lamv = np.concatenate([np.asarray(inputs[k], np.float32).reshape(1, 64) for k in ("lam_q1", "lam_k1", "lam_q2", "lam_k2")], axis=0)
    fixt = np.stack([_fix_tables(0), _fix_tables(1)], axis=0).astype(np.float32)
    maps = []
    for c in range(ncores):
        b = c
        xb = np.ascontiguousarray(x[b])
        xh = np.zeros((2, 256, D), np.float32)
        xh[0, 128:256] = xb[HQ:HQ + 128]
        xh[1, 0:128] = xb[HQ - 128:HQ]
        m = {
            "x": xb, "xhalo": xh, "pos": np.ascontiguousarray(posn[b]),
            "g_mix": np.asarray(inputs["g_mix"], np.float32).reshape(D),
            "w_in": np.asarray(inputs["w_in"], np.float32)[0],
            "w_pool_mix": np.asarray(inputs["w_pool_mix"], np.float32)[0],
            "pool_scale": np.asarray(inputs["pool_scale"], np.float32).reshape(D),
            "w_pool_out": np.asarray(inputs["w_pool_out"], np.float32)[0],
            "lamv": lamv,
            "g_subln": np.asarray(inputs["g_subln"], np.float32).reshape(128),
            "w_attn_out": np.asarray(inputs["w_attn_out"], np.float32)[0],
            "w_out": np.asarray(inputs["w_out"], np.float32)[0],
            "g_ffn": np.asarray(inputs["g_ffn"], np.float32).reshape(D),
            "w_router": np.asarray(inputs["w_router"], np.float32)[0],
            "w_gate": np.asarray(inputs["w_gate"], np.float32)[0],
            "w_up": np.asarray(inputs["w_up"], np.float32)[0],
            "w_down": np.asarray(inputs["w_down"], np.float32)[0],
            "g_final": np.asarray(inputs["g_final"], np.float32).reshape(D),
            "cfa": cfa_np, "cba": cb_np, "fixt": fixt,
        }
        if not moe:
            for k in ("w_gate", "w_up", "w_down"):
                m.pop(k)
        maps.append(m)
    return maps


def kernel(**inputs):
    nc = build()
    maps = make_in_maps(inputs, 4, moe=True)
    res = run_bass_kernel_spmd(nc, maps, core_ids=list(range(4)))
    return np.stack([np.asarray(r["out"], np.float32) for r in res.results], axis=0)
```
